# Optimizing a Trainium2 kernel written in Bass

```python
import math
import jax, jax.numpy as jnp
from jax import lax
import numpy as np

D_MODEL = 2048
BATCH = 8
SEQ = 4096
DEPTH = 4

N_MIXERS = 3
N_RET_LAYERS = len(range(0, DEPTH, N_MIXERS))
N_CONV_LAYERS = len(range(1, DEPTH, N_MIXERS))
N_DIFF_LAYERS = len(range(2, DEPTH, N_MIXERS))

RET_HEADS = 8
RET_DK = D_MODEL // RET_HEADS
RET_DV = 2 * RET_DK
RET_CHUNK = 128

CONV_WIDTH = 31

DIFF_HEADS = 8
DIFF_DH = D_MODEL // DIFF_HEADS // 2
Q_BLOCK = 128

D_FF = 5632
FFN_CONV_WIDTH = 3

ROPE_THETA = 10000.0
EPS = 1e-6
NEG_INF = -1e30

kernel_name = "hybrid_retention_conformer_diffattn_trunk"


def rms_norm(x, g):
    xf = x.astype(jnp.float32)
    y = xf * lax.rsqrt(jnp.mean(xf * xf, axis=-1, keepdims=True) + EPS)
    return (y * g.astype(jnp.float32)).astype(x.dtype)


def rotary(t, positions):
    d = t.shape[-1]
    inv_freq = ROPE_THETA ** (-jnp.arange(0, d, 2, dtype=jnp.float32) / d)
    ang = positions.astype(jnp.float32)[..., None] * inv_freq
    ang = ang.reshape(ang.shape[:2] + (1,) * (t.ndim - 3) + (d // 2,))
    cos, sin = jnp.cos(ang), jnp.sin(ang)
    tf = t.astype(jnp.float32)
    t1, t2 = tf[..., : d // 2], tf[..., d // 2:]
    return jnp.concatenate([t1 * cos - t2 * sin, t2 * cos + t1 * sin], axis=-1).astype(t.dtype)


def causal_dwconv(x, w):
    width, ch = w.shape
    return lax.conv_general_dilated(
        x, w[:, None, :].astype(x.dtype), window_strides=(1,), padding=[(width - 1, 0)],
        dimension_numbers=('NWC', 'WIO', 'NWC'), feature_group_count=ch)


def ada_modulation(c, w, b):
    m = jax.nn.silu(c) @ w + b
    shift, scale, gate = jnp.split(m[:, None, :], 3, axis=-1)
    return shift, scale, gate


def retention(h, positions, w_in, w_out):
    B, S, _ = h.shape
    H, dk, dv = RET_HEADS, RET_DK, RET_DV
    f32 = jnp.float32
    q, k, v, g = jnp.split(h @ w_in, [H * dk, 2 * H * dk, 2 * H * dk + H * dv], axis=-1)
    q = rotary(q.reshape(B, S, H, dk), positions).astype(f32)
    k = rotary(k.reshape(B, S, H, dk), positions).astype(f32) * (dk ** -0.5)
    v = v.reshape(B, S, H, dv).astype(f32)
    log_gamma = jnp.log1p(-jnp.exp2(-5.0 - jnp.arange(H, dtype=f32)))
    idx = jnp.arange(RET_CHUNK, dtype=f32)
    rel = idx[:, None] - idx[None, :]
    decay = jnp.where(rel >= 0, jnp.exp(jnp.maximum(rel, 0.0)[None] * log_gamma[:, None, None]), 0.0)
    cross_decay = jnp.exp((idx + 1.0)[None] * log_gamma[:, None])
    state_decay = jnp.exp((RET_CHUNK - 1.0 - idx)[None] * log_gamma[:, None])
    chunk_decay = jnp.exp(RET_CHUNK * log_gamma)
    n_chunks = S // RET_CHUNK

    def to_chunks(t):
        return t.reshape(B, n_chunks, RET_CHUNK, H, t.shape[-1]).transpose(1, 0, 3, 2, 4)

    def step(state, qkv):
        qc, kc, vc = qkv
        scores = jnp.einsum('bhqd,bhkd->bhqk', qc, kc) * decay
        inner = jnp.einsum('bhqk,bhkv->bhqv', scores, vc)
        cross = jnp.einsum('bhqd,bhdv->bhqv', qc, state) * cross_decay[:, :, None]
        new_state = state * chunk_decay[:, None, None] + jnp.einsum(
            'bhkd,bhkv->bhdv', kc * state_decay[:, :, None], vc)
        return new_state, inner + cross

    state0 = jnp.zeros((B, H, dk, dv), f32)
    _, o = lax.scan(step, state0, (to_chunks(q), to_chunks(k), to_chunks(v)))
    o = o.transpose(1, 0, 3, 2, 4).reshape(B, S, H, dv)
    mu = jnp.mean(o, axis=-1, keepdims=True)
    var = jnp.mean(jnp.square(o - mu), axis=-1, keepdims=True)
    o = ((o - mu) * lax.rsqrt(var + EPS)).reshape(B, S, H * dv).astype(h.dtype)
    return (jax.nn.silu(g) * o) @ w_out


def conformer_conv(h, w_pw1, b_pw1, w_dw, b_dw, ln_g, ln_b, w_pw2, b_pw2):
    a, b = jnp.split(h @ w_pw1 + b_pw1, 2, axis=-1)
    u = a * jax.nn.sigmoid(b)
    u = causal_dwconv(u, w_dw) + b_dw
    uf = u.astype(jnp.float32)
    mu = jnp.mean(uf, axis=-1, keepdims=True)
    var = jnp.mean(jnp.square(uf - mu), axis=-1, keepdims=True)
    un = (uf - mu) * lax.rsqrt(var + EPS) * ln_g.astype(jnp.float32) + ln_b.astype(jnp.float32)
    u = jax.nn.silu(un).astype(h.dtype)
    return u @ w_pw2 + b_pw2


def diff_attention(h, positions, w_in, lam_params, subln_g, w_out, layer_idx):
    B, S, D = h.shape
    H, dh = DIFF_HEADS, DIFF_DH
    f32 = jnp.float32
    q, k, v = jnp.split(h @ w_in, 3, axis=-1)
    q = rotary(q.reshape(B, S, H, 2, dh), positions)
    k = rotary(k.reshape(B, S, H, 2, dh), positions)
    v = v.reshape(B, S, H, 2 * dh).astype(f32)
    lambda_init = 0.8 - 0.6 * math.exp(-0.3 * layer_idx)
    lp = lam_params.astype(f32)
    lam = jnp.exp(jnp.sum(lp[0] * lp[1])) - jnp.exp(jnp.sum(lp[2] * lp[3])) + lambda_init
    n_blocks = S // Q_BLOCK
    q_blocks = q.reshape(B, n_blocks, Q_BLOCK, H, 2, dh).transpose(1, 0, 2, 3, 4, 5)
    key_pos = jnp.arange(S)
    scale = dh ** -0.5

    def attend(args):
        q_blk, blk = args
        s = jnp.einsum('bqhcd,bkhcd->bhcqk', q_blk, k).astype(f32) * scale
        q_pos = blk * Q_BLOCK + jnp.arange(Q_BLOCK)
        s = jnp.where(key_pos[None, :] <= q_pos[:, None], s, NEG_INF)
        p = jax.nn.softmax(s, axis=-1)
        w = p[:, :, 0] - lam * p[:, :, 1]
        return jnp.einsum('bhqk,bkhv->bqhv', w, v)

    o = lax.map(attend, (q_blocks, jnp.arange(n_blocks)))
    o = o.transpose(1, 0, 2, 3, 4).reshape(B, S, H, 2 * dh)
    o = rms_norm(o, subln_g) * (1.0 - lambda_init)
    return o.reshape(B, S, D).astype(h.dtype) @ w_out


def conv_ffn(h, w_in, w_dw, w_down):
    gate, up = jnp.split(h @ w_in, 2, axis=-1)
    gate = causal_dwconv(gate, w_dw)
    return (jax.nn.silu(gate) * up) @ w_down


def setup_inputs(seed: int = 0) -> dict:
    key = jax.random.key(seed)
    ks = jax.random.split(key, 24)
    f32 = jnp.float32
    D, F = D_MODEL, D_FF
    nrm = lambda k, shape, s: jax.random.normal(k, shape, f32) * s
    ret_in_w = 2 * RET_HEADS * RET_DK + 2 * RET_HEADS * RET_DV
    return {
        'x': jax.random.normal(ks[0], (BATCH, SEQ, D), f32),
        'c': jax.random.normal(ks[1], (BATCH, D), f32),
        'positions': jnp.broadcast_to(jnp.arange(SEQ, dtype=jnp.int32)[None, :], (BATCH, SEQ)),
        'mod_w': nrm(ks[2], (DEPTH, 2, D, 3 * D), 0.5 * D ** -0.5),
        'mod_b': nrm(ks[3], (DEPTH, 2, 3 * D), 0.02),
        'norm_g': 1.0 + nrm(ks[4], (DEPTH, 4, D), 0.02),
        'ret_w_in': nrm(ks[5], (N_RET_LAYERS, D, ret_in_w), D ** -0.5),
        'ret_w_out': nrm(ks[6], (N_RET_LAYERS, RET_HEADS * RET_DV, D), (RET_HEADS * RET_DV) ** -0.5),
        'conv_w_pw1': nrm(ks[7], (N_CONV_LAYERS, D, 2 * D), D ** -0.5),
        'conv_b_pw1': nrm(ks[8], (N_CONV_LAYERS, 2 * D), 0.02),
        'conv_w_dw': nrm(ks[9], (N_CONV_LAYERS, CONV_WIDTH, D), CONV_WIDTH ** -0.5),
        'conv_b_dw': nrm(ks[10], (N_CONV_LAYERS, D), 0.02),
        'conv_ln_g': 1.0 + nrm(ks[11], (N_CONV_LAYERS, D), 0.02),
        'conv_ln_b': nrm(ks[12], (N_CONV_LAYERS, D), 0.02),
        'conv_w_pw2': nrm(ks[13], (N_CONV_LAYERS, D, D), D ** -0.5),
        'conv_b_pw2': nrm(ks[14], (N_CONV_LAYERS, D), 0.02),
        'diff_w_in': nrm(ks[15], (N_DIFF_LAYERS, D, 3 * D), D ** -0.5),
        'diff_lambda': nrm(ks[16], (N_DIFF_LAYERS, 4, DIFF_DH), 0.1),
        'diff_subln_g': 1.0 + nrm(ks[17], (N_DIFF_LAYERS, 2 * DIFF_DH), 0.02),
        'diff_w_out': nrm(ks[18], (N_DIFF_LAYERS, D, D), D ** -0.5),
        'ffn_w_in': nrm(ks[19], (DEPTH, D, 2 * F), D ** -0.5),
        'ffn_w_dw': nrm(ks[20], (DEPTH, FFN_CONV_WIDTH, F), FFN_CONV_WIDTH ** -0.5),
        'ffn_w_down': nrm(ks[21], (DEPTH, F, D), F ** -0.5),
    }


def reference(x, c, positions, mod_w, mod_b, norm_g, ret_w_in, ret_w_out,
              conv_w_pw1, conv_b_pw1, conv_w_dw, conv_b_dw, conv_ln_g, conv_ln_b,
              conv_w_pw2, conv_b_pw2, diff_w_in, diff_lambda, diff_subln_g, diff_w_out,
              ffn_w_in, ffn_w_dw, ffn_w_down):
    for i in range(DEPTH):
        kind, slot = i % N_MIXERS, i // N_MIXERS
        shift, scale, gate = ada_modulation(c, mod_w[i, 0], mod_b[i, 0])
        h = rms_norm(x, norm_g[i, 0]) * (1.0 + scale) + shift
        if kind == 0:
            y = retention(h, positions, ret_w_in[slot], ret_w_out[slot])
        elif kind == 1:
            y = conformer_conv(h, conv_w_pw1[slot], conv_b_pw1[slot], conv_w_dw[slot], conv_b_dw[slot],
                               conv_ln_g[slot], conv_ln_b[slot], conv_w_pw2[slot], conv_b_pw2[slot])
        else:
            y = diff_attention(h, positions, diff_w_in[slot], diff_lambda[slot], diff_subln_g[slot],
                               diff_w_out[slot], i)
        x = x + gate * rms_norm(y, norm_g[i, 1])
        shift, scale, gate = ada_modulation(c, mod_w[i, 1], mod_b[i, 1])
        h = rms_norm(x, norm_g[i, 2]) * (1.0 + scale) + shift
        y = conv_ffn(h, ffn_w_in[i], ffn_w_dw[i], ffn_w_down[i])
        x = x + gate * rms_norm(y, norm_g[i, 3])
    return x
```

```python
import math
from contextlib import ExitStack

import numpy as np
import concourse.bass as bass
import concourse.mybir as mybir
from concourse.bass_utils import run_bass_kernel_spmd

F32 = mybir.dt.float32
BF16 = mybir.dt.bfloat16
I32 = mybir.dt.int32
AF = mybir.ActivationFunctionType
ALU = mybir.AluOpType
AX = mybir.AxisListType

D = 2048
DC = 16
T = 512
DFF = 5632
FC = 44
EPS = 1e-6
DEPTH = 4
RET_H, RET_DK, RET_DV = 8, 256, 512
DIFF_H, DIFF_DH = 8, 128
CONV_W = 31
ROPE_THETA = 10000.0


class Buf:
    __slots__ = ("name", "w", "r", "sem", "semv")

    def __init__(self, name):
        self.name = name
        self.w = None
        self.r = []
        self.sem = None
        self.semv = 0


class Ctx:
    def __init__(self, nc):
        self.nc = nc
        self.es = ExitStack()
        self.eng = {"pe": nc.tensor, "act": nc.scalar, "dve": nc.vector, "pool": nc.gpsimd, "sp": nc.sync}
        self.cnt = {}
        for n in ("pe", "act", "dve", "pool"):
            self.cnt[n] = [self.es.enter_context(nc.semaphore("c_" + n)), 0]
        self.waited = {}
        self.nsem = 4

    def sb(self, name, shape, dt):
        return self.es.enter_context(self.nc.sbuf_tensor(name, shape, dt))

    def psum(self, name, shape, dt):
        return self.es.enter_context(self.nc.psum_tensor(name, shape, dt))

    def dram(self, name, shape, dt):
        return self.nc.dram_tensor(name, shape, dt, kind="Internal").ap()

    def wait(self, cons, t):
        if t is None:
            return
        sem, val = t
        key = (cons, id(sem))
        if self.waited.get(key, 0) >= val:
            return
        if cons == "pe" and sem is self.cnt["pe"][0]:
            return
        self.eng[cons].wait_ge(sem, val)
        self.waited[key] = val

    def _deps(self, cons, reads, writes):
        for b in reads:
            self.wait(cons, b.w)
        for b in writes:
            self.wait(cons, b.w)
            for t in b.r:
                self.wait(cons, t)

    @staticmethod
    def _addr(b, t):
        for i, (s, v) in enumerate(b.r):
            if s is t[0]:
                if v < t[1]:
                    b.r[i] = t
                return
        b.r.append(t)

    def _upd(self, t, reads, writes):
        for b in reads:
            self._addr(b, t)
        for b in writes:
            b.w = t
            b.r = []

    def op(self, e, fn, reads=(), writes=(), tick=True):
        self._deps(e, reads, writes)
        inst = fn()
        c = self.cnt[e]
        if tick:
            c[1] += 1
            inst.then_inc(c[0], 1)
            t = (c[0], c[1])
        else:
            t = (c[0], c[1] + 1)
        self._upd(t, reads, writes)
        return t

    def dma(self, q, out, in_, reads=(), writes=(), sembuf=None, **kw):
        self._deps(q, reads, writes)
        b = sembuf if sembuf is not None else (writes[0] if writes else reads[0])
        if b.sem is None:
            b.sem = self.es.enter_context(self.nc.semaphore("d%d" % self.nsem))
            self.nsem += 1
        b.semv += 16
        self.eng[q].dma_start(out=out, in_=in_, **kw).then_inc(b.sem, 16)
        t = (b.sem, b.semv)
        self._upd(t, reads, writes)
        return t

    def drain(self, e, bufs):
        for b in bufs:
            self.wait(e, b.w)
            for t in b.r:
                self.wait(e, t)


def build(S, layer_kinds, with_mixer=True, with_ffn=True):
    NT = S // T
    nc = bass.Bass("TRN2", target_bir_lowering=False)
    cx = Ctx(nc)
    es = cx.es

    def din(name, shape, dt=F32):
        return nc.dram_tensor(name, shape, dt, kind="ExternalInput").ap()

    x_in = din("x", [S, D])
    c_in = din("c", [DC, 128])
    pos_in = din("positions", [1, S], I32)
    mod_w = din("mod_w", [DEPTH, 2, D, 3 * D])
    mod_b = din("mod_b", [DEPTH * 2, 3 * D])
    norm_g = din("norm_g", [DEPTH * 4 * DC, 128])
    ret_w_in = din("ret_w_in", [2, D, 12288])
    ret_w_out = din("ret_w_out", [2, 4096, D])
    conv_w_pw1 = din("conv_w_pw1", [1, D, 2 * D])
    conv_b_pw1 = din("conv_b_pw1", [32, 128])
    conv_w_dw = din("conv_w_dw", [CONV_W * DC, 128])
    conv_vecs = din("conv_vecs", [4 * DC, 128])
    conv_w_pw2 = din("conv_w_pw2", [1, D, D])
    diff_w_in = din("diff_w_in", [1, D, 3 * D])
    diff_lambda = din("diff_lambda", [1, 4 * 128])
    diff_subln_g = din("diff_subln_g", [1, 256])
    diff_w_out = din("diff_w_out", [1, D, D])
    ffn_w_in = din("ffn_w_in", [DEPTH, D, 2 * DFF])
    ffn_w_dw = din("ffn_w_dw", [DEPTH * 3 * FC, 128])
    ffn_w_down = din("ffn_w_down", [DEPTH, DFF, D])
    ident_in = din("identc", [128, 128])
    ropec_in = din("ropec", [128, 4])
    retc_in = din("retc", [128, 8 * 128 * 2 + 8])
    out_d = nc.dram_tensor("out", [S, D], F32, kind="ExternalOutput").ap()
    kinds = [k for (k, _, _) in layer_kinds] if with_mixer else []
    tabs = {}
    tabs_b = Buf("tabs")
    if 0 in kinds:
        tabs["r"] = (cx.dram("cosR", [128, S], F32), cx.dram("sinR", [128, S], F32))
    if 2 in kinds:
        tabs["d"] = (cx.dram("cosD", [128, S], F32), cx.dram("sinD", [128, S], F32))

    xT = cx.dram("xT", [D, S], F32)
    xT_b = Buf("xT")
    modrow_d = cx.dram("modrow", [DEPTH * 2, 3 * D], F32)
    modrow_b = Buf("modrow")

    ident = cx.sb("ident", [128, 128], F32)
    ident_bf = cx.sb("ident_bf", [128, 128], BF16)
    ones_bf = cx.sb("ones_bf", [128, 128], BF16)
    cT = cx.sb("cT", [128, DC], F32)
    gT = cx.sb("gT", [128, DEPTH * 4 * DC], F32)
    modT = cx.sb("modT", [128, DEPTH * 2 * 48], F32)
    AT = cx.sb("AT", [128, DEPTH * 2 * DC], F32)
    GT = cx.sb("GT", [128, DEPTH * 2 * DC], F32)
    fdwT = cx.sb("fdwT", [128, DEPTH * 3 * FC], F32)
    epsb = cx.sb("epsb", [128, 1], F32)
    consts_b = Buf("consts")

    psb = [cx.psum("ps%d" % i, [128, 512], F32) for i in range(7)]
    psB = [Buf("ps%d" % i) for i in range(7)]
    pst = cx.psum("pst", [128, 1024], BF16)
    pst_b = [Buf("pst0"), Buf("pst1")]

    xy = cx.sb("xy", [128, DC, T], F32)
    xy_b = Buf("xy")
    xc = [cx.sb("xc%d" % i, [128, T], F32) for i in range(2)]
    xc_b = [Buf("xc%d" % i) for i in range(2)]
    hbuf = cx.sb("hbuf", [128, DC, T], BF16)
    h_b = Buf("h")
    sq = [cx.sb("sq%d" % i, [128, T], BF16) for i in range(2)]
    sq_b = [Buf("sq%d" % i) for i in range(2)]
    tmpf = [cx.sb("tmpf%d" % i, [128, T], F32) for i in range(2)]
    tmpf_b = [Buf("tmpf%d" % i) for i in range(2)]
    rstd = cx.sb("rstd", [128, T], F32)
    rstd_b = Buf("rstd")

    def act(fn, reads=(), writes=()):
        return cx.op("act", fn, reads, writes)

    def dve(fn, reads=(), writes=()):
        return cx.op("dve", fn, reads, writes)

    def pool(fn, reads=(), writes=()):
        return cx.op("pool", fn, reads, writes)

    def mm(out_ap, lhsT, rhs, start, stop, reads, writes, tick=None):
        return cx.op("pe", lambda: nc.tensor.matmul(out_ap, lhsT, rhs, start=start, stop=stop),
                     reads, writes, tick=(stop if tick is None else tick))

    def tr(out_ap, in_ap, idn, reads, writes, tick=True):
        return cx.op("pe", lambda: nc.tensor.transpose(out_ap, in_ap, idn), reads, writes, tick=tick)

    class WS:
        def __init__(self, name, src, K, N, SW, col0=0):
            self.KC = K // 128
            self.NS = N // SW
            self.SW = SW
            self.t = cx.dram(name, [128, self.NS, self.KC, SW], BF16)
            self.b = Buf(name)
            self.b.sem = es.enter_context(nc.semaphore("w_" + name))
            self.todo = []
            for k in range(self.KC):
                self.todo.append((self.t[:, :, k, :],
                                  src[k * 128:(k + 1) * 128, col0:col0 + N].rearrange("p (s w) -> p s w", w=SW)))
            self.b.w = (self.b.sem, 16 * len(self.todo))

        def issue(self, n):
            for _ in range(min(n, len(self.todo))):
                o, i = self.todo.pop(0)
                nc.gpsimd.dma_start(out=o, in_=i).then_inc(self.b.sem, 16)

        def slab(self, s):
            return self.t[:, s, :, :]

    tile_hook = [None]

    stg = cx.sb("stg", [128, 128], F32)
    stg_b = Buf("stg")

    def load_T(dst_ap, src_rows, R, src_b=None):
        cx.dma("sp", stg[0:R, :], src_rows, reads=(() if src_b is None else (src_b,)), writes=(stg_b,), sembuf=stg_b)
        tr(psb[0][:, 0:R], stg[0:R, :], ident[0:R, 0:R], reads=(stg_b, consts_b), writes=(psB[0],))
        dve(lambda: nc.vector.tensor_copy(out=dst_ap, in_=psb[0][:, 0:R]), reads=(psB[0],), writes=(consts_b,))

    cx.dma("sp", ident[:, :], ident_in[:, :], writes=(consts_b,))
    dve(lambda: nc.vector.tensor_copy(out=ident_bf[:, :], in_=ident[:, :]), reads=(consts_b,), writes=(consts_b,))
    dve(lambda: nc.vector.memset(ones_bf[:, :], 1.0), writes=(consts_b,))
    dve(lambda: nc.vector.memset(epsb[:, :], EPS), writes=(consts_b,))
    load_T(cT[:, :], c_in[:, :], DC)
    act(lambda: nc.scalar.activation(out=cT[:, :], in_=cT[:, :], func=AF.Silu), reads=(consts_b,), writes=(consts_b,))
    for i in range(0, DEPTH * 4 * DC, 128):
        load_T(gT[:, i:i + 128], norm_g[i:i + 128, :], 128)
    nf = DEPTH * 3 * FC
    for i in range(0, nf, 128):
        r = min(128, nf - i)
        load_T(fdwT[:, i:i + r], ffn_w_dw[i:i + r, :], r)

    with ExitStack() as mes:
        mwa = [mes.enter_context(nc.sbuf_tensor("mw%d" % i, [128, DC, 512], F32)) for i in range(2)]
        mw_b = [Buf("mw%d" % i) for i in range(2)]
        mrow = mes.enter_context(nc.sbuf_tensor("mrow", [1, 3 * D], F32))
        mrow_b = Buf("mrow")
        mbias = mes.enter_context(nc.sbuf_tensor("mbias", [1, 3 * D], F32))
        mbias_b = Buf("mbias")
        it = 0
        for (kind, slot, li) in layer_kinds:
            for sub in range(2):
                if (sub == 0 and not with_mixer) or (sub == 1 and not with_ffn):
                    continue
                ls = li * 2 + sub
                cx.dma("sp", mbias[:, :], mod_b[ls:ls + 1, :], writes=(mbias_b,))
                for n in range(12):
                    w = mwa[it % 2]
                    wb = mw_b[it % 2]
                    cx.dma("sp", w[:, :, :],
                           mod_w[li, sub, :, n * 512:(n + 1) * 512].rearrange("(k p) n -> p k n", p=128),
                           writes=(wb,))
                    pb = 1 + (it % 2)
                    for k in range(DC):
                        mm(psb[pb][0:1, :], cT[:, k:k + 1], w[:, k, :], k == 0, k == DC - 1,
                           reads=(wb, consts_b), writes=(psB[pb],))
                    dve(lambda pb=pb, n=n: nc.vector.tensor_tensor(out=mrow[:, n * 512:(n + 1) * 512], in0=psb[pb][0:1, :],
                                                                   in1=mbias[:, n * 512:(n + 1) * 512], op=ALU.add),
                        reads=(psB[pb], mbias_b), writes=(mrow_b,))
                    it += 1
                cx.dma("pool", modrow_d[ls:ls + 1, :], mrow[:, :], reads=(mrow_b,), writes=(modrow_b,), sembuf=mrow_b)
                load_T(modT[:, ls * 48:(ls + 1) * 48], modrow_d[ls, :].rearrange("(r p) -> r p", p=128), 48, modrow_b)
                sc = modT[:, ls * 48 + 16: ls * 48 + 32]
                gt = modT[:, ls * 48 + 32: ls * 48 + 48]
                gpre = gT[:, (li * 4 + 2 * sub) * DC:(li * 4 + 2 * sub + 1) * DC]
                gpost = gT[:, (li * 4 + 2 * sub + 1) * DC:(li * 4 + 2 * sub + 2) * DC]
                dve(lambda sc=sc, gpre=gpre, ls=ls: nc.vector.scalar_tensor_tensor(
                    out=AT[:, ls * DC:(ls + 1) * DC], in0=sc, scalar=1.0, in1=gpre, op0=ALU.add, op1=ALU.mult),
                    reads=(consts_b,), writes=(consts_b,))
                dve(lambda gt=gt, gpost=gpost, ls=ls: nc.vector.tensor_tensor(
                    out=GT[:, ls * DC:(ls + 1) * DC], in0=gt, in1=gpost, op=ALU.mult),
                    reads=(consts_b,), writes=(consts_b,))
        cx.drain("pe", mw_b)
        cx.drain("dve", [mrow_b, mbias_b] + mw_b)
        cx.drain("pool", [mrow_b])
        cx.drain("sp", [mbias_b] + mw_b)

    def tile_sumsq_rstd(src, src_b):
        for c in range(DC):
            i = c % 2
            act(lambda c=c, i=i: nc.scalar.activation(out=sq[i][:, :], in_=src[:, c, :], func=AF.Square),
                reads=(src_b,), writes=(sq_b[i],))
            mm(psb[0][:, :], ones_bf[:, :], sq[i][:, :], c == 0, c == DC - 1, reads=(sq_b[i], consts_b), writes=(psB[0],),
               tick=True)
        act(lambda: nc.scalar.activation(out=rstd[:, :], in_=psb[0][:, :], func=AF.Sqrt, bias=epsb[:, 0:1], scale=1.0 / D),
            reads=(psB[0], consts_b), writes=(rstd_b,))
        dve(lambda: nc.vector.reciprocal(out=rstd[:, :], in_=rstd[:, :]), reads=(rstd_b,), writes=(rstd_b,))

    def load_x_tile(t, dst, dst_b):
        cx.dma("sp", dst[:, :, :], xT[:, t * T:(t + 1) * T].rearrange("(c p) n -> p c n", p=128),
               reads=(xT_b,), writes=(dst_b,))

    def norm_tile(t, ls):
        if tile_hook[0] is not None:
            tile_hook[0](t)
        load_x_tile(t, xy, xy_b)
        tile_sumsq_rstd(xy, xy_b)
        for c in range(DC):
            i = c % 2
            dve(lambda c=c, i=i: nc.vector.scalar_tensor_tensor(
                out=tmpf[i][:, :], in0=xy[:, c, :], scalar=AT[:, ls * DC + c: ls * DC + c + 1], in1=rstd[:, :],
                op0=ALU.mult, op1=ALU.mult), reads=(xy_b, rstd_b, consts_b), writes=(tmpf_b[i],))
            act(lambda c=c, i=i: nc.scalar.activation(
                out=hbuf[:, c, :], in_=tmpf[i][:, :], func=AF.Identity,
                bias=modT[:, ls * 48 + c: ls * 48 + c + 1], scale=1.0), reads=(tmpf_b[i], consts_b), writes=(h_b,))

    def resid_tile(t, ls):
        tile_sumsq_rstd(xy, xy_b)
        for c in range(DC):
            i = c % 2
            cx.dma("sp", xc[i][:, :], xT[c * 128:(c + 1) * 128, t * T:(t + 1) * T], reads=(xT_b,), writes=(xc_b[i],))
            dve(lambda c=c, i=i: nc.vector.scalar_tensor_tensor(
                out=tmpf[i][:, :], in0=xy[:, c, :], scalar=GT[:, ls * DC + c: ls * DC + c + 1], in1=rstd[:, :],
                op0=ALU.mult, op1=ALU.mult), reads=(xy_b, rstd_b, consts_b), writes=(tmpf_b[i],))
            pool(lambda c=c, i=i: nc.gpsimd.tensor_tensor(out=xc[i][:, :], in0=xc[i][:, :], in1=tmpf[i][:, :], op=ALU.add),
                 reads=(tmpf_b[i], xc_b[i]), writes=(xc_b[i],))
            cx.dma("pool", xT[c * 128:(c + 1) * 128, t * T:(t + 1) * T], xc[i][:, :],
                   reads=(xc_b[i],), writes=(xT_b,), sembuf=xc_b[i])

    with ExitStack() as tes:
        xin4 = tes.enter_context(nc.sbuf_tensor("xin4", [128, 4, D], F32))
        xin_b = Buf("xin4")
        for t in range(NT):
            cx.dma("sp", xin4[:, :, :], x_in[t * T:(t + 1) * T, :].rearrange("(b p) f -> p b f", p=128), writes=(xin_b,))
            for c in range(DC):
                pb = 1 + (c % 2)
                for b in range(4):
                    tr(psb[pb][:, b * 128:(b + 1) * 128], xin4[:, b, c * 128:(c + 1) * 128], ident[:, :],
                       reads=(xin_b, consts_b), writes=(psB[pb],), tick=(b == 3))
                act(lambda c=c, pb=pb: nc.scalar.copy(out=xy[:, c, :], in_=psb[pb][:, :]), reads=(psB[pb],), writes=(xy_b,))
            cx.dma("pool", xT[:, t * T:(t + 1) * T].rearrange("(c p) n -> p c n", p=128), xy[:, :, :],
                   reads=(xy_b,), writes=(xT_b,), sembuf=xy_b)
        for e in ("pe", "sp"):
            cx.drain(e, [xin_b])

    ropec = cx.sb("ropec_sb", [128, 4], F32)
    cx.dma("sp", ropec[:, :], ropec_in[:, :], writes=(consts_b,))
    TWO_PI_HI = 6.28125
    TWO_PI_LO = 2.0 * math.pi - 6.28125
    if tabs:
        with ExitStack() as tes:
            posi = tes.enter_context(nc.sbuf_tensor("posi", [128, T], I32))
            posf = tes.enter_context(nc.sbuf_tensor("posf", [128, T], F32))
            ang = tes.enter_context(nc.sbuf_tensor("ang", [128, T], F32))
            uu = tes.enter_context(nc.sbuf_tensor("uu", [128, T], F32))
            ki = tes.enter_context(nc.sbuf_tensor("ki", [128, T], I32))
            kf = tes.enter_context(nc.sbuf_tensor("kf", [128, T], F32))
            rr = [tes.enter_context(nc.sbuf_tensor("rr%d" % i, [128, T], F32)) for i in range(2)]
            tb_ = {n: Buf(n) for n in ("posi", "posf", "ang", "uu", "ki", "kf", "rr0", "rr1")}
            it = 0
            for t in range(NT):
                cx.dma("sp", posi[:, :], pos_in[0:1, t * T:(t + 1) * T].partition_broadcast(128), writes=(tb_["posi"],))
                dve(lambda: nc.vector.tensor_copy(out=posf[:, :], in_=posi[:, :]), reads=(tb_["posi"],), writes=(tb_["posf"],))
                for key, col in (("r", 0), ("d", 1)):
                    if key not in tabs:
                        continue
                    dve(lambda col=col: nc.vector.tensor_scalar(out=ang[:, :], in0=posf[:, :], scalar1=ropec[:, col:col + 1],
                                                                scalar2=None, op0=ALU.mult),
                        reads=(tb_["posf"], consts_b), writes=(tb_["ang"],))
                    for ph, which in ((math.pi / 2, 0), (0.0, 1)):
                        r = rr[it % 2]
                        rb = tb_["rr%d" % (it % 2)]
                        it += 1
                        dve(lambda ph=ph: nc.vector.tensor_scalar(out=uu[:, :], in0=ang[:, :], scalar1=1.0 / (2 * math.pi),
                                                                  scalar2=ph / (2 * math.pi), op0=ALU.mult, op1=ALU.add),
                            reads=(tb_["ang"],), writes=(tb_["uu"],))
                        dve(lambda: nc.vector.tensor_copy(out=ki[:, :], in_=uu[:, :]), reads=(tb_["uu"],), writes=(tb_["ki"],))
                        dve(lambda: nc.vector.tensor_copy(out=kf[:, :], in_=ki[:, :]), reads=(tb_["ki"],), writes=(tb_["kf"],))
                        dve(lambda r=r: nc.vector.scalar_tensor_tensor(out=r[:, :], in0=kf[:, :], scalar=-TWO_PI_HI, in1=ang[:, :],
                                                                       op0=ALU.mult, op1=ALU.add),
                            reads=(tb_["kf"], tb_["ang"]), writes=(rb,))
                        dve(lambda r=r: nc.vector.scalar_tensor_tensor(out=r[:, :], in0=kf[:, :], scalar=-TWO_PI_LO, in1=r[:, :],
                                                                       op0=ALU.mult, op1=ALU.add),
                            reads=(tb_["kf"], rb), writes=(rb,))
                        dve(lambda r=r, ph=ph: nc.vector.tensor_scalar(out=r[:, :], in0=r[:, :], scalar1=ph, scalar2=math.pi,
                                                                       op0=ALU.add, op1=ALU.min),
                            reads=(rb,), writes=(rb,))
                        dve(lambda r=r: nc.vector.tensor_scalar(out=r[:, :], in0=r[:, :], scalar1=-math.pi, scalar2=None,
                                                                op0=ALU.max), reads=(rb,), writes=(rb,))
                        act(lambda r=r: nc.scalar.activation(out=r[:, :], in_=r[:, :], func=AF.Sin), reads=(rb,), writes=(rb,))
                        cx.dma("pool", tabs[key][which][:, t * T:(t + 1) * T], r[:, :], reads=(rb,), writes=(), sembuf=rb)
            for r_ in ("rr0", "rr1"):
                tabs_b.w = None
            cx.drain("dve", list(tb_.values()))
            cx.drain("pool", list(tb_.values()))
            cx.drain("sp", list(tb_.values()))
            cx.drain("act", list(tb_.values()))
    uid = [0]

    def sbt(st, name, shape, dt):
        uid[0] += 1
        return st.enter_context(nc.sbuf_tensor("%s_%d" % (name, uid[0]), shape, dt))

    def conv_layer(li, slot, w1s, w2s):
        ls = li * 2
        with ExitStack() as fes:
            cb1T = sbt(fes, "cb1T", [128, 32], F32)
            cdwT = sbt(fes, "cdwT", [128, CONV_W * DC], F32)
            cvT = sbt(fes, "cvT", [128, 4 * DC], F32)
            load_T(cb1T[:, :], conv_b_pw1[:, :], 32)
            for i in range(0, CONV_W * DC, 128):
                r = min(128, CONV_W * DC - i)
                load_T(cdwT[:, i:i + r], conv_w_dw[i:i + r, :], r)
            load_T(cvT[:, :], conv_vecs[:, :], 64)
            wa = [sbt(fes, "wa", [128, DC, 256], BF16) for i in range(2)]
            wb = [sbt(fes, "wb", [128, DC, 256], BF16) for i in range(2)]
            wa_b = [Buf("wa") for i in range(2)]
            wb_b = [Buf("wb") for i in range(2)]
            ub = [sbt(fes, "ub", [128, T + 30], F32) for i in range(2)]
            ub_b = [Buf("ub") for i in range(2)]
            sgc = [sbt(fes, "sgc", [128, T], F32) for i in range(2)]
            sgc_b = [Buf("sgc") for i in range(2)]
            vbuf = sbt(fes, "vbuf", [128, DC, T], F32)
            v_b = [Buf("v%d" % j) for j in range(DC)]
            chalo = sbt(fes, "chalo", [128, DC, 30], F32)
            chalo_b = Buf("chalo")
            mu = sbt(fes, "mu", [128, T], F32)
            mu_b = Buf("mu")
            m2 = sbt(fes, "m2", [128, T], F32)
            m2_b = Buf("m2")
            dve(lambda: nc.vector.memset(chalo[:, :, :], 0.0), writes=(chalo_b,))
            for t in range(NT):
                norm_tile(t, ls)
                for s2 in range(DC // 2):
                    i3 = s2 % 2
                    cx.dma("sp", wa[i3][:, :, :], w1s.slab(s2), reads=(w1s.b,), writes=(wa_b[i3],))
                    cx.dma("sp", wb[i3][:, :, :], w1s.slab(DC // 2 + s2), reads=(w1s.b,), writes=(wb_b[i3],))
                    for jj in range(2):
                        j = s2 * 2 + jj
                        i2 = j % 2
                        ap_, bp_ = 1 + i2, 3 + i2
                        for k in range(DC):
                            mm(psb[ap_][:, :], wa[i3][:, k, jj * 128:(jj + 1) * 128], hbuf[:, k, :], k == 0, k == DC - 1,
                               reads=(wa_b[i3], h_b), writes=(psB[ap_],))
                        for k in range(DC):
                            mm(psb[bp_][:, :], wb[i3][:, k, jj * 128:(jj + 1) * 128], hbuf[:, k, :], k == 0, k == DC - 1,
                               reads=(wb_b[i3], h_b), writes=(psB[bp_],))
                        u = ub[i2]
                        act(lambda i2=i2, bp_=bp_, j=j: nc.scalar.activation(out=sgc[i2][:, :], in_=psb[bp_][:, :], func=AF.Sigmoid,
                                                                            bias=cb1T[:, DC + j:DC + j + 1], scale=1.0),
                            reads=(psB[bp_], consts_b), writes=(sgc_b[i2],))
                        act(lambda u=u, j=j: nc.scalar.copy(out=u[:, 0:30], in_=chalo[:, j, :]), reads=(chalo_b,), writes=(ub_b[i2],))
                        dve(lambda u=u, i2=i2, ap_=ap_, j=j: nc.vector.scalar_tensor_tensor(
                            out=u[:, 30:T + 30], in0=psb[ap_][:, :], scalar=cb1T[:, j:j + 1], in1=sgc[i2][:, :],
                            op0=ALU.add, op1=ALU.mult), reads=(psB[ap_], sgc_b[i2], consts_b), writes=(ub_b[i2],))
                        act(lambda u=u, j=j: nc.scalar.activation(out=vbuf[:, j, :], in_=u[:, 30:T + 30], func=AF.Identity,
                                                                  bias=cvT[:, j:j + 1], scale=cdwT[:, 30 * DC + j:30 * DC + j + 1]),
                            reads=(ub_b[i2], consts_b), writes=(v_b[j],))
                        for tau in range(30):
                            dve(lambda u=u, j=j, tau=tau: nc.vector.scalar_tensor_tensor(
                                out=vbuf[:, j, :], in0=u[:, tau:tau + T], scalar=cdwT[:, tau * DC + j:tau * DC + j + 1],
                                in1=vbuf[:, j, :], op0=ALU.mult, op1=ALU.add), reads=(ub_b[i2], v_b[j], consts_b), writes=(v_b[j],))
                        dve(lambda u=u, j=j: nc.vector.tensor_copy(out=chalo[:, j, :], in_=u[:, T:T + 30]),
                            reads=(ub_b[i2],), writes=(chalo_b,))
                for j in range(DC):
                    i = j % 2
                    act(lambda j=j, i=i: nc.scalar.copy(out=sq[i][:, :], in_=vbuf[:, j, :]), reads=(v_b[j],), writes=(sq_b[i],))
                    mm(psb[0][:, :], ones_bf[:, :], sq[i][:, :], j == 0, j == DC - 1, reads=(sq_b[i], consts_b), writes=(psB[0],),
                       tick=True)
                for j in range(DC):
                    i = j % 2
                    act(lambda j=j, i=i: nc.scalar.activation(out=sq[i][:, :], in_=vbuf[:, j, :], func=AF.Square),
                        reads=(v_b[j],), writes=(sq_b[i],))
                    mm(psb[5][:, :], ones_bf[:, :], sq[i][:, :], j == 0, j == DC - 1, reads=(sq_b[i], consts_b), writes=(psB[5],),
                       tick=True)
                dve(lambda: nc.vector.tensor_scalar(out=mu[:, :], in0=psb[0][:, :], scalar1=1.0 / D, scalar2=None, op0=ALU.mult),
                    reads=(psB[0],), writes=(mu_b,))
                dve(lambda: nc.vector.tensor_tensor(out=m2[:, :], in0=mu[:, :], in1=mu[:, :], op=ALU.mult),
                    reads=(mu_b,), writes=(m2_b,))
                dve(lambda: nc.vector.scalar_tensor_tensor(out=m2[:, :], in0=psb[5][:, :], scalar=1.0 / D, in1=m2[:, :],
                                                           op0=ALU.mult, op1=ALU.subtract), reads=(psB[5], m2_b), writes=(m2_b,))
                act(lambda: nc.scalar.activation(out=rstd[:, :], in_=m2[:, :], func=AF.Sqrt, bias=epsb[:, 0:1], scale=1.0),
                    reads=(m2_b, consts_b), writes=(rstd_b,))
                dve(lambda: nc.vector.reciprocal(out=rstd[:, :], in_=rstd[:, :]), reads=(rstd_b,), writes=(rstd_b,))
                for j in range(DC):
                    i = j % 2
                    dve(lambda j=j, i=i: nc.vector.tensor_tensor(out=tmpf[i][:, :], in0=vbuf[:, j, :], in1=mu[:, :], op=ALU.subtract),
                        reads=(v_b[j], mu_b), writes=(tmpf_b[i],))
                    dve(lambda j=j, i=i: nc.vector.scalar_tensor_tensor(
                        out=tmpf[i][:, :], in0=tmpf[i][:, :], scalar=cvT[:, DC + j:DC + j + 1], in1=rstd[:, :],
                        op0=ALU.mult, op1=ALU.mult), reads=(tmpf_b[i], rstd_b, consts_b), writes=(tmpf_b[i],))
                    act(lambda j=j, i=i: nc.scalar.activation(out=hbuf[:, j, :], in_=tmpf[i][:, :], func=AF.Silu,
                                                              bias=cvT[:, 2 * DC + j:2 * DC + j + 1], scale=1.0),
                        reads=(tmpf_b[i], consts_b), writes=(h_b,))
                for s2 in range(DC // 2):
                    i3 = s2 % 2
                    cx.dma("sp", wa[i3][:, :, :], w2s.slab(s2), reads=(w2s.b,), writes=(wa_b[i3],))
                    for mi in range(2):
                        m = s2 * 2 + mi
                        yp = yb4[m % 4]
                        for k in range(DC):
                            mm(psb[yp][:, :], wa[i3][:, k, mi * 128:(mi + 1) * 128], hbuf[:, k, :], k == 0, k == DC - 1,
                               reads=(wa_b[i3], h_b), writes=(psB[yp],))
                        act(lambda m=m, yp=yp: nc.scalar.activation(out=xy[:, m, :], in_=psb[yp][:, :], func=AF.Identity,
                                                                    bias=cvT[:, 3 * DC + m:3 * DC + m + 1], scale=1.0),
                            reads=(psB[yp], consts_b), writes=(xy_b,))
                resid_tile(t, ls)
            allb = wa_b + wb_b + ub_b + sgc_b + v_b + [chalo_b, mu_b, m2_b, consts_b]
            for e in ("pe", "act", "dve", "sp", "pool"):
                cx.drain(e, allb)

    def ret_layer(li, slot, wqk, wvg, wo):
        ls = li * 2
        cosd, sind = tabs["r"]
        with ExitStack() as fes:
            retc = sbt(fes, "retc", [128, 2056], F32)
            cx.dma("sp", retc[:, :], retc_in[:, :], writes=(consts_b,))
            decT = lambda h: retc[:, h * 128:(h + 1) * 128]
            cdb = lambda h: retc[:, 1024 + h * 128:1024 + (h + 1) * 128]
            sdv = lambda h: retc[:, 2048 + h:2048 + h + 1]
            cs = [sbt(fes, "cs", [128, T], F32) for i in range(4)]
            cs_b = Buf("cs")
            wsl = [sbt(fes, "wsl", [128, DC, 512], BF16) for i in range(2)]
            wsl_b = [Buf("wsl") for i in range(2)]
            qr = sbt(fes, "qr", [128, 2, 2, T], BF16)
            kr = sbt(fes, "kr", [128, 2, 2, T], BF16)
            qr_b = [Buf("qr") for i in range(2)]
            kr_b = [Buf("kr") for i in range(2)]
            vsb = sbt(fes, "vsb", [128, 4, 2, 512], BF16)
            gsb = sbt(fes, "gsb", [128, 4, 2, 512], BF16)
            v_b = [[Buf("v") for hh in range(2)] for tb in range(4)]
            g_b = [[Buf("g") for hh in range(2)] for tb in range(4)]
            oT = sbt(fes, "oT", [128, 8, T], BF16)
            oT_b = Buf("oT")
            stf = sbt(fes, "stf", [128, 8, 2, 512], F32)
            stf_b = [Buf("stf") for h in range(8)]
            sbf = [sbt(fes, "sbf", [128, 2, 512], BF16) for i in range(2)]
            sbf_b = [Buf("sbf") for i in range(2)]
            rt = [sbt(fes, "rt", [128, T], F32) for i in range(4)]
            rt_b = [Buf("rt") for i in range(4)]
            pt = [sbt(fes, "pt", [128, 128], BF16) for i in range(2)]
            pt_b = [Buf("pt") for i in range(2)]
            ktm = [sbt(fes, "ktm", [128, 256], BF16) for i in range(2)]
            ktm_b = [Buf("ktm") for i in range(2)]
            qs = [sbt(fes, "qs", [128, 2, 128], BF16) for i in range(2)]
            qs_b = [Buf("qs") for i in range(2)]
            on = [sbt(fes, "on", [128, 512], F32) for i in range(2)]
            on_b = [Buf("on") for i in range(2)]
            st6 = [sbt(fes, "st6", [128, 6], F32) for i in range(2)]
            mv = [sbt(fes, "mv", [128, 4], F32) for i in range(2)]
            mv_b = [Buf("mv") for i in range(2)]
            dve(lambda: nc.vector.memset(stf[:, :, :, :], 0.0), writes=tuple(stf_b))
            sl = [0]

            def next_slab():
                sl[0] += 1
                return sl[0] % 2

            pc = [0]
            for t in range(NT):
                norm_tile(t, ls)
                cx.dma("sp", cs[0][:, :], cosd[:, t * T:(t + 1) * T], reads=(tabs_b,), writes=(cs_b,))
                cx.dma("sp", cs[1][:, :], sind[:, t * T:(t + 1) * T], reads=(tabs_b,), writes=(cs_b,))
                for i in range(2):
                    dve(lambda i=i: nc.vector.tensor_scalar(out=cs[2 + i][:, :], in0=cs[i][:, :], scalar1=1.0 / 16.0, scalar2=None,
                                                            op0=ALU.mult), reads=(cs_b,), writes=(cs_b,))
                for g in range(4):
                    for hh in range(2):
                        h = 2 * g + hh
                        for which in range(2):
                            si = next_slab()
                            w = wsl[si]
                            cx.dma("sp", w[:, :, 0:256], wqk.slab(which * 8 + h), reads=(wqk.b,), writes=(wsl_b[si],))
                            i2 = pc[0] % 2
                            pc[0] += 1
                            p1, p2 = 1 + i2, 3 + i2
                            for k in range(DC):
                                mm(psb[p1][:, :], w[:, k, 0:128], hbuf[:, k, :], k == 0, k == DC - 1,
                                   reads=(wsl_b[si], h_b), writes=(psB[p1],))
                            for k in range(DC):
                                mm(psb[p2][:, :], w[:, k, 128:256], hbuf[:, k, :], k == 0, k == DC - 1,
                                   reads=(wsl_b[si], h_b), writes=(psB[p2],))
                            c_, s_ = (cs[0], cs[1]) if which == 0 else (cs[2], cs[3])
                            dst = qr if which == 0 else kr
                            dst_b = (qr_b if which == 0 else kr_b)[hh]
                            for (pa, ca, pb_, cb_, half, op_) in ((p1, c_, p2, s_, 0, ALU.subtract), (p2, c_, p1, s_, 1, ALU.add)):
                                a_, b_ = (0, 1) if half == 0 else (2, 3)
                                dve(lambda pa=pa, ca=ca, a_=a_: nc.vector.tensor_tensor(out=rt[a_][:, :], in0=psb[pa][:, :],
                                                                                         in1=ca[:, :], op=ALU.mult),
                                    reads=(psB[pa], cs_b), writes=(rt_b[a_],))
                                dve(lambda pb_=pb_, cb_=cb_, b_=b_: nc.vector.tensor_tensor(out=rt[b_][:, :], in0=psb[pb_][:, :],
                                                                                             in1=cb_[:, :], op=ALU.mult),
                                    reads=(psB[pb_], cs_b), writes=(rt_b[b_],))
                                pool(lambda dst=dst, hh=hh, half=half, a_=a_, b_=b_, op_=op_: nc.gpsimd.tensor_tensor(
                                    out=dst[:, hh, half, :], in0=rt[a_][:, :], in1=rt[b_][:, :], op=op_),
                                    reads=(rt_b[a_], rt_b[b_]), writes=(dst_b,))
                    for hh in range(2):
                        h = 2 * g + hh
                        for which in range(2):
                            si = next_slab()
                            w = wsl[si]
                            cx.dma("sp", w[:, :, :], wvg.slab(which * 8 + h), reads=(wvg.b,), writes=(wsl_b[si],))
                            for tb in range(4):
                                i2 = pc[0] % 2
                                pc[0] += 1
                                vp = 5 + i2
                                for k in range(DC):
                                    mm(psb[vp][:, :], hbuf[:, k, tb * 128:(tb + 1) * 128], w[:, k, :], k == 0, k == DC - 1,
                                       reads=(wsl_b[si], h_b), writes=(psB[vp],))
                                if which == 0:
                                    act(lambda tb=tb, hh=hh, vp=vp: nc.scalar.copy(out=vsb[:, tb, hh, :], in_=psb[vp][:, :]),
                                        reads=(psB[vp],), writes=(v_b[tb][hh],))
                                else:
                                    act(lambda tb=tb, hh=hh, vp=vp: nc.scalar.activation(out=gsb[:, tb, hh, :], in_=psb[vp][:, :],
                                                                                        func=AF.Silu),
                                        reads=(psB[vp],), writes=(g_b[tb][hh],))
                    for tb in range(4):
                        tsl = slice(tb * 128, (tb + 1) * 128)
                        for hh in range(2):
                            h = 2 * g + hh
                            gam128 = float(np.exp(128.0 * np.log1p(-np.exp2(-5.0 - h))))
                            for half in range(2):
                                mm(psb[0][:, hh * 128:(hh + 1) * 128], kr[:, hh, half, tsl], qr[:, hh, half, tsl], half == 0, half == 1,
                                   reads=(kr_b[hh], qr_b[hh]), writes=(psB[0],))
                            dve(lambda hh=hh, h=h: nc.vector.tensor_tensor(out=pt[hh][:, :], in0=psb[0][:, hh * 128:(hh + 1) * 128],
                                                                          in1=decT(h), op=ALU.mult),
                                reads=(psB[0], consts_b), writes=(pt_b[hh],))
                            for half in range(2):
                                tr(pst[:, hh * 512 + half * 128: hh * 512 + (half + 1) * 128], kr[:, hh, half, tsl], ident_bf[:, :],
                                   reads=(kr_b[hh], consts_b), writes=(pst_b[hh],), tick=(half == 1))
                            dve(lambda hh=hh, h=h: nc.vector.tensor_scalar(out=ktm[hh][:, :], in0=pst[:, hh * 512: hh * 512 + 256],
                                                                          scalar1=sdv(h), scalar2=None, op0=ALU.mult),
                                reads=(pst_b[hh], consts_b), writes=(ktm_b[hh],))
                            for half in range(2):
                                pool(lambda hh=hh, h=h, half=half, tsl=tsl: nc.gpsimd.tensor_tensor(
                                    out=qs[hh][:, half, :], in0=qr[:, hh, half, tsl], in1=cdb(h), op=ALU.mult),
                                    reads=(qr_b[hh], consts_b), writes=(qs_b[hh],))
                            act(lambda hh=hh, h=h: nc.scalar.copy(out=sbf[hh][:, :, :], in_=stf[:, h, :, :]),
                                reads=(stf_b[h],), writes=(sbf_b[hh],))
                            op_ = 1 + hh
                            mm(psb[op_][:, :], pt[hh][:, :], vsb[:, tb, hh, :], True, False,
                               reads=(pt_b[hh], v_b[tb][hh]), writes=(psB[op_],))
                            mm(psb[op_][:, :], qs[hh][:, 0, :], sbf[hh][:, 0, :], False, False,
                               reads=(qs_b[hh], sbf_b[hh]), writes=(psB[op_],))
                            mm(psb[op_][:, :], qs[hh][:, 1, :], sbf[hh][:, 1, :], False, True,
                               reads=(qs_b[hh], sbf_b[hh]), writes=(psB[op_],))
                            for half in range(2):
                                nsb = 3 + 2 * hh + half
                                mm(psb[nsb][:, :], ktm[hh][:, half * 128:(half + 1) * 128], vsb[:, tb, hh, :], True, True,
                                   reads=(ktm_b[hh], v_b[tb][hh]), writes=(psB[nsb],))
                                dve(lambda h=h, half=half, nsb=nsb, gam128=gam128: nc.vector.scalar_tensor_tensor(
                                    out=stf[:, h, half, :], in0=stf[:, h, half, :], scalar=gam128, in1=psb[nsb][:, :],
                                    op0=ALU.mult, op1=ALU.add), reads=(psB[nsb], stf_b[h]), writes=(stf_b[h],))
                            dve(lambda hh=hh, op_=op_: nc.vector.bn_stats(out=st6[hh][:, :], in_=psb[op_][:, :]),
                                reads=(psB[op_],), writes=(mv_b[hh],))
                            dve(lambda hh=hh: nc.vector.bn_aggr(out=mv[hh][:, 0:2], in_=st6[hh][:, :]), reads=(mv_b[hh],), writes=(mv_b[hh],))
                            act(lambda hh=hh: nc.scalar.activation(out=mv[hh][:, 2:3], in_=mv[hh][:, 1:2], func=AF.Sqrt,
                                                                   bias=epsb[:, 0:1], scale=1.0),
                                reads=(mv_b[hh], consts_b), writes=(mv_b[hh],))
                            dve(lambda hh=hh: nc.vector.reciprocal(out=mv[hh][:, 2:3], in_=mv[hh][:, 2:3]), reads=(mv_b[hh],), writes=(mv_b[hh],))
                            dve(lambda hh=hh: nc.vector.scalar_tensor_tensor(out=mv[hh][:, 3:4], in0=mv[hh][:, 0:1], scalar=-1.0,
                                                                             in1=mv[hh][:, 2:3], op0=ALU.mult, op1=ALU.mult),
                                reads=(mv_b[hh],), writes=(mv_b[hh],))
                            act(lambda hh=hh, op_=op_: nc.scalar.activation(out=on[hh][:, :], in_=psb[op_][:, :], func=AF.Identity,
                                                                            bias=mv[hh][:, 3:4], scale=mv[hh][:, 2:3]),
                                reads=(psB[op_], mv_b[hh]), writes=(on_b[hh],))
                            pool(lambda hh=hh, tb=tb: nc.gpsimd.tensor_tensor(out=gsb[:, tb, hh, :], in0=on[hh][:, :],
                                                                              in1=gsb[:, tb, hh, :], op=ALU.mult),
                                 reads=(on_b[hh], g_b[tb][hh]), writes=(g_b[tb][hh],))
                    for tb in range(4):
                        for hh in range(2):
                            for q4 in range(4):
                                jc = hh * 4 + q4
                                tr(pst[:, jc * 128:(jc + 1) * 128], gsb[:, tb, hh, q4 * 128:(q4 + 1) * 128], ident_bf[:, :],
                                   reads=(g_b[tb][hh], consts_b), writes=(pst_b[hh],), tick=(q4 == 3))
                        act(lambda tb=tb: nc.scalar.copy(out=oT[:, :, tb * 128:(tb + 1) * 128],
                                                         in_=pst[:, :].rearrange("p (j n) -> p j n", j=8)),
                            reads=(pst_b[0], pst_b[1]), writes=(oT_b,))
                    for s2 in range(DC // 2):
                        si = next_slab()
                        w = wsl[si]
                        cx.dma("sp", w[:, 0:8, 0:256], wo.t[:, s2, 8 * g:8 * g + 8, :], reads=(wo.b,), writes=(wsl_b[si],))
                        for mi in range(2):
                            m = s2 * 2 + mi
                            yp = 5 + (m % 2)
                            for jj in range(8):
                                mm(psb[yp][:, :], w[:, jj, mi * 128:(mi + 1) * 128], oT[:, jj, :], jj == 0, jj == 7,
                                   reads=(wsl_b[si], oT_b), writes=(psB[yp],))
                            if g == 0:
                                act(lambda m=m, yp=yp: nc.scalar.copy(out=xy[:, m, :], in_=psb[yp][:, :]),
                                    reads=(psB[yp],), writes=(xy_b,))
                            else:
                                dve(lambda m=m, yp=yp: nc.vector.tensor_tensor(out=xy[:, m, :], in0=xy[:, m, :], in1=psb[yp][:, :],
                                                                               op=ALU.add), reads=(psB[yp], xy_b), writes=(xy_b,))
                resid_tile(t, ls)
            allb = (wsl_b + qr_b + kr_b + [b for r in v_b for b in r] + [b for r in g_b for b in r] + [oT_b, cs_b, consts_b]
                    + stf_b + sbf_b + rt_b + pt_b + ktm_b + qs_b + on_b + mv_b)
            for e in ("pe", "act", "dve", "sp", "pool"):
                cx.drain(e, allb)

    yb4 = [5, 6, 1, 2]

    tric_in = din("tric", [128, 128])

    class WSperm:
        def __init__(self, name, src):
            self.t = cx.dram(name, [128, 16, DC, 256], BF16)
            self.b = Buf(name)
            self.b.sem = es.enter_context(nc.semaphore("w_" + name))
            self.todo = []
            for k in range(DC):
                sv = src[k * 128:(k + 1) * 128, 0:4096].rearrange("p (s c hf f) -> p s c hf f", s=16, c=2, hf=2, f=64)
                for hf in range(2):
                    self.todo.append((self.t[:, :, k, hf * 128:(hf + 1) * 128].rearrange("p s (c f) -> p s c f", c=2),
                                      sv[:, :, :, hf, :]))
            self.b.w = (self.b.sem, 16 * len(self.todo))

        def issue(self, n):
            for _ in range(min(n, len(self.todo))):
                o, i = self.todo.pop(0)
                nc.gpsimd.dma_start(out=o, in_=i).then_inc(self.b.sem, 16)

        def slab(self, s):
            return self.t[:, s, :, :]

    def diff_weights(li, slot):
        return [WSperm("w_dqk%d" % li, diff_w_in[slot]), WS("w_dv%d" % li, diff_w_in[slot], D, 2048, 512, col0=4096),
                WS("w_do%d" % li, diff_w_out[slot], D, D, 256)]

    def diff_layer(li, slot, wts):
        wqk, wv, wo = wts
        ls = li * 2
        cosd, sind = tabs["d"]
        NBK = S // 128
        lam_init = 0.8 - 0.6 * math.exp(-0.3 * li)
        qT_d = cx.dram("dq%d" % li, [8, 2, 128, S], BF16)
        kT_d = cx.dram("dk%d" % li, [8, 2, 128, S], BF16)
        V_d = cx.dram("dv%d" % li, [S, 8, 256], BF16)
        oT_d = cx.dram("do%d" % li, [D, S], BF16)
        scr_b = Buf("dscr")
        with ExitStack() as fes:
            cs = [sbt(fes, "cs", [128, T], F32) for i in range(2)]
            cs_b = Buf("cs")
            wsl = [sbt(fes, "wsl", [128, DC, 512], BF16) for i in range(2)]
            wsl_b = [Buf("wsl") for i in range(2)]
            rt = [sbt(fes, "rt", [128, T], F32) for i in range(4)]
            rt_b = [Buf("rt") for i in range(4)]
            ro = [sbt(fes, "ro", [128, T], BF16) for i in range(4)]
            ro_b = [Buf("ro") for i in range(4)]
            vst = [sbt(fes, "vst", [128, 512], BF16) for i in range(2)]
            vst_b = [Buf("vst") for i in range(2)]
            sl = 0
            pc = 0
            rc = 0
            for t in range(NT):
                norm_tile(t, ls)
                cx.dma("sp", cs[0][:, :], cosd[:, t * T:(t + 1) * T], writes=(cs_b,))
                cx.dma("sp", cs[1][:, :], sind[:, t * T:(t + 1) * T], writes=(cs_b,))
                for h in range(8):
                    for which in range(2):
                        sl += 1
                        si = sl % 2
                        w = wsl[si]
                        cx.dma("sp", w[:, :, 0:256], wqk.slab(which * 8 + h), reads=(wqk.b,), writes=(wsl_b[si],))
                        i2 = pc % 2
                        pc += 1
                        p1, p2 = 1 + i2, 3 + i2
                        for k in range(DC):
                            mm(psb[p1][:, :], w[:, k, 0:128], hbuf[:, k, :], k == 0, k == DC - 1, reads=(wsl_b[si], h_b), writes=(psB[p1],))
                        for k in range(DC):
                            mm(psb[p2][:, :], w[:, k, 128:256], hbuf[:, k, :], k == 0, k == DC - 1, reads=(wsl_b[si], h_b), writes=(psB[p2],))
                        dstd = qT_d if which == 0 else kT_d
                        for (pa, pb_, half, op_) in ((p1, p2, 0, ALU.subtract), (p2, p1, 1, ALU.add)):
                            a_, b_ = (0, 1) if half == 0 else (2, 3)
                            oi = rc % 4
                            rc += 1
                            dve(lambda pa=pa, a_=a_: nc.vector.tensor_tensor(out=rt[a_][:, :], in0=psb[pa][:, :], in1=cs[0][:, :], op=ALU.mult),
                                reads=(psB[pa], cs_b), writes=(rt_b[a_],))
                            dve(lambda pb_=pb_, b_=b_: nc.vector.tensor_tensor(out=rt[b_][:, :], in0=psb[pb_][:, :], in1=cs[1][:, :], op=ALU.mult),
                                reads=(psB[pb_], cs_b), writes=(rt_b[b_],))
                            pool(lambda oi=oi, a_=a_, b_=b_, op_=op_: nc.gpsimd.tensor_tensor(out=ro[oi][:, :], in0=rt[a_][:, :],
                                                                                             in1=rt[b_][:, :], op=op_),
                                 reads=(rt_b[a_], rt_b[b_]), writes=(ro_b[oi],))
                            cx.dma("pool", dstd[h, half, :, t * T:(t + 1) * T], ro[oi][:, :], reads=(ro_b[oi],), writes=(),
                                   sembuf=ro_b[oi])
                for s4 in range(4):
                    sl += 1
                    si = sl % 2
                    w = wsl[si]
                    cx.dma("sp", w[:, :, :], wv.slab(s4), reads=(wv.b,), writes=(wsl_b[si],))
                    for tb in range(4):
                        i2 = pc % 2
                        pc += 1
                        vp = 5 + i2
                        for k in range(DC):
                            mm(psb[vp][:, :], hbuf[:, k, tb * 128:(tb + 1) * 128], w[:, k, :], k == 0, k == DC - 1,
                               reads=(wsl_b[si], h_b), writes=(psB[vp],))
                        act(lambda i2=i2, vp=vp: nc.scalar.copy(out=vst[i2][:, :], in_=psb[vp][:, :]), reads=(psB[vp],), writes=(vst_b[i2],))
                        r0 = t * T + tb * 128
                        cx.dma("pool", V_d[r0:r0 + 128, 2 * s4:2 * s4 + 2, :], vst[i2][:, :].rearrange("p (h v) -> p h v", h=2),
                               reads=(vst_b[i2],), writes=(), sembuf=vst_b[i2])
            allb = wsl_b + rt_b + ro_b + vst_b + [cs_b]
            for e in ("pe", "act", "dve", "sp", "pool"):
                cx.drain(e, allb)
        with ExitStack() as fes:
            kT = sbt(fes, "kT", [128, 2, S], BF16)
            qT = sbt(fes, "qT", [128, 2, S], BF16)
            Vh = sbt(fes, "Vh", [128, NBK, 257], BF16)
            kqv_b = Buf("kqv")
            oTh = sbt(fes, "oTh", [128, 2, S], BF16)
            oTh_b = Buf("oTh")
            pT = [sbt(fes, "pT", [128, 512], BF16) for i in range(2)]
            pT_b = [Buf("pT") for i in range(2)]
            oc = [sbt(fes, "oc", [128, 4, 257], F32) for i in range(2)]
            oc_b = [[Buf("oc") for qb in range(4)] for i in range(2)]
            of = [sbt(fes, "of", [128, 256], F32) for i in range(2)]
            of_b = [Buf("of") for i in range(2)]
            ob = [sbt(fes, "ob", [128, 256], BF16) for i in range(2)]
            ob_b = [Buf("ob") for i in range(2)]
            junk = sbt(fes, "junk", [128, 256], F32)
            sm = [sbt(fes, "sm", [128, 8], F32) for i in range(2)]
            sm_b = [Buf("sm") for i in range(2)]
            tri = sbt(fes, "tri", [128, 128], BF16)
            trif = sbt(fes, "trif", [128, 128], F32)
            lamb = sbt(fes, "lamb", [128, 512], F32)
            lw = sbt(fes, "lw", [128, 256], F32)
            lam = sbt(fes, "lam", [128, 4], F32)
            sgl = sbt(fes, "sgl", [128, 256], F32)
            dc_b = Buf("dconst")
            cx.dma("sp", trif[:, :], tric_in[:, :], writes=(dc_b,))
            dve(lambda: nc.vector.tensor_copy(out=tri[:, :], in_=trif[:, :]), reads=(dc_b,), writes=(dc_b,))
            cx.dma("sp", lamb[:, :], diff_lambda[0:1, :].partition_broadcast(128), writes=(dc_b,))
            cx.dma("sp", sgl[:, :], diff_subln_g[0:1, :].partition_broadcast(128), writes=(dc_b,))
            dve(lambda: nc.vector.tensor_scalar(out=sgl[:, :], in0=sgl[:, :], scalar1=1.0 - lam_init, scalar2=None, op0=ALU.mult),
                reads=(dc_b,), writes=(dc_b,))
            for i in range(2):
                dve(lambda i=i: nc.vector.tensor_tensor(out=lw[:, i * 128:(i + 1) * 128], in0=lamb[:, i * 256:i * 256 + 128],
                                                        in1=lamb[:, i * 256 + 128:i * 256 + 256], op=ALU.mult),
                    reads=(dc_b,), writes=(dc_b,))
                dve(lambda i=i: nc.vector.tensor_reduce(out=lam[:, i:i + 1], in_=lw[:, i * 128:(i + 1) * 128], axis=AX.X, op=ALU.add),
                    reads=(dc_b,), writes=(dc_b,))
            act(lambda: nc.scalar.activation(out=lam[:, 0:2], in_=lam[:, 0:2], func=AF.Exp), reads=(dc_b,), writes=(dc_b,))
            dve(lambda: nc.vector.tensor_tensor(out=lam[:, 2:3], in0=lam[:, 1:2], in1=lam[:, 0:1], op=ALU.subtract),
                reads=(dc_b,), writes=(dc_b,))
            dve(lambda: nc.vector.tensor_scalar(out=lam[:, 2:3], in0=lam[:, 2:3], scalar1=-lam_init, scalar2=None, op0=ALU.add),
                reads=(dc_b,), writes=(dc_b,))
            dve(lambda: nc.vector.memset(Vh[:, :, 256:257], 1.0), writes=(kqv_b,))
            scale = float(DIFF_DH) ** -0.5
            NQ = S // 512
            kbi = 0
            fi = 0
            for h in range(8):
                for c in range(2):
                    for hf in range(2):
                        cx.dma("sp", kT[hf * 64:(hf + 1) * 64, c, :], kT_d[h, hf, c * 64:(c + 1) * 64, :], writes=(kqv_b,))
                        cx.dma("sp", qT[hf * 64:(hf + 1) * 64, c, :], qT_d[h, hf, c * 64:(c + 1) * 64, :], writes=(kqv_b,))
                cx.dma("sp", Vh[:, :, 0:256], V_d[:, h, :].rearrange("(b p) v -> p b v", p=128), writes=(kqv_b,))
                for qt in range(NQ):
                    for c in range(2):
                        nkb = 4 * qt + 4
                        for kb in range(nkb):
                            sp_ = 5 + (kbi % 2)
                            pi = kbi % 2
                            kbi += 1
                            mm(psb[sp_][:, :], kT[:, c, kb * 128:(kb + 1) * 128], qT[:, c, qt * 512:(qt + 1) * 512], True, True,
                               reads=(kqv_b,), writes=(psB[sp_],))
                            act(lambda sp_=sp_, pi=pi: nc.scalar.activation(out=pT[pi][:, :], in_=psb[sp_][:, :], func=AF.Exp, scale=scale),
                                reads=(psB[sp_],), writes=(pT_b[pi],))
                            d = kb - 4 * qt
                            if d >= 0:
                                pool(lambda pi=pi, d=d: nc.gpsimd.tensor_tensor(out=pT[pi][:, d * 128:(d + 1) * 128],
                                                                                in0=pT[pi][:, d * 128:(d + 1) * 128], in1=tri[:, :], op=ALU.mult),
                                     reads=(pT_b[pi], dc_b), writes=(pT_b[pi],))
                            for qb in range(max(d, 0), 4):
                                mm(psb[1 + qb][:, 0:257], pT[pi][:, qb * 128:(qb + 1) * 128], Vh[:, kb, :], kb == 0, kb == 4 * qt + qb,
                                   reads=(pT_b[pi], kqv_b), writes=(psB[1 + qb],))
                        for qb in range(4):
                            act(lambda c=c, qb=qb: nc.scalar.copy(out=oc[c][:, qb, :], in_=psb[1 + qb][:, 0:257]),
                                reads=(psB[1 + qb],), writes=(oc_b[c][qb],))
                    for qb in range(4):
                        f = fi % 2
                        fi += 1
                        s_ = sm[f]
                        dve(lambda s_=s_, qb=qb: nc.vector.reciprocal(out=s_[:, 0:1], in_=oc[0][:, qb, 256:257]),
                            reads=(oc_b[0][qb],), writes=(sm_b[f],))
                        dve(lambda s_=s_, qb=qb: nc.vector.reciprocal(out=s_[:, 1:2], in_=oc[1][:, qb, 256:257]),
                            reads=(oc_b[1][qb],), writes=(sm_b[f],))
                        dve(lambda s_=s_: nc.vector.tensor_tensor(out=s_[:, 1:2], in0=s_[:, 1:2], in1=lam[:, 2:3], op=ALU.mult),
                            reads=(sm_b[f], dc_b), writes=(sm_b[f],))
                        dve(lambda s_=s_, qb=qb, f=f: nc.vector.tensor_scalar(out=of[f][:, :], in0=oc[0][:, qb, 0:256], scalar1=s_[:, 0:1],
                                                                             scalar2=None, op0=ALU.mult),
                            reads=(oc_b[0][qb], sm_b[f]), writes=(of_b[f],))
                        dve(lambda s_=s_, qb=qb, f=f: nc.vector.scalar_tensor_tensor(out=of[f][:, :], in0=oc[1][:, qb, 0:256], scalar=s_[:, 1:2],
                                                                                    in1=of[f][:, :], op0=ALU.mult, op1=ALU.add),
                            reads=(oc_b[1][qb], sm_b[f], of_b[f]), writes=(of_b[f],))
                        act(lambda s_=s_, f=f: nc.scalar.activation(out=junk[:, :], in_=of[f][:, :], func=AF.Square, accum_out=s_[:, 2:3]),
                            reads=(of_b[f],), writes=(sm_b[f],))
                        act(lambda s_=s_: nc.scalar.activation(out=s_[:, 3:4], in_=s_[:, 2:3], func=AF.Sqrt, bias=epsb[:, 0:1], scale=1.0 / 256.0),
                            reads=(sm_b[f], consts_b), writes=(sm_b[f],))
                        dve(lambda s_=s_: nc.vector.reciprocal(out=s_[:, 3:4], in_=s_[:, 3:4]), reads=(sm_b[f],), writes=(sm_b[f],))
                        dve(lambda s_=s_, f=f: nc.vector.scalar_tensor_tensor(out=ob[f][:, :], in0=of[f][:, :], scalar=s_[:, 3:4], in1=sgl[:, :],
                                                                             op0=ALU.mult, op1=ALU.mult),
                            reads=(of_b[f], sm_b[f], dc_b), writes=(ob_b[f],))
                        for j in range(2):
                            tr(pst[:, f * 512 + j * 128: f * 512 + (j + 1) * 128], ob[f][:, j * 128:(j + 1) * 128], ident_bf[:, :],
                               reads=(ob_b[f], consts_b), writes=(pst_b[f],), tick=(j == 1))
                        q0 = qt * 512 + qb * 128
                        act(lambda f=f, q0=q0: nc.scalar.copy(out=oTh[:, :, q0:q0 + 128],
                                                              in_=pst[:, f * 512: f * 512 + 256].rearrange("p (j n) -> p j n", j=2)),
                            reads=(pst_b[f],), writes=(oTh_b,))
                cx.dma("pool", oT_d[h * 256:(h + 1) * 256, :].rearrange("(j p) n -> p j n", p=128), oTh[:, :, :],
                       reads=(oTh_b,), writes=(), sembuf=oTh_b)
            allb = [kqv_b, oTh_b, dc_b] + pT_b + [b for r in oc_b for b in r] + of_b + ob_b + sm_b
            for e in ("pe", "act", "dve", "sp", "pool"):
                cx.drain(e, allb)
        with ExitStack() as fes:
            wsl = [sbt(fes, "wsl", [128, DC, 256], BF16) for i in range(2)]
            wsl_b = [Buf("wsl") for i in range(2)]
            for t in range(NT):
                cx.dma("sp", hbuf[:, :, :], oT_d[:, t * T:(t + 1) * T].rearrange("(c p) n -> p c n", p=128), writes=(h_b,))
                for s2 in range(DC // 2):
                    si = s2 % 2
                    cx.dma("sp", wsl[si][:, :, :], wo.slab(s2), reads=(wo.b,), writes=(wsl_b[si],))
                    for mi in range(2):
                        m = s2 * 2 + mi
                        yp = yb4[m % 4]
                        for k in range(DC):
                            mm(psb[yp][:, :], wsl[si][:, k, mi * 128:(mi + 1) * 128], hbuf[:, k, :], k == 0, k == DC - 1,
                               reads=(wsl_b[si], h_b), writes=(psB[yp],))
                        act(lambda m=m, yp=yp: nc.scalar.copy(out=xy[:, m, :], in_=psb[yp][:, :]), reads=(psB[yp],), writes=(xy_b,))
                resid_tile(t, ls)
            for e in ("pe", "act", "dve", "sp", "pool"):
                cx.drain(e, wsl_b)


    def ffn_layer(li, win, wdn):
        ls = li * 2 + 1
        with ExitStack() as fes:
            NB = 2
            wg = [fes.enter_context(nc.sbuf_tensor("wg%d_%d" % (li, i), [128, DC, 256], BF16)) for i in range(NB)]
            wu = [fes.enter_context(nc.sbuf_tensor("wu%d_%d" % (li, i), [128, DC, 256], BF16)) for i in range(NB)]
            wg_b = [Buf("wg%d" % i) for i in range(NB)]
            wu_b = [Buf("wu%d" % i) for i in range(NB)]
            wd = [fes.enter_context(nc.sbuf_tensor("wd%d_%d" % (li, i), [128, FC // 2, 256], BF16)) for i in range(3)]
            wd_b = [Buf("wd%d" % i) for i in range(3)]
            abuf = fes.enter_context(nc.sbuf_tensor("abuf%d" % li, [128, FC, T], BF16))
            a_b = [Buf("a%d" % j) for j in range(FC)]
            gb = [fes.enter_context(nc.sbuf_tensor("gb%d_%d" % (li, i), [128, T + 2], F32)) for i in range(2)]
            gb_b = [Buf("gb%d" % i) for i in range(2)]
            sg = [fes.enter_context(nc.sbuf_tensor("sg%d_%d" % (li, i), [128, T], F32)) for i in range(2)]
            sg_b = [Buf("sg%d" % i) for i in range(2)]
            halo = fes.enter_context(nc.sbuf_tensor("halo%d" % li, [128, FC, 2], F32))
            halo_b = Buf("halo")
            dve(lambda: nc.vector.memset(halo[:, :, :], 0.0), writes=(halo_b,))
            yb = [5, 6, 1, 2]

            def tap(tp, j):
                i = (li * 3 + tp) * FC + j
                return fdwT[:, i:i + 1]

            for t in range(NT):
                norm_tile(t, ls)
                for s2 in range(FC // 2):
                    i3 = s2 % NB
                    cx.dma("sp", wg[i3][:, :, :], win.slab(s2), reads=(win.b,), writes=(wg_b[i3],))
                    cx.dma("sp", wu[i3][:, :, :], win.slab(FC // 2 + s2), reads=(win.b,), writes=(wu_b[i3],))
                    for jj in range(2):
                        j = s2 * 2 + jj
                        i2 = j % 2
                        gp, up = 1 + i2, 3 + i2
                        for k in range(DC):
                            mm(psb[gp][:, :], wg[i3][:, k, jj * 128:(jj + 1) * 128], hbuf[:, k, :], k == 0, k == DC - 1,
                               reads=(wg_b[i3], h_b), writes=(psB[gp],))
                        for k in range(DC):
                            mm(psb[up][:, :], wu[i3][:, k, jj * 128:(jj + 1) * 128], hbuf[:, k, :], k == 0, k == DC - 1,
                               reads=(wu_b[i3], h_b), writes=(psB[up],))
                        g = gb[i2]
                        act(lambda g=g, gp=gp: nc.scalar.copy(out=g[:, 2:T + 2], in_=psb[gp][:, :]),
                            reads=(psB[gp],), writes=(gb_b[i2],))
                        act(lambda g=g, j=j: nc.scalar.copy(out=g[:, 0:2], in_=halo[:, j, :]),
                            reads=(halo_b,), writes=(gb_b[i2],))
                        act(lambda gp=gp, i2=i2, j=j: nc.scalar.activation(out=tmpf[i2][:, :], in_=psb[gp][:, :],
                                                                           func=AF.Identity, scale=tap(2, j)),
                            reads=(psB[gp], consts_b), writes=(tmpf_b[i2],))
                        dve(lambda g=g, i2=i2, j=j: nc.vector.scalar_tensor_tensor(
                            out=tmpf[i2][:, :], in0=g[:, 1:T + 1], scalar=tap(1, j), in1=tmpf[i2][:, :],
                            op0=ALU.mult, op1=ALU.add), reads=(gb_b[i2], tmpf_b[i2], consts_b), writes=(tmpf_b[i2],))
                        dve(lambda g=g, i2=i2, j=j: nc.vector.scalar_tensor_tensor(
                            out=tmpf[i2][:, :], in0=g[:, 0:T], scalar=tap(0, j), in1=tmpf[i2][:, :],
                            op0=ALU.mult, op1=ALU.add), reads=(gb_b[i2], tmpf_b[i2], consts_b), writes=(tmpf_b[i2],))
                        dve(lambda g=g, j=j: nc.vector.tensor_copy(out=halo[:, j, :], in_=g[:, T:T + 2]),
                            reads=(gb_b[i2],), writes=(halo_b,))
                        act(lambda i2=i2: nc.scalar.activation(out=sg[i2][:, :], in_=tmpf[i2][:, :], func=AF.Silu),
                            reads=(tmpf_b[i2],), writes=(sg_b[i2],))
                        dve(lambda i2=i2, up=up, j=j: nc.vector.tensor_tensor(out=abuf[:, j, :], in0=sg[i2][:, :],
                                                                             in1=psb[up][:, :], op=ALU.mult),
                            reads=(sg_b[i2], psB[up]), writes=(a_b[j],))
                pc = 0
                for s2 in range(DC // 2):
                    for half in range(2):
                        w3 = pc % 3
                        pc += 1
                        cx.dma("sp", wd[w3][:, :, :], wdn.t[:, s2, half * 22:(half + 1) * 22, :], reads=(wdn.b,),
                               writes=(wd_b[w3],))
                        for mi in range(2):
                            m = s2 * 2 + mi
                            yp = yb[m % 4]
                            for jj in range(22):
                                j = half * 22 + jj
                                mm(psb[yp][:, :], wd[w3][:, jj, mi * 128:(mi + 1) * 128], abuf[:, j, :], j == 0, j == FC - 1,
                                   reads=(wd_b[w3], a_b[j]), writes=(psB[yp],), tick=(jj == 21))
                            if half == 1:
                                act(lambda m=m, yp=yp: nc.scalar.copy(out=xy[:, m, :], in_=psb[yp][:, :]),
                                    reads=(psB[yp],), writes=(xy_b,))
                resid_tile(t, ls)
            allb = wg_b + wu_b + wd_b + a_b + gb_b + sg_b + [halo_b]
            for e in ("pe", "act", "dve", "sp", "pool"):
                cx.drain(e, allb)

    subs = []
    for (kind, slot, li) in layer_kinds:
        if with_mixer and kind == 0:
            w = [WS("w_rqk%d" % li, ret_w_in[slot], D, 4096, 256), WS("w_rvg%d" % li, ret_w_in[slot], D, 8192, 512, col0=4096),
                 WS("w_ro%d" % li, ret_w_out[slot], 4096, D, 256)]
            subs.append((lambda li=li, slot=slot, w=w: ret_layer(li, slot, w[0], w[1], w[2]), w))
        if with_mixer and kind == 1:
            w = [WS("w_c1_%d" % li, conv_w_pw1[slot], D, 2 * D, 256), WS("w_c2_%d" % li, conv_w_pw2[slot], D, D, 256)]
            subs.append((lambda li=li, slot=slot, w=w: conv_layer(li, slot, w[0], w[1]), w))
        if with_mixer and kind == 2:
            w = diff_weights(li, slot)
            subs.append((lambda li=li, slot=slot, w=w: diff_layer(li, slot, w), w))
        if with_ffn:
            w = [WS("w_fin%d" % li, ffn_w_in[li], D, 2 * DFF, 256), WS("w_fdn%d" % li, ffn_w_down[li], DFF, D, 256)]
            subs.append((lambda li=li, w=w: ffn_layer(li, w[0], w[1]), w))
    for w in subs[0][1]:
        w.issue(10 ** 9)
    for i, (run, _) in enumerate(subs):
        nxt = subs[i + 1][1] if i + 1 < len(subs) else []
        tot = sum(len(w.todo) for w in nxt)
        per = -(-tot // NT) if tot else 0

        def hook(t, nxt=nxt, per=per):
            n = per
            for w in nxt:
                k = min(n, len(w.todo))
                w.issue(k)
                n -= k
        tile_hook[0] = hook
        run()
        for w in nxt:
            w.issue(10 ** 9)
    tile_hook[0] = None

    with ExitStack() as tes:
        ob4 = tes.enter_context(nc.sbuf_tensor("ob4", [128, 4, D], F32))
        ob_b = Buf("ob4")
        for t in range(NT):
            load_x_tile(t, xy, xy_b)
            for b in range(4):
                for q in range(4):
                    pb = 1 + (q % 2)
                    for cc in range(4):
                        c = q * 4 + cc
                        tr(psb[pb][:, cc * 128:(cc + 1) * 128], xy[:, c, b * 128:(b + 1) * 128], ident[:, :],
                           reads=(xy_b, consts_b), writes=(psB[pb],), tick=(cc == 3))
                    act(lambda b=b, q=q, pb=pb: nc.scalar.copy(out=ob4[:, b, q * 512:(q + 1) * 512], in_=psb[pb][:, :]),
                        reads=(psB[pb],), writes=(ob_b,))
            cx.dma("pool", out_d[t * T:(t + 1) * T, :].rearrange("(b p) f -> p b f", p=128), ob4[:, :, :],
                   reads=(ob_b,), writes=(), sembuf=ob_b)
        cx.drain("pool", [ob_b])
        cx.drain("sp", [ob_b])
        cx.drain("act", [ob_b])
    es.close()
    return nc


LAYERS = [(i % 3, i // 3, i) for i in range(DEPTH)]


def _ropec():
    o = np.zeros((128, 4), np.float32)
    p = np.arange(128)
    o[:, 0] = (np.float32(ROPE_THETA) ** (-(2 * p).astype(np.float32) / np.float32(256))).astype(np.float32)
    o[:, 1] = (np.float32(ROPE_THETA) ** (-(2 * (p % 64)).astype(np.float32) / np.float32(128))).astype(np.float32)
    return o


def _retc():
    o = np.zeros((128, 8 * 128 * 2 + 8), np.float32)
    idx = np.arange(128, dtype=np.float64)
    for h in range(8):
        lg = np.log1p(-np.exp2(-5.0 - h))
        rel = idx[None, :] - idx[:, None]
        o[:, h * 128:(h + 1) * 128] = np.where(rel >= 0, np.exp(np.maximum(rel, 0) * lg), 0.0)
        o[:, 1024 + h * 128:1024 + (h + 1) * 128] = np.exp((idx + 1.0) * lg)[None, :]
        o[:, 2048 + h] = np.exp((127.0 - idx) * lg)
    return o


def make_in_map(b, S, x, c, positions, mod_w, mod_b, norm_g, ret_w_in, ret_w_out, conv_w_pw1, conv_b_pw1,
                conv_w_dw, conv_b_dw, conv_ln_g, conv_ln_b, conv_w_pw2, conv_b_pw2, diff_w_in, diff_lambda,
                diff_subln_g, diff_w_out, ffn_w_in, ffn_w_dw, ffn_w_down):
    f = lambda a: np.ascontiguousarray(np.asarray(a, dtype=np.float32))
    return {
        "x": f(x[b, :S]),
        "c": f(c[b]).reshape(DC, 128),
        "positions": np.ascontiguousarray(np.asarray(positions[b, :S], dtype=np.int32)).reshape(1, S),
        "mod_w": f(mod_w), "mod_b": f(mod_b).reshape(DEPTH * 2, 3 * D),
        "norm_g": f(norm_g).reshape(DEPTH * 4 * DC, 128),
        "ret_w_in": f(ret_w_in), "ret_w_out": f(ret_w_out),
        "conv_w_pw1": f(conv_w_pw1), "conv_b_pw1": f(conv_b_pw1).reshape(32, 128),
        "conv_w_dw": f(conv_w_dw).reshape(CONV_W * DC, 128),
        "conv_vecs": np.concatenate([f(conv_b_dw).reshape(DC, 128), f(conv_ln_g).reshape(DC, 128),
                                     f(conv_ln_b).reshape(DC, 128), f(conv_b_pw2).reshape(DC, 128)], axis=0),
        "conv_w_pw2": f(conv_w_pw2),
        "diff_w_in": f(diff_w_in), "diff_lambda": f(diff_lambda).reshape(1, 512),
        "diff_subln_g": f(diff_subln_g).reshape(1, 256), "diff_w_out": f(diff_w_out),
        "ffn_w_in": f(ffn_w_in), "ffn_w_dw": f(ffn_w_dw).reshape(DEPTH * 3 * FC, 128),
        "ffn_w_down": f(ffn_w_down),
        "identc": np.eye(128, dtype=np.float32),
        "ropec": _ropec(), "retc": _retc(),
        "tric": np.triu(np.ones((128, 128), np.float32)),
    }


def kernel(**inputs):
    S = 4096
    B = 8
    nc = build(S, LAYERS)
    in_maps = [make_in_map(b, S, **inputs) for b in range(B)]
    res = run_bass_kernel_spmd(nc, in_maps, core_ids=list(range(B)))
    return np.stack([np.asarray(r["out"], dtype=np.float32) for r in res.results], axis=0)
```

```python
import math
from contextlib import ExitStack

import numpy as np
import concourse.bass as bass
import concourse.mybir as mybir
from concourse.bass_utils import run_bass_kernel_spmd

F32 = mybir.dt.float32
BF16 = mybir.dt.bfloat16
I32 = mybir.dt.int32
AF = mybir.ActivationFunctionType
ALU = mybir.AluOpType
AX = mybir.AxisListType

D = 2048
DC = 16
T = 512
DFF = 5632
FC = 44
EPS = 1e-6
DEPTH = 4
RET_H, RET_DK, RET_DV = 8, 256, 512
DIFF_H, DIFF_DH = 8, 128
CONV_W = 31
ROPE_THETA = 10000.0


class Buf:
    __slots__ = ("name", "w", "r", "sem", "semv")

    def __init__(self, name):
        self.name = name
        self.w = None
        self.r = []
        self.sem = None
        self.semv = 0


class Ctx:
    def __init__(self, nc):
        self.nc = nc
        self.es = ExitStack()
        self.eng = {"pe": nc.tensor, "act": nc.scalar, "dve": nc.vector, "pool": nc.gpsimd, "sp": nc.sync}
        self.cnt = {}
        for n in ("pe", "act", "dve", "pool"):
            self.cnt[n] = [self.es.enter_context(nc.semaphore("c_" + n)), 0]
        self.waited = {}
        self.nsem = 4

    def sb(self, name, shape, dt):
        return self.es.enter_context(self.nc.sbuf_tensor(name, shape, dt))

    def psum(self, name, shape, dt):
        return self.es.enter_context(self.nc.psum_tensor(name, shape, dt))

    def dram(self, name, shape, dt):
        return self.nc.dram_tensor(name, shape, dt, kind="Internal").ap()

    def wait(self, cons, t):
        if t is None:
            return
        sem, val = t
        key = (cons, id(sem))
        if self.waited.get(key, 0) >= val:
            return
        if cons == "pe" and sem is self.cnt["pe"][0]:
            return
        self.eng[cons].wait_ge(sem, val)
        self.waited[key] = val

    def _deps(self, cons, reads, writes):
        for b in reads:
            self.wait(cons, b.w)
        for b in writes:
            self.wait(cons, b.w)
            for t in b.r:
                self.wait(cons, t)

    @staticmethod
    def _addr(b, t):
        for i, (s, v) in enumerate(b.r):
            if s is t[0]:
                if v < t[1]:
                    b.r[i] = t
                return
        b.r.append(t)

    def _upd(self, t, reads, writes):
        for b in reads:
            self._addr(b, t)
        for b in writes:
            b.w = t
            b.r = []

    def op(self, e, fn, reads=(), writes=(), tick=True):
        self._deps(e, reads, writes)
        inst = fn()
        c = self.cnt[e]
        if tick:
            c[1] += 1
            inst.then_inc(c[0], 1)
            t = (c[0], c[1])
        else:
            t = (c[0], c[1] + 1)
        self._upd(t, reads, writes)
        return t

    def dma(self, q, out, in_, reads=(), writes=(), sembuf=None, **kw):
        self._deps(q, reads, writes)
        b = sembuf if sembuf is not None else (writes[0] if writes else reads[0])
        if b.sem is None:
            b.sem = self.es.enter_context(self.nc.semaphore("d%d" % self.nsem))
            self.nsem += 1
        b.semv += 16
        self.eng[q].dma_start(out=out, in_=in_, **kw).then_inc(b.sem, 16)
        t = (b.sem, b.semv)
        self._upd(t, reads, writes)
        return t

    def drain(self, e, bufs):
        for b in bufs:
            self.wait(e, b.w)
            for t in b.r:
                self.wait(e, t)


def build(S, layer_kinds, with_mixer=True, with_ffn=True):
    NT = S // T
    nc = bass.Bass("TRN2", target_bir_lowering=False)
    cx = Ctx(nc)
    es = cx.es

    def din(name, shape, dt=F32):
        return nc.dram_tensor(name, shape, dt, kind="ExternalInput").ap()

    x_in = din("x", [S, D])
    c_in = din("c", [DC, 128])
    pos_in = din("positions", [1, S], I32)
    mod_w = din("mod_w", [DEPTH, 2, D, 3 * D])
    mod_b = din("mod_b", [DEPTH * 2, 3 * D])
    norm_g = din("norm_g", [DEPTH * 4 * DC, 128])
    ret_w_in = din("ret_w_in", [2, D, 12288])
    ret_w_out = din("ret_w_out", [2, 4096, D])
    conv_w_pw1 = din("conv_w_pw1", [1, D, 2 * D])
    conv_b_pw1 = din("conv_b_pw1", [32, 128])
    conv_w_dw = din("conv_w_dw", [CONV_W * DC, 128])
    conv_vecs = din("conv_vecs", [4 * DC, 128])
    conv_w_pw2 = din("conv_w_pw2", [1, D, D])
    diff_w_in = din("diff_w_in", [1, D, 3 * D])
    diff_lambda = din("diff_lambda", [1, 4 * 128])
    diff_subln_g = din("diff_subln_g", [1, 256])
    diff_w_out = din("diff_w_out", [1, D, D])
    ffn_w_in = din("ffn_w_in", [DEPTH, D, 2 * DFF])
    ffn_w_dw = din("ffn_w_dw", [DEPTH * 3 * FC, 128])
    ffn_w_down = din("ffn_w_down", [DEPTH, DFF, D])
    ident_in = din("identc", [128, 128])
    ropec_in = din("ropec", [128, 4])
    retc_in = din("retc", [128, 8 * 128 * 2 + 8])
    out_d = nc.dram_tensor("out", [S, D], F32, kind="ExternalOutput").ap()
    kinds = [k for (k, _, _) in layer_kinds] if with_mixer else []
    tabs = {}
    tabs_b = Buf("tabs")
    if 0 in kinds:
        tabs["r"] = (cx.dram("cosR", [128, S], F32), cx.dram("sinR", [128, S], F32))
    if 2 in kinds:
        tabs["d"] = (cx.dram("cosD", [128, S], F32), cx.dram("sinD", [128, S], F32))

    xT = cx.dram("xT", [D, S], F32)
    xT_b = Buf("xT")
    modrow_d = cx.dram("modrow", [DEPTH * 2, 3 * D], F32)
    modrow_b = Buf("modrow")

    ident = cx.sb("ident", [128, 128], F32)
    ident_bf = cx.sb("ident_bf", [128, 128], BF16)
    ones_bf = cx.sb("ones_bf", [128, 128], BF16)
    cT = cx.sb("cT", [128, DC], F32)
    gT = cx.sb("gT", [128, DEPTH * 4 * DC], F32)
    modT = cx.sb("modT", [128, DEPTH * 2 * 48], F32)
    AT = cx.sb("AT", [128, DEPTH * 2 * DC], F32)
    GT = cx.sb("GT", [128, DEPTH * 2 * DC], F32)
    fdwT = cx.sb("fdwT", [128, DEPTH * 3 * FC], F32)
    epsb = cx.sb("epsb", [128, 1], F32)
    consts_b = Buf("consts")

    psb = [cx.psum("ps%d" % i, [128, 512], F32) for i in range(7)]
    psB = [Buf("ps%d" % i) for i in range(7)]
    pst = cx.psum("pst", [128, 1024], BF16)
    pst_b = [Buf("pst0"), Buf("pst1")]

    xy = cx.sb("xy", [128, DC, T], F32)
    xy_b = Buf("xy")
    hbuf0 = cx.sb("hbuf0", [128, DC, T], BF16)
    hbuf0_b = Buf("h0")
    nxc = [cx.sb("nxc%d" % i, [128, T], F32) for i in range(4)]
    nxc_b = [Buf("nxc%d" % i) for i in range(4)]
    rsq = [cx.sb("rsq%d" % i, [128, T], BF16) for i in range(2)]
    rsq_b = [Buf("rsq%d" % i) for i in range(2)]
    rtmp = [cx.sb("rtmp%d" % i, [128, T], F32) for i in range(4)]
    rtmp_b = [Buf("rtmp%d" % i) for i in range(4)]
    xT_w = []
    rstd2 = cx.sb("rstd2", [128, T], F32)
    rstd2_b = Buf("rstd2")
    ntmp = [cx.sb("ntmp%d" % i, [128, T], F32) for i in range(2)]
    ntmp_b = [Buf("ntmp%d" % i) for i in range(2)]
    sq = [cx.sb("sq%d" % i, [128, T], BF16) for i in range(2)]
    sq_b = [Buf("sq%d" % i) for i in range(2)]
    tmpf = [cx.sb("tmpf%d" % i, [128, T], F32) for i in range(2)]
    tmpf_b = [Buf("tmpf%d" % i) for i in range(2)]
    rstd = cx.sb("rstd", [128, T], F32)
    rstd_b = Buf("rstd")

    def act(fn, reads=(), writes=()):
        return cx.op("act", fn, reads, writes)

    def dve(fn, reads=(), writes=()):
        return cx.op("dve", fn, reads, writes)

    def pool(fn, reads=(), writes=()):
        return cx.op("pool", fn, reads, writes)

    def mm(out_ap, lhsT, rhs, start, stop, reads, writes, tick=None):
        return cx.op("pe", lambda: nc.tensor.matmul(out_ap, lhsT, rhs, start=start, stop=stop),
                     reads, writes, tick=(stop if tick is None else tick))

    def tr(out_ap, in_ap, idn, reads, writes, tick=True):
        return cx.op("pe", lambda: nc.tensor.transpose(out_ap, in_ap, idn), reads, writes, tick=tick)

    class WS:
        def __init__(self, name, src, K, N, SW, col0=0):
            self.KC = K // 128
            self.NS = N // SW
            self.SW = SW
            self.t = cx.dram(name, [128, self.NS, self.KC, SW], BF16)
            self.b = Buf(name)
            self.b.sem = es.enter_context(nc.semaphore("w_" + name))
            self.todo = []
            for k in range(self.KC):
                self.todo.append((self.t[:, :, k, :],
                                  src[k * 128:(k + 1) * 128, col0:col0 + N].rearrange("p (s w) -> p s w", w=SW)))
            self.b.w = (self.b.sem, 16 * len(self.todo))

        def issue(self, n):
            for _ in range(min(n, len(self.todo))):
                o, i = self.todo.pop(0)
                nc.gpsimd.dma_start(out=o, in_=i).then_inc(self.b.sem, 16)

        def slab(self, s):
            return self.t[:, s, :, :]

    tile_hook = [None]

    stg = cx.sb("stg", [128, 128], F32)
    stg_b = Buf("stg")

    def load_T(dst_ap, src_rows, R, src_b=None):
        cx.dma("sp", stg[0:R, :], src_rows, reads=(() if src_b is None else (src_b,)), writes=(stg_b,), sembuf=stg_b)
        tr(psb[0][:, 0:R], stg[0:R, :], ident[0:R, 0:R], reads=(stg_b, consts_b), writes=(psB[0],))
        dve(lambda: nc.vector.tensor_copy(out=dst_ap, in_=psb[0][:, 0:R]), reads=(psB[0],), writes=(consts_b,))

    cx.dma("sp", ident[:, :], ident_in[:, :], writes=(consts_b,))
    dve(lambda: nc.vector.tensor_copy(out=ident_bf[:, :], in_=ident[:, :]), reads=(consts_b,), writes=(consts_b,))
    dve(lambda: nc.vector.memset(ones_bf[:, :], 1.0), writes=(consts_b,))
    dve(lambda: nc.vector.memset(epsb[:, :], EPS), writes=(consts_b,))
    load_T(cT[:, :], c_in[:, :], DC)
    act(lambda: nc.scalar.activation(out=cT[:, :], in_=cT[:, :], func=AF.Silu), reads=(consts_b,), writes=(consts_b,))
    for i in range(0, DEPTH * 4 * DC, 128):
        load_T(gT[:, i:i + 128], norm_g[i:i + 128, :], 128)
    nf = DEPTH * 3 * FC
    for i in range(0, nf, 128):
        r = min(128, nf - i)
        load_T(fdwT[:, i:i + r], ffn_w_dw[i:i + r, :], r)

    with ExitStack() as mes:
        mwa = [mes.enter_context(nc.sbuf_tensor("mw%d" % i, [128, DC, 256], F32)) for i in range(2)]
        mw_b = [Buf("mw%d" % i) for i in range(2)]
        mrow = mes.enter_context(nc.sbuf_tensor("mrow", [1, 3 * D], F32))
        mrow_b = Buf("mrow")
        mbias = mes.enter_context(nc.sbuf_tensor("mbias", [1, 3 * D], F32))
        mbias_b = Buf("mbias")
        it = 0
        for (kind, slot, li) in layer_kinds:
            for sub in range(2):
                if (sub == 0 and not with_mixer) or (sub == 1 and not with_ffn):
                    continue
                ls = li * 2 + sub
                cx.dma("sp", mbias[:, :], mod_b[ls:ls + 1, :], writes=(mbias_b,))
                for n in range(24):
                    w = mwa[it % 2]
                    wb = mw_b[it % 2]
                    cx.dma("sp", w[:, :, :],
                           mod_w[li, sub, :, n * 256:(n + 1) * 256].rearrange("(k p) n -> p k n", p=128),
                           writes=(wb,))
                    pb = 1 + (it % 2)
                    for k in range(DC):
                        mm(psb[pb][0:1, 0:256], cT[:, k:k + 1], w[:, k, :], k == 0, k == DC - 1,
                           reads=(wb, consts_b), writes=(psB[pb],))
                    dve(lambda pb=pb, n=n: nc.vector.tensor_tensor(out=mrow[:, n * 256:(n + 1) * 256], in0=psb[pb][0:1, 0:256],
                                                                   in1=mbias[:, n * 256:(n + 1) * 256], op=ALU.add),
                        reads=(psB[pb], mbias_b), writes=(mrow_b,))
                    it += 1
                cx.dma("pool", modrow_d[ls:ls + 1, :], mrow[:, :], reads=(mrow_b,), writes=(modrow_b,), sembuf=mrow_b)
                load_T(modT[:, ls * 48:(ls + 1) * 48], modrow_d[ls, :].rearrange("(r p) -> r p", p=128), 48, modrow_b)
                sc = modT[:, ls * 48 + 16: ls * 48 + 32]
                gt = modT[:, ls * 48 + 32: ls * 48 + 48]
                gpre = gT[:, (li * 4 + 2 * sub) * DC:(li * 4 + 2 * sub + 1) * DC]
                gpost = gT[:, (li * 4 + 2 * sub + 1) * DC:(li * 4 + 2 * sub + 2) * DC]
                dve(lambda sc=sc, gpre=gpre, ls=ls: nc.vector.scalar_tensor_tensor(
                    out=AT[:, ls * DC:(ls + 1) * DC], in0=sc, scalar=1.0, in1=gpre, op0=ALU.add, op1=ALU.mult),
                    reads=(consts_b,), writes=(consts_b,))
                dve(lambda gt=gt, gpost=gpost, ls=ls: nc.vector.tensor_tensor(
                    out=GT[:, ls * DC:(ls + 1) * DC], in0=gt, in1=gpost, op=ALU.mult),
                    reads=(consts_b,), writes=(consts_b,))
        cx.drain("pe", mw_b)
        cx.drain("dve", [mrow_b, mbias_b] + mw_b)
        cx.drain("pool", [mrow_b])
        cx.drain("sp", [mbias_b] + mw_b)

    def wait_xT(q):
        last = {}
        for (sem, v) in xT_w:
            if id(sem) not in last or last[id(sem)][1] < v:
                last[id(sem)] = (sem, v)
        xT_w[:] = list(last.values())
        for t_ in xT_w:
            cx.wait(q, t_)

    def load_x_tile(t, dst, dst_b):
        wait_xT("sp")
        cx.dma("sp", dst[:, :, :], xT[:, t * T:(t + 1) * T].rearrange("(c p) n -> p c n", p=128),
               reads=(xT_b,), writes=(dst_b,))

    NL = 4

    def norm_tile(t, ls, dst=None):
        if tile_hook[0] is not None:
            tile_hook[0](t)
        hbuf, h_b = dst if dst is not None else (hbuf0, hbuf0_b)
        wait_xT("act")

        def issue(c):
            i = c % NL
            cx.dma("act", nxc[i][:, :], xT[c * 128:(c + 1) * 128, t * T:(t + 1) * T], reads=(xT_b,), writes=(nxc_b[i],))

        for c in range(NL):
            issue(c)
        for c in range(DC):
            i = c % 2
            n_ = c % NL
            act(lambda c=c, i=i, n_=n_: nc.scalar.activation(out=sq[i][:, :], in_=nxc[n_][:, :], func=AF.Square),
                reads=(nxc_b[n_],), writes=(sq_b[i],))
            mm(psb[0][:, :], ones_bf[:, :], sq[i][:, :], c == 0, c == DC - 1, reads=(sq_b[i], consts_b), writes=(psB[0],),
               tick=True)
            if c + NL < DC:
                issue(c + NL)
        for c in range(NL):
            issue(c)
        act(lambda: nc.scalar.activation(out=rstd[:, :], in_=psb[0][:, :], func=AF.Sqrt, bias=epsb[:, 0:1], scale=1.0 / D),
            reads=(psB[0], consts_b), writes=(rstd_b,))
        dve(lambda: nc.vector.reciprocal(out=rstd[:, :], in_=rstd[:, :]), reads=(rstd_b,), writes=(rstd_b,))
        for c in range(DC):
            i = c % 2
            n_ = c % NL
            dve(lambda c=c, i=i, n_=n_: nc.vector.scalar_tensor_tensor(
                out=ntmp[i][:, :], in0=nxc[n_][:, :], scalar=AT[:, ls * DC + c: ls * DC + c + 1], in1=rstd[:, :],
                op0=ALU.mult, op1=ALU.mult), reads=(nxc_b[n_], rstd_b, consts_b), writes=(ntmp_b[i],))
            act(lambda c=c, i=i, hbuf=hbuf: nc.scalar.activation(
                out=hbuf[:, c, :], in_=ntmp[i][:, :], func=AF.Identity,
                bias=modT[:, ls * 48 + c: ls * 48 + c + 1], scale=1.0), reads=(ntmp_b[i], consts_b), writes=(h_b,))
            if c + NL < DC:
                issue(c + NL)

    def resid_tile(t, ls):
        for c in range(DC):
            i = c % 2
            act(lambda c=c, i=i: nc.scalar.activation(out=rsq[i][:, :], in_=xy[:, c, :], func=AF.Square),
                reads=(xy_b,), writes=(rsq_b[i],))
            mm(psb[0][:, :], ones_bf[:, :], rsq[i][:, :], c == 0, c == DC - 1, reads=(rsq_b[i], consts_b), writes=(psB[0],),
               tick=True)
        act(lambda: nc.scalar.activation(out=rstd2[:, :], in_=psb[0][:, :], func=AF.Sqrt, bias=epsb[:, 0:1], scale=1.0 / D),
            reads=(psB[0], consts_b), writes=(rstd2_b,))
        dve(lambda: nc.vector.reciprocal(out=rstd2[:, :], in_=rstd2[:, :]), reads=(rstd2_b,), writes=(rstd2_b,))
        for c in range(DC):
            i = c % 4
            dve(lambda c=c, i=i: nc.vector.scalar_tensor_tensor(
                out=rtmp[i][:, :], in0=xy[:, c, :], scalar=GT[:, ls * DC + c: ls * DC + c + 1], in1=rstd2[:, :],
                op0=ALU.mult, op1=ALU.mult), reads=(xy_b, rstd2_b, consts_b), writes=(rtmp_b[i],))
            cx.dma("pool", xT[c * 128:(c + 1) * 128, t * T:(t + 1) * T], rtmp[i][:, :],
                   reads=(rtmp_b[i],), writes=(), sembuf=rtmp_b[i], accum_op=ALU.add)
            xT_w.append((rtmp_b[i].sem, rtmp_b[i].semv))

    with ExitStack() as tes:
        xin4 = tes.enter_context(nc.sbuf_tensor("xin4", [128, 4, D], F32))
        xin_b = Buf("xin4")
        for t in range(NT):
            cx.dma("sp", xin4[:, :, :], x_in[t * T:(t + 1) * T, :].rearrange("(b p) f -> p b f", p=128), writes=(xin_b,))
            for c in range(DC):
                pb = 1 + (c % 2)
                for b in range(4):
                    tr(psb[pb][:, b * 128:(b + 1) * 128], xin4[:, b, c * 128:(c + 1) * 128], ident[:, :],
                       reads=(xin_b, consts_b), writes=(psB[pb],), tick=(b == 3))
                act(lambda c=c, pb=pb: nc.scalar.copy(out=xy[:, c, :], in_=psb[pb][:, :]), reads=(psB[pb],), writes=(xy_b,))
            cx.dma("pool", xT[:, t * T:(t + 1) * T].rearrange("(c p) n -> p c n", p=128), xy[:, :, :],
                   reads=(xy_b,), writes=(xT_b,), sembuf=xy_b)
        for e in ("pe", "sp"):
            cx.drain(e, [xin_b])

    ropec = cx.sb("ropec_sb", [128, 4], F32)
    cx.dma("sp", ropec[:, :], ropec_in[:, :], writes=(consts_b,))
    TWO_PI_HI = 6.28125
    TWO_PI_LO = 2.0 * math.pi - 6.28125
    if tabs:
        with ExitStack() as tes:
            posi = tes.enter_context(nc.sbuf_tensor("posi", [128, T], I32))
            posf = tes.enter_context(nc.sbuf_tensor("posf", [128, T], F32))
            ang = tes.enter_context(nc.sbuf_tensor("ang", [128, T], F32))
            uu = tes.enter_context(nc.sbuf_tensor("uu", [128, T], F32))
            ki = tes.enter_context(nc.sbuf_tensor("ki", [128, T], I32))
            kf = tes.enter_context(nc.sbuf_tensor("kf", [128, T], F32))
            rr = [tes.enter_context(nc.sbuf_tensor("rr%d" % i, [128, T], F32)) for i in range(2)]
            tb_ = {n: Buf(n) for n in ("posi", "posf", "ang", "uu", "ki", "kf", "rr0", "rr1")}
            it = 0
            for t in range(NT):
                cx.dma("sp", posi[:, :], pos_in[0:1, t * T:(t + 1) * T].partition_broadcast(128), writes=(tb_["posi"],))
                dve(lambda: nc.vector.tensor_copy(out=posf[:, :], in_=posi[:, :]), reads=(tb_["posi"],), writes=(tb_["posf"],))
                for key, col in (("r", 0), ("d", 1)):
                    if key not in tabs:
                        continue
                    dve(lambda col=col: nc.vector.tensor_scalar(out=ang[:, :], in0=posf[:, :], scalar1=ropec[:, col:col + 1],
                                                                scalar2=None, op0=ALU.mult),
                        reads=(tb_["posf"], consts_b), writes=(tb_["ang"],))
                    for ph, which in ((math.pi / 2, 0), (0.0, 1)):
                        r = rr[it % 2]
                        rb = tb_["rr%d" % (it % 2)]
                        it += 1
                        dve(lambda ph=ph: nc.vector.tensor_scalar(out=uu[:, :], in0=ang[:, :], scalar1=1.0 / (2 * math.pi),
                                                                  scalar2=ph / (2 * math.pi), op0=ALU.mult, op1=ALU.add),
                            reads=(tb_["ang"],), writes=(tb_["uu"],))
                        dve(lambda: nc.vector.tensor_copy(out=ki[:, :], in_=uu[:, :]), reads=(tb_["uu"],), writes=(tb_["ki"],))
                        dve(lambda: nc.vector.tensor_copy(out=kf[:, :], in_=ki[:, :]), reads=(tb_["ki"],), writes=(tb_["kf"],))
                        dve(lambda r=r: nc.vector.scalar_tensor_tensor(out=r[:, :], in0=kf[:, :], scalar=-TWO_PI_HI, in1=ang[:, :],
                                                                       op0=ALU.mult, op1=ALU.add),
                            reads=(tb_["kf"], tb_["ang"]), writes=(rb,))
                        dve(lambda r=r: nc.vector.scalar_tensor_tensor(out=r[:, :], in0=kf[:, :], scalar=-TWO_PI_LO, in1=r[:, :],
                                                                       op0=ALU.mult, op1=ALU.add),
                            reads=(tb_["kf"], rb), writes=(rb,))
                        dve(lambda r=r, ph=ph: nc.vector.tensor_scalar(out=r[:, :], in0=r[:, :], scalar1=ph, scalar2=math.pi,
                                                                       op0=ALU.add, op1=ALU.min),
                            reads=(rb,), writes=(rb,))
                        dve(lambda r=r: nc.vector.tensor_scalar(out=r[:, :], in0=r[:, :], scalar1=-math.pi, scalar2=None,
                                                                op0=ALU.max), reads=(rb,), writes=(rb,))
                        act(lambda r=r: nc.scalar.activation(out=r[:, :], in_=r[:, :], func=AF.Sin), reads=(rb,), writes=(rb,))
                        cx.dma("pool", tabs[key][which][:, t * T:(t + 1) * T], r[:, :], reads=(rb,), writes=(), sembuf=rb)
            for r_ in ("rr0", "rr1"):
                tabs_b.w = None
            cx.drain("dve", list(tb_.values()))
            cx.drain("pool", list(tb_.values()))
            cx.drain("sp", list(tb_.values()))
            cx.drain("act", list(tb_.values()))
    uid = [0]

    def sbt(st, name, shape, dt):
        uid[0] += 1
        return st.enter_context(nc.sbuf_tensor("%s_%d" % (name, uid[0]), shape, dt))

    def conv_layer(li, slot, w1s, w2s):
        ls = li * 2
        with ExitStack() as fes:
            cb1T = sbt(fes, "cb1T", [128, 32], F32)
            cdwT = sbt(fes, "cdwT", [128, CONV_W * DC], F32)
            cvT = sbt(fes, "cvT", [128, 4 * DC], F32)
            load_T(cb1T[:, :], conv_b_pw1[:, :], 32)
            for i in range(0, CONV_W * DC, 128):
                r = min(128, CONV_W * DC - i)
                load_T(cdwT[:, i:i + r], conv_w_dw[i:i + r, :], r)
            load_T(cvT[:, :], conv_vecs[:, :], 64)
            wa = [sbt(fes, "wa", [128, DC, 256], BF16) for i in range(2)]
            wb = [sbt(fes, "wb", [128, DC, 256], BF16) for i in range(2)]
            wa_b = [Buf("wa") for i in range(2)]
            wb_b = [Buf("wb") for i in range(2)]
            ub = [sbt(fes, "ub", [128, T + 30], F32) for i in range(2)]
            ub_b = [Buf("ub") for i in range(2)]
            sgc = [sbt(fes, "sgc", [128, T], F32) for i in range(2)]
            sgc_b = [Buf("sgc") for i in range(2)]
            vbuf = sbt(fes, "vbuf", [128, DC, T], F32)
            v_b = [Buf("v%d" % j) for j in range(DC)]
            chalo = sbt(fes, "chalo", [128, DC, 30], F32)
            chalo_b = Buf("chalo")
            mu = sbt(fes, "mu", [128, T], F32)
            mu_b = Buf("mu")
            m2 = sbt(fes, "m2", [128, T], F32)
            m2_b = Buf("m2")
            dve(lambda: nc.vector.memset(chalo[:, :, :], 0.0), writes=(chalo_b,))
            hs = sbt(fes, "hs", [128, DC, T], BF16)
            hs_b = Buf("hs")
            norm_tile(0, ls)
            hbuf, h_b = hbuf0, hbuf0_b
            for t in range(NT):
                for s2 in range(DC // 2):
                    i3 = s2 % 2
                    cx.dma("sp", wa[i3][:, :, :], w1s.slab(s2), reads=(w1s.b,), writes=(wa_b[i3],))
                    cx.dma("sp", wb[i3][:, :, :], w1s.slab(DC // 2 + s2), reads=(w1s.b,), writes=(wb_b[i3],))
                    for jj in range(2):
                        j = s2 * 2 + jj
                        i2 = j % 2
                        ap_, bp_ = 1 + i2, 3 + i2
                        for k in range(DC):
                            mm(psb[ap_][:, :], wa[i3][:, k, jj * 128:(jj + 1) * 128], hbuf[:, k, :], k == 0, k == DC - 1,
                               reads=(wa_b[i3], h_b), writes=(psB[ap_],))
                        for k in range(DC):
                            mm(psb[bp_][:, :], wb[i3][:, k, jj * 128:(jj + 1) * 128], hbuf[:, k, :], k == 0, k == DC - 1,
                               reads=(wb_b[i3], h_b), writes=(psB[bp_],))
                        u = ub[i2]
                        act(lambda i2=i2, bp_=bp_, j=j: nc.scalar.activation(out=sgc[i2][:, :], in_=psb[bp_][:, :], func=AF.Sigmoid,
                                                                            bias=cb1T[:, DC + j:DC + j + 1], scale=1.0),
                            reads=(psB[bp_], consts_b), writes=(sgc_b[i2],))
                        act(lambda u=u, j=j: nc.scalar.copy(out=u[:, 0:30], in_=chalo[:, j, :]), reads=(chalo_b,), writes=(ub_b[i2],))
                        dve(lambda u=u, i2=i2, ap_=ap_, j=j: nc.vector.scalar_tensor_tensor(
                            out=u[:, 30:T + 30], in0=psb[ap_][:, :], scalar=cb1T[:, j:j + 1], in1=sgc[i2][:, :],
                            op0=ALU.add, op1=ALU.mult), reads=(psB[ap_], sgc_b[i2], consts_b), writes=(ub_b[i2],))
                        act(lambda u=u, j=j: nc.scalar.activation(out=vbuf[:, j, :], in_=u[:, 30:T + 30], func=AF.Identity,
                                                                  bias=cvT[:, j:j + 1], scale=cdwT[:, 30 * DC + j:30 * DC + j + 1]),
                            reads=(ub_b[i2], consts_b), writes=(v_b[j],))
                        for tau in range(30):
                            dve(lambda u=u, j=j, tau=tau: nc.vector.scalar_tensor_tensor(
                                out=vbuf[:, j, :], in0=u[:, tau:tau + T], scalar=cdwT[:, tau * DC + j:tau * DC + j + 1],
                                in1=vbuf[:, j, :], op0=ALU.mult, op1=ALU.add), reads=(ub_b[i2], v_b[j], consts_b), writes=(v_b[j],))
                        dve(lambda u=u, j=j: nc.vector.tensor_copy(out=chalo[:, j, :], in_=u[:, T:T + 30]),
                            reads=(ub_b[i2],), writes=(chalo_b,))
                if t + 1 < NT:
                    norm_tile(t + 1, ls)
                for j in range(DC):
                    i = j % 2
                    act(lambda j=j, i=i: nc.scalar.copy(out=sq[i][:, :], in_=vbuf[:, j, :]), reads=(v_b[j],), writes=(sq_b[i],))
                    mm(psb[0][:, :], ones_bf[:, :], sq[i][:, :], j == 0, j == DC - 1, reads=(sq_b[i], consts_b), writes=(psB[0],),
                       tick=True)
                for j in range(DC):
                    i = j % 2
                    act(lambda j=j, i=i: nc.scalar.activation(out=sq[i][:, :], in_=vbuf[:, j, :], func=AF.Square),
                        reads=(v_b[j],), writes=(sq_b[i],))
                    mm(psb[5][:, :], ones_bf[:, :], sq[i][:, :], j == 0, j == DC - 1, reads=(sq_b[i], consts_b), writes=(psB[5],),
                       tick=True)
                dve(lambda: nc.vector.tensor_scalar(out=mu[:, :], in0=psb[0][:, :], scalar1=1.0 / D, scalar2=None, op0=ALU.mult),
                    reads=(psB[0],), writes=(mu_b,))
                dve(lambda: nc.vector.tensor_tensor(out=m2[:, :], in0=mu[:, :], in1=mu[:, :], op=ALU.mult),
                    reads=(mu_b,), writes=(m2_b,))
                dve(lambda: nc.vector.scalar_tensor_tensor(out=m2[:, :], in0=psb[5][:, :], scalar=1.0 / D, in1=m2[:, :],
                                                           op0=ALU.mult, op1=ALU.subtract), reads=(psB[5], m2_b), writes=(m2_b,))
                act(lambda: nc.scalar.activation(out=rstd[:, :], in_=m2[:, :], func=AF.Sqrt, bias=epsb[:, 0:1], scale=1.0),
                    reads=(m2_b, consts_b), writes=(rstd_b,))
                dve(lambda: nc.vector.reciprocal(out=rstd[:, :], in_=rstd[:, :]), reads=(rstd_b,), writes=(rstd_b,))
                for j in range(DC):
                    i = j % 2
                    dve(lambda j=j, i=i: nc.vector.tensor_tensor(out=tmpf[i][:, :], in0=vbuf[:, j, :], in1=mu[:, :], op=ALU.subtract),
                        reads=(v_b[j], mu_b), writes=(tmpf_b[i],))
                    dve(lambda j=j, i=i: nc.vector.scalar_tensor_tensor(
                        out=tmpf[i][:, :], in0=tmpf[i][:, :], scalar=cvT[:, DC + j:DC + j + 1], in1=rstd[:, :],
                        op0=ALU.mult, op1=ALU.mult), reads=(tmpf_b[i], rstd_b, consts_b), writes=(tmpf_b[i],))
                    act(lambda j=j, i=i: nc.scalar.activation(out=hs[:, j, :], in_=tmpf[i][:, :], func=AF.Silu,
                                                              bias=cvT[:, 2 * DC + j:2 * DC + j + 1], scale=1.0),
                        reads=(tmpf_b[i], consts_b), writes=(hs_b,))
                for s2 in range(DC // 2):
                    i3 = s2 % 2
                    cx.dma("sp", wa[i3][:, :, :], w2s.slab(s2), reads=(w2s.b,), writes=(wa_b[i3],))
                    for mi in range(2):
                        m = s2 * 2 + mi
                        yp = yb4[m % 4]
                        for k in range(DC):
                            mm(psb[yp][:, :], wa[i3][:, k, mi * 128:(mi + 1) * 128], hs[:, k, :], k == 0, k == DC - 1,
                               reads=(wa_b[i3], hs_b), writes=(psB[yp],))
                        act(lambda m=m, yp=yp: nc.scalar.activation(out=xy[:, m, :], in_=psb[yp][:, :], func=AF.Identity,
                                                                    bias=cvT[:, 3 * DC + m:3 * DC + m + 1], scale=1.0),
                            reads=(psB[yp], consts_b), writes=(xy_b,))
                resid_tile(t, ls)
            allb = wa_b + wb_b + ub_b + sgc_b + v_b + [chalo_b, mu_b, m2_b, consts_b, hs_b]
            for e in ("pe", "act", "dve", "sp", "pool"):
                cx.drain(e, allb)

    def ret_layer(li, slot, wqk, wvg, wo):
        ls = li * 2
        cosd, sind = tabs["r"]
        with ExitStack() as fes:
            retc = sbt(fes, "retc", [128, 2056], F32)
            cx.dma("sp", retc[:, :], retc_in[:, :], writes=(consts_b,))
            decT = lambda h: retc[:, h * 128:(h + 1) * 128]
            cdb = lambda h: retc[:, 1024 + h * 128:1024 + (h + 1) * 128]
            sdv = lambda h: retc[:, 2048 + h:2048 + h + 1]
            cs = [sbt(fes, "cs", [128, T], F32) for i in range(2)]
            cs_b = Buf("cs")
            wsl = [sbt(fes, "wsl", [128, DC, 512], BF16) for i in range(2)]
            wsl_b = [Buf("wsl") for i in range(2)]
            qr = sbt(fes, "qr", [128, 2, 2, T], BF16)
            kr = sbt(fes, "kr", [128, 2, 2, T], BF16)
            qr_b = [Buf("qr") for i in range(2)]
            kr_b = [Buf("kr") for i in range(2)]
            vsb = sbt(fes, "vsb", [128, 4, 2, 512], BF16)
            gsb = sbt(fes, "gsb", [128, 4, 2, 512], BF16)
            v_b = [[Buf("v") for hh in range(2)] for tb in range(4)]
            g_b = [[Buf("g") for hh in range(2)] for tb in range(4)]
            oT = sbt(fes, "oT", [128, 8, T], BF16)
            oT_b = Buf("oT")
            stf = sbt(fes, "stf", [128, 8, 2, 512], F32)
            stf_b = [Buf("stf") for h in range(8)]
            sbf = [sbt(fes, "sbf", [128, 2, 512], BF16) for i in range(2)]
            sbf_b = [Buf("sbf") for i in range(2)]
            rt = tmpf
            rt_b = tmpf_b
            pt = [sbt(fes, "pt", [128, 128], BF16) for i in range(2)]
            pt_b = [Buf("pt") for i in range(2)]
            ktm = [sbt(fes, "ktm", [128, 256], BF16) for i in range(2)]
            ktm_b = [Buf("ktm") for i in range(2)]
            qs = [sbt(fes, "qs", [128, 2, 128], BF16) for i in range(2)]
            qs_b = [Buf("qs") for i in range(2)]
            on = [sbt(fes, "on", [128, 512], BF16) for i in range(2)]
            on_b = [Buf("on") for i in range(2)]
            st6 = [sbt(fes, "st6", [128, 6], F32) for i in range(2)]
            mv = [sbt(fes, "mv", [128, 4], F32) for i in range(2)]
            mv_b = [Buf("mv") for i in range(2)]
            dve(lambda: nc.vector.memset(stf[:, :, :, :], 0.0), writes=tuple(stf_b))
            sl = [0]

            def next_slab():
                sl[0] += 1
                return sl[0] % 2

            pc = [0]
            norm_tile(0, ls)
            hbuf, h_b = hbuf0, hbuf0_b
            for t in range(NT):
                cx.dma("sp", cs[0][:, :], cosd[:, t * T:(t + 1) * T], reads=(tabs_b,), writes=(cs_b,))
                cx.dma("sp", cs[1][:, :], sind[:, t * T:(t + 1) * T], reads=(tabs_b,), writes=(cs_b,))
                for g in range(4):
                    for hh in range(2):
                        h = 2 * g + hh
                        for which in range(2):
                            si = next_slab()
                            w = wsl[si]
                            cx.dma("sp", w[:, :, 0:256], wqk.slab(which * 8 + h), reads=(wqk.b,), writes=(wsl_b[si],))
                            i2 = pc[0] % 2
                            pc[0] += 1
                            p1, p2 = 1 + i2, 3 + i2
                            for k in range(DC):
                                mm(psb[p1][:, :], w[:, k, 0:128], hbuf[:, k, :], k == 0, k == DC - 1,
                                   reads=(wsl_b[si], h_b), writes=(psB[p1],))
                            for k in range(DC):
                                mm(psb[p2][:, :], w[:, k, 128:256], hbuf[:, k, :], k == 0, k == DC - 1,
                                   reads=(wsl_b[si], h_b), writes=(psB[p2],))
                            c_, s_ = cs[0], cs[1]
                            dst = qr if which == 0 else kr
                            dst_b = (qr_b if which == 0 else kr_b)[hh]
                            for (pa, ca, pb_, cb_, half, op_) in ((p1, c_, p2, s_, 0, ALU.subtract), (p2, c_, p1, s_, 1, ALU.add)):
                                a_, b_ = 0, 1
                                dve(lambda pa=pa, ca=ca, a_=a_: nc.vector.tensor_tensor(out=rt[a_][:, :], in0=psb[pa][:, :],
                                                                                         in1=ca[:, :], op=ALU.mult),
                                    reads=(psB[pa], cs_b), writes=(rt_b[a_],))
                                dve(lambda pb_=pb_, cb_=cb_, b_=b_: nc.vector.tensor_tensor(out=rt[b_][:, :], in0=psb[pb_][:, :],
                                                                                             in1=cb_[:, :], op=ALU.mult),
                                    reads=(psB[pb_], cs_b), writes=(rt_b[b_],))
                                pool(lambda dst=dst, hh=hh, half=half, a_=a_, b_=b_, op_=op_: nc.gpsimd.tensor_tensor(
                                    out=dst[:, hh, half, :], in0=rt[a_][:, :], in1=rt[b_][:, :], op=op_),
                                    reads=(rt_b[a_], rt_b[b_]), writes=(dst_b,))
                    for hh in range(2):
                        h = 2 * g + hh
                        for which in range(2):
                            si = next_slab()
                            w = wsl[si]
                            cx.dma("sp", w[:, :, :], wvg.slab(which * 8 + h), reads=(wvg.b,), writes=(wsl_b[si],))
                            for tb in range(4):
                                i2 = pc[0] % 2
                                pc[0] += 1
                                vp = 5 + i2
                                for k in range(DC):
                                    mm(psb[vp][:, :], hbuf[:, k, tb * 128:(tb + 1) * 128], w[:, k, :], k == 0, k == DC - 1,
                                       reads=(wsl_b[si], h_b), writes=(psB[vp],))
                                if which == 0:
                                    act(lambda tb=tb, hh=hh, vp=vp: nc.scalar.copy(out=vsb[:, tb, hh, :], in_=psb[vp][:, :]),
                                        reads=(psB[vp],), writes=(v_b[tb][hh],))
                                else:
                                    act(lambda tb=tb, hh=hh, vp=vp: nc.scalar.activation(out=gsb[:, tb, hh, :], in_=psb[vp][:, :],
                                                                                        func=AF.Silu),
                                        reads=(psB[vp],), writes=(g_b[tb][hh],))
                    if g == 3 and t + 1 < NT:
                        norm_tile(t + 1, ls)
                    for tb in range(4):
                        tsl = slice(tb * 128, (tb + 1) * 128)
                        for hh in range(2):
                            h = 2 * g + hh
                            gam128 = float(np.exp(128.0 * np.log1p(-np.exp2(-5.0 - h))))
                            for half in range(2):
                                mm(psb[0][:, hh * 128:(hh + 1) * 128], kr[:, hh, half, tsl], qr[:, hh, half, tsl], half == 0, half == 1,
                                   reads=(kr_b[hh], qr_b[hh]), writes=(psB[0],))
                            dve(lambda hh=hh, h=h: nc.vector.tensor_tensor(out=pt[hh][:, :], in0=psb[0][:, hh * 128:(hh + 1) * 128],
                                                                          in1=decT(h), op=ALU.mult),
                                reads=(psB[0], consts_b), writes=(pt_b[hh],))
                            for half in range(2):
                                tr(pst[:, hh * 512 + half * 128: hh * 512 + (half + 1) * 128], kr[:, hh, half, tsl], ident_bf[:, :],
                                   reads=(kr_b[hh], consts_b), writes=(pst_b[hh],), tick=(half == 1))
                            dve(lambda hh=hh, h=h: nc.vector.tensor_scalar(out=ktm[hh][:, :], in0=pst[:, hh * 512: hh * 512 + 256],
                                                                          scalar1=sdv(h), scalar2=None, op0=ALU.mult),
                                reads=(pst_b[hh], consts_b), writes=(ktm_b[hh],))
                            for half in range(2):
                                pool(lambda hh=hh, h=h, half=half, tsl=tsl: nc.gpsimd.tensor_tensor(
                                    out=qs[hh][:, half, :], in0=qr[:, hh, half, tsl], in1=cdb(h), op=ALU.mult),
                                    reads=(qr_b[hh], consts_b), writes=(qs_b[hh],))
                            act(lambda hh=hh, h=h: nc.scalar.copy(out=sbf[hh][:, :, :], in_=stf[:, h, :, :]),
                                reads=(stf_b[h],), writes=(sbf_b[hh],))
                            op_ = 1 + hh
                            mm(psb[op_][:, :], pt[hh][:, :], vsb[:, tb, hh, :], True, False,
                               reads=(pt_b[hh], v_b[tb][hh]), writes=(psB[op_],))
                            mm(psb[op_][:, :], qs[hh][:, 0, :], sbf[hh][:, 0, :], False, False,
                               reads=(qs_b[hh], sbf_b[hh]), writes=(psB[op_],))
                            mm(psb[op_][:, :], qs[hh][:, 1, :], sbf[hh][:, 1, :], False, True,
                               reads=(qs_b[hh], sbf_b[hh]), writes=(psB[op_],))
                            for half in range(2):
                                nsb = 3 + 2 * hh + half
                                mm(psb[nsb][:, :], ktm[hh][:, half * 128:(half + 1) * 128], vsb[:, tb, hh, :], True, True,
                                   reads=(ktm_b[hh], v_b[tb][hh]), writes=(psB[nsb],))
                                dve(lambda h=h, half=half, nsb=nsb, gam128=gam128: nc.vector.scalar_tensor_tensor(
                                    out=stf[:, h, half, :], in0=stf[:, h, half, :], scalar=gam128, in1=psb[nsb][:, :],
                                    op0=ALU.mult, op1=ALU.add), reads=(psB[nsb], stf_b[h]), writes=(stf_b[h],))
                            dve(lambda hh=hh, op_=op_: nc.vector.bn_stats(out=st6[hh][:, :], in_=psb[op_][:, :]),
                                reads=(psB[op_],), writes=(mv_b[hh],))
                            dve(lambda hh=hh: nc.vector.bn_aggr(out=mv[hh][:, 0:2], in_=st6[hh][:, :]), reads=(mv_b[hh],), writes=(mv_b[hh],))
                            act(lambda hh=hh: nc.scalar.activation(out=mv[hh][:, 2:3], in_=mv[hh][:, 1:2], func=AF.Sqrt,
                                                                   bias=epsb[:, 0:1], scale=1.0),
                                reads=(mv_b[hh], consts_b), writes=(mv_b[hh],))
                            dve(lambda hh=hh: nc.vector.reciprocal(out=mv[hh][:, 2:3], in_=mv[hh][:, 2:3]), reads=(mv_b[hh],), writes=(mv_b[hh],))
                            dve(lambda hh=hh: nc.vector.scalar_tensor_tensor(out=mv[hh][:, 3:4], in0=mv[hh][:, 0:1], scalar=-1.0,
                                                                             in1=mv[hh][:, 2:3], op0=ALU.mult, op1=ALU.mult),
                                reads=(mv_b[hh],), writes=(mv_b[hh],))
                            act(lambda hh=hh, op_=op_: nc.scalar.activation(out=on[hh][:, :], in_=psb[op_][:, :], func=AF.Identity,
                                                                            bias=mv[hh][:, 3:4], scale=mv[hh][:, 2:3]),
                                reads=(psB[op_], mv_b[hh]), writes=(on_b[hh],))
                            pool(lambda hh=hh, tb=tb: nc.gpsimd.tensor_tensor(out=gsb[:, tb, hh, :], in0=on[hh][:, :],
                                                                              in1=gsb[:, tb, hh, :], op=ALU.mult),
                                 reads=(on_b[hh], g_b[tb][hh]), writes=(g_b[tb][hh],))
                    for tb in range(4):
                        for hh in range(2):
                            for q4 in range(4):
                                jc = hh * 4 + q4
                                tr(pst[:, jc * 128:(jc + 1) * 128], gsb[:, tb, hh, q4 * 128:(q4 + 1) * 128], ident_bf[:, :],
                                   reads=(g_b[tb][hh], consts_b), writes=(pst_b[hh],), tick=(q4 == 3))
                        act(lambda tb=tb: nc.scalar.copy(out=oT[:, :, tb * 128:(tb + 1) * 128],
                                                         in_=pst[:, :].rearrange("p (j n) -> p j n", j=8)),
                            reads=(pst_b[0], pst_b[1]), writes=(oT_b,))
                    for s2 in range(DC // 2):
                        si = next_slab()
                        w = wsl[si]
                        cx.dma("sp", w[:, 0:8, 0:256], wo.t[:, s2, 8 * g:8 * g + 8, :], reads=(wo.b,), writes=(wsl_b[si],))
                        for mi in range(2):
                            m = s2 * 2 + mi
                            yp = 5 + (m % 2)
                            for jj in range(8):
                                mm(psb[yp][:, :], w[:, jj, mi * 128:(mi + 1) * 128], oT[:, jj, :], jj == 0, jj == 7,
                                   reads=(wsl_b[si], oT_b), writes=(psB[yp],))
                            if g == 0:
                                act(lambda m=m, yp=yp: nc.scalar.copy(out=xy[:, m, :], in_=psb[yp][:, :]),
                                    reads=(psB[yp],), writes=(xy_b,))
                            else:
                                dve(lambda m=m, yp=yp: nc.vector.tensor_tensor(out=xy[:, m, :], in0=xy[:, m, :], in1=psb[yp][:, :],
                                                                               op=ALU.add), reads=(psB[yp], xy_b), writes=(xy_b,))
                resid_tile(t, ls)
            allb = (wsl_b + qr_b + kr_b + [b for r in v_b for b in r] + [b for r in g_b for b in r] + [oT_b, cs_b, consts_b]
                    + stf_b + sbf_b + rt_b + pt_b + ktm_b + qs_b + on_b + mv_b)
            for e in ("pe", "act", "dve", "sp", "pool"):
                cx.drain(e, allb)

    yb4 = [5, 6, 1, 2]

    tric_in = din("tric", [128, 128])

    class WSperm:
        def __init__(self, name, src):
            self.t = cx.dram(name, [128, 16, DC, 256], BF16)
            self.b = Buf(name)
            self.b.sem = es.enter_context(nc.semaphore("w_" + name))
            self.todo = []
            for k in range(DC):
                sv = src[k * 128:(k + 1) * 128, 0:4096].rearrange("p (s c hf f) -> p s c hf f", s=16, c=2, hf=2, f=64)
                for hf in range(2):
                    self.todo.append((self.t[:, :, k, hf * 128:(hf + 1) * 128].rearrange("p s (c f) -> p s c f", c=2),
                                      sv[:, :, :, hf, :]))
            self.b.w = (self.b.sem, 16 * len(self.todo))

        def issue(self, n):
            for _ in range(min(n, len(self.todo))):
                o, i = self.todo.pop(0)
                nc.gpsimd.dma_start(out=o, in_=i).then_inc(self.b.sem, 16)

        def slab(self, s):
            return self.t[:, s, :, :]

    def diff_weights(li, slot):
        return [WSperm("w_dqk%d" % li, diff_w_in[slot]), WS("w_dv%d" % li, diff_w_in[slot], D, 2048, 512, col0=4096),
                WS("w_do%d" % li, diff_w_out[slot], D, D, 256)]

    def diff_layer(li, slot, wts):
        wqk, wv, wo = wts
        ls = li * 2
        cosd, sind = tabs["d"]
        NBK = S // 128
        lam_init = 0.8 - 0.6 * math.exp(-0.3 * li)
        qT_d = cx.dram("dq%d" % li, [8, 2, 128, S], BF16)
        kT_d = cx.dram("dk%d" % li, [8, 2, 128, S], BF16)
        V_d = cx.dram("dv%d" % li, [S, 8, 256], BF16)
        oT_d = cx.dram("do%d" % li, [D, S], BF16)
        scr_b = Buf("dscr")
        with ExitStack() as fes:
            cs = [sbt(fes, "cs", [128, T], F32) for i in range(2)]
            cs_b = Buf("cs")
            wsl = [sbt(fes, "wsl", [128, DC, 512], BF16) for i in range(2)]
            wsl_b = [Buf("wsl") for i in range(2)]
            rt = [sbt(fes, "rt", [128, T], F32) for i in range(4)]
            rt_b = [Buf("rt") for i in range(4)]
            ro = [sbt(fes, "ro", [128, T], BF16) for i in range(4)]
            ro_b = [Buf("ro") for i in range(4)]
            vst = [sbt(fes, "vst", [128, 512], BF16) for i in range(2)]
            vst_b = [Buf("vst") for i in range(2)]
            sl = 0
            pc = 0
            rc = 0
            hA = [(hbuf0, hbuf0_b), (sbt(fes, "hb1", [128, DC, T], BF16), Buf("hb1"))]
            norm_tile(0, ls, hA[0])
            for t in range(NT):
                hbuf, h_b = hA[t % 2]
                cx.dma("sp", cs[0][:, :], cosd[:, t * T:(t + 1) * T], writes=(cs_b,))
                cx.dma("sp", cs[1][:, :], sind[:, t * T:(t + 1) * T], writes=(cs_b,))
                for h in range(8):
                    if h == 4 and t + 1 < NT:
                        norm_tile(t + 1, ls, hA[(t + 1) % 2])
                    for which in range(2):
                        sl += 1
                        si = sl % 2
                        w = wsl[si]
                        cx.dma("sp", w[:, :, 0:256], wqk.slab(which * 8 + h), reads=(wqk.b,), writes=(wsl_b[si],))
                        i2 = pc % 2
                        pc += 1
                        p1, p2 = 1 + i2, 3 + i2
                        for k in range(DC):
                            mm(psb[p1][:, :], w[:, k, 0:128], hbuf[:, k, :], k == 0, k == DC - 1, reads=(wsl_b[si], h_b), writes=(psB[p1],))
                        for k in range(DC):
                            mm(psb[p2][:, :], w[:, k, 128:256], hbuf[:, k, :], k == 0, k == DC - 1, reads=(wsl_b[si], h_b), writes=(psB[p2],))
                        dstd = qT_d if which == 0 else kT_d
                        for (pa, pb_, half, op_) in ((p1, p2, 0, ALU.subtract), (p2, p1, 1, ALU.add)):
                            a_, b_ = (0, 1) if half == 0 else (2, 3)
                            oi = rc % 4
                            rc += 1
                            dve(lambda pa=pa, a_=a_: nc.vector.tensor_tensor(out=rt[a_][:, :], in0=psb[pa][:, :], in1=cs[0][:, :], op=ALU.mult),
                                reads=(psB[pa], cs_b), writes=(rt_b[a_],))
                            dve(lambda pb_=pb_, b_=b_: nc.vector.tensor_tensor(out=rt[b_][:, :], in0=psb[pb_][:, :], in1=cs[1][:, :], op=ALU.mult),
                                reads=(psB[pb_], cs_b), writes=(rt_b[b_],))
                            pool(lambda oi=oi, a_=a_, b_=b_, op_=op_: nc.gpsimd.tensor_tensor(out=ro[oi][:, :], in0=rt[a_][:, :],
                                                                                             in1=rt[b_][:, :], op=op_),
                                 reads=(rt_b[a_], rt_b[b_]), writes=(ro_b[oi],))
                            cx.dma("pool", dstd[h, half, :, t * T:(t + 1) * T], ro[oi][:, :], reads=(ro_b[oi],), writes=(),
                                   sembuf=ro_b[oi])
                for s4 in range(4):
                    sl += 1
                    si = sl % 2
                    w = wsl[si]
                    cx.dma("sp", w[:, :, :], wv.slab(s4), reads=(wv.b,), writes=(wsl_b[si],))
                    for tb in range(4):
                        i2 = pc % 2
                        pc += 1
                        vp = 5 + i2
                        for k in range(DC):
                            mm(psb[vp][:, :], hbuf[:, k, tb * 128:(tb + 1) * 128], w[:, k, :], k == 0, k == DC - 1,
                               reads=(wsl_b[si], h_b), writes=(psB[vp],))
                        act(lambda i2=i2, vp=vp: nc.scalar.copy(out=vst[i2][:, :], in_=psb[vp][:, :]), reads=(psB[vp],), writes=(vst_b[i2],))
                        r0 = t * T + tb * 128
                        cx.dma("pool", V_d[r0:r0 + 128, 2 * s4:2 * s4 + 2, :], vst[i2][:, :].rearrange("p (h v) -> p h v", h=2),
                               reads=(vst_b[i2],), writes=(), sembuf=vst_b[i2])
            allb = wsl_b + rt_b + ro_b + vst_b + [cs_b, hA[1][1]]
            for e in ("pe", "act", "dve", "sp", "pool"):
                cx.drain(e, allb)
        with ExitStack() as fes:
            kT = sbt(fes, "kT", [128, 2, S], BF16)
            qT = sbt(fes, "qT", [128, 2, S], BF16)
            Vh = sbt(fes, "Vh", [128, NBK, 257], BF16)
            kqv_b = Buf("kqv")
            oTh = sbt(fes, "oTh", [128, 2, S], BF16)
            oTh_b = Buf("oTh")
            pT = [sbt(fes, "pT", [128, 512], BF16) for i in range(2)]
            pT_b = [Buf("pT") for i in range(2)]
            oc = [sbt(fes, "oc", [128, 4, 257], F32) for i in range(2)]
            oc_b = [[Buf("oc") for qb in range(4)] for i in range(2)]
            of = [sbt(fes, "of", [128, 256], F32) for i in range(2)]
            of_b = [Buf("of") for i in range(2)]
            ob = [sbt(fes, "ob", [128, 256], BF16) for i in range(2)]
            ob_b = [Buf("ob") for i in range(2)]
            junk = sbt(fes, "junk", [128, 256], F32)
            sm = [sbt(fes, "sm", [128, 8], F32) for i in range(2)]
            sm_b = [Buf("sm") for i in range(2)]
            tri = sbt(fes, "tri", [128, 128], BF16)
            trif = sbt(fes, "trif", [128, 128], F32)
            lamb = sbt(fes, "lamb", [128, 512], F32)
            lw = sbt(fes, "lw", [128, 256], F32)
            lam = sbt(fes, "lam", [128, 4], F32)
            sgl = sbt(fes, "sgl", [128, 256], F32)
            dc_b = Buf("dconst")
            cx.dma("sp", trif[:, :], tric_in[:, :], writes=(dc_b,))
            dve(lambda: nc.vector.tensor_copy(out=tri[:, :], in_=trif[:, :]), reads=(dc_b,), writes=(dc_b,))
            cx.dma("sp", lamb[:, :], diff_lambda[0:1, :].partition_broadcast(128), writes=(dc_b,))
            cx.dma("sp", sgl[:, :], diff_subln_g[0:1, :].partition_broadcast(128), writes=(dc_b,))
            dve(lambda: nc.vector.tensor_scalar(out=sgl[:, :], in0=sgl[:, :], scalar1=1.0 - lam_init, scalar2=None, op0=ALU.mult),
                reads=(dc_b,), writes=(dc_b,))
            for i in range(2):
                dve(lambda i=i: nc.vector.tensor_tensor(out=lw[:, i * 128:(i + 1) * 128], in0=lamb[:, i * 256:i * 256 + 128],
                                                        in1=lamb[:, i * 256 + 128:i * 256 + 256], op=ALU.mult),
                    reads=(dc_b,), writes=(dc_b,))
                dve(lambda i=i: nc.vector.tensor_reduce(out=lam[:, i:i + 1], in_=lw[:, i * 128:(i + 1) * 128], axis=AX.X, op=ALU.add),
                    reads=(dc_b,), writes=(dc_b,))
            act(lambda: nc.scalar.activation(out=lam[:, 0:2], in_=lam[:, 0:2], func=AF.Exp), reads=(dc_b,), writes=(dc_b,))
            dve(lambda: nc.vector.tensor_tensor(out=lam[:, 2:3], in0=lam[:, 1:2], in1=lam[:, 0:1], op=ALU.subtract),
                reads=(dc_b,), writes=(dc_b,))
            dve(lambda: nc.vector.tensor_scalar(out=lam[:, 2:3], in0=lam[:, 2:3], scalar1=-lam_init, scalar2=None, op0=ALU.add),
                reads=(dc_b,), writes=(dc_b,))
            dve(lambda: nc.vector.memset(Vh[:, :, 256:257], 1.0), writes=(kqv_b,))
            scale = float(DIFF_DH) ** -0.5
            NQ = S // 512
            kbi = 0
            fi = 0
            for h in range(8):
                for c in range(2):
                    for hf in range(2):
                        cx.dma("sp", kT[hf * 64:(hf + 1) * 64, c, :], kT_d[h, hf, c * 64:(c + 1) * 64, :], writes=(kqv_b,))
                        cx.dma("sp", qT[hf * 64:(hf + 1) * 64, c, :], qT_d[h, hf, c * 64:(c + 1) * 64, :], writes=(kqv_b,))
                cx.dma("sp", Vh[:, :, 0:256], V_d[:, h, :].rearrange("(b p) v -> p b v", p=128), writes=(kqv_b,))
                its = [(qt, c, kb) for qt in range(NQ) for c in range(2) for kb in range(4 * qt + 4)]
                slot = {}

                def front(i):
                    nonlocal kbi
                    qt, c, kb = its[i]
                    sp_ = 5 + (kbi % 2)
                    pi = kbi % 2
                    kbi += 1
                    slot[i] = pi
                    mm(psb[sp_][:, :], kT[:, c, kb * 128:(kb + 1) * 128], qT[:, c, qt * 512:(qt + 1) * 512], True, True,
                       reads=(kqv_b,), writes=(psB[sp_],))
                    act(lambda sp_=sp_, pi=pi: nc.scalar.activation(out=pT[pi][:, :], in_=psb[sp_][:, :], func=AF.Exp, scale=scale),
                        reads=(psB[sp_],), writes=(pT_b[pi],))
                    d = kb - 4 * qt
                    if d >= 0:
                        dve(lambda pi=pi, d=d: nc.vector.tensor_tensor(out=pT[pi][:, d * 128:(d + 1) * 128],
                                                                       in0=pT[pi][:, d * 128:(d + 1) * 128], in1=tri[:, :], op=ALU.mult),
                            reads=(pT_b[pi], dc_b), writes=(pT_b[pi],))

                def back(i):
                    nonlocal fi
                    qt, c, kb = its[i]
                    pi = slot.pop(i)
                    d = kb - 4 * qt
                    for qb in range(max(d, 0), 4):
                        mm(psb[1 + qb][:, 0:257], pT[pi][:, qb * 128:(qb + 1) * 128], Vh[:, kb, :], kb == 0, kb == 4 * qt + qb,
                           reads=(pT_b[pi], kqv_b), writes=(psB[1 + qb],))
                    if kb != 4 * qt + 3:
                        return
                    for qb in range(4):
                        act(lambda c=c, qb=qb: nc.scalar.copy(out=oc[c][:, qb, :], in_=psb[1 + qb][:, 0:257]),
                            reads=(psB[1 + qb],), writes=(oc_b[c][qb],))
                    if c != 1:
                        return
                    for qb in range(4):
                        f = fi % 2
                        fi += 1
                        s_ = sm[f]
                        dve(lambda s_=s_, qb=qb: nc.vector.reciprocal(out=s_[:, 0:1], in_=oc[0][:, qb, 256:257]),
                            reads=(oc_b[0][qb],), writes=(sm_b[f],))
                        dve(lambda s_=s_, qb=qb: nc.vector.reciprocal(out=s_[:, 1:2], in_=oc[1][:, qb, 256:257]),
                            reads=(oc_b[1][qb],), writes=(sm_b[f],))
                        dve(lambda s_=s_: nc.vector.tensor_tensor(out=s_[:, 1:2], in0=s_[:, 1:2], in1=lam[:, 2:3], op=ALU.mult),
                            reads=(sm_b[f], dc_b), writes=(sm_b[f],))
                        dve(lambda s_=s_, qb=qb, f=f: nc.vector.tensor_scalar(out=of[f][:, :], in0=oc[0][:, qb, 0:256], scalar1=s_[:, 0:1],
                                                                             scalar2=None, op0=ALU.mult),
                            reads=(oc_b[0][qb], sm_b[f]), writes=(of_b[f],))
                        dve(lambda s_=s_, qb=qb, f=f: nc.vector.scalar_tensor_tensor(out=of[f][:, :], in0=oc[1][:, qb, 0:256], scalar=s_[:, 1:2],
                                                                                    in1=of[f][:, :], op0=ALU.mult, op1=ALU.add),
                            reads=(oc_b[1][qb], sm_b[f], of_b[f]), writes=(of_b[f],))
                        act(lambda s_=s_, f=f: nc.scalar.activation(out=junk[:, :], in_=of[f][:, :], func=AF.Square, accum_out=s_[:, 2:3]),
                            reads=(of_b[f],), writes=(sm_b[f],))
                        act(lambda s_=s_: nc.scalar.activation(out=s_[:, 3:4], in_=s_[:, 2:3], func=AF.Sqrt, bias=epsb[:, 0:1], scale=1.0 / 256.0),
                            reads=(sm_b[f], consts_b), writes=(sm_b[f],))
                        dve(lambda s_=s_: nc.vector.reciprocal(out=s_[:, 3:4], in_=s_[:, 3:4]), reads=(sm_b[f],), writes=(sm_b[f],))
                        dve(lambda s_=s_, f=f: nc.vector.scalar_tensor_tensor(out=ob[f][:, :], in0=of[f][:, :], scalar=s_[:, 3:4], in1=sgl[:, :],
                                                                             op0=ALU.mult, op1=ALU.mult),
                            reads=(of_b[f], sm_b[f], dc_b), writes=(ob_b[f],))
                        for j in range(2):
                            tr(pst[:, f * 512 + j * 128: f * 512 + (j + 1) * 128], ob[f][:, j * 128:(j + 1) * 128], ident_bf[:, :],
                               reads=(ob_b[f], consts_b), writes=(pst_b[f],), tick=(j == 1))
                        q0 = qt * 512 + qb * 128
                        act(lambda f=f, q0=q0: nc.scalar.copy(out=oTh[:, :, q0:q0 + 128],
                                                              in_=pst[:, f * 512: f * 512 + 256].rearrange("p (j n) -> p j n", j=2)),
                            reads=(pst_b[f],), writes=(oTh_b,))

                front(0)
                for i in range(len(its)):
                    if i + 1 < len(its):
                        front(i + 1)
                    back(i)
                cx.dma("pool", oT_d[h * 256:(h + 1) * 256, :].rearrange("(j p) n -> p j n", p=128), oTh[:, :, :],
                       reads=(oTh_b,), writes=(), sembuf=oTh_b)
            allb = [kqv_b, oTh_b, dc_b] + pT_b + [b for r in oc_b for b in r] + of_b + ob_b + sm_b
            for e in ("pe", "act", "dve", "sp", "pool"):
                cx.drain(e, allb)
        with ExitStack() as fes:
            wsl = [sbt(fes, "wsl", [128, DC, 256], BF16) for i in range(2)]
            wsl_b = [Buf("wsl") for i in range(2)]
            hC = [(hbuf0, hbuf0_b), (sbt(fes, "hc1", [128, DC, T], BF16), Buf("hc1"))]
            for t in range(NT):
                hbuf, h_b = hC[t % 2]
                cx.dma("sp", hbuf[:, :, :], oT_d[:, t * T:(t + 1) * T].rearrange("(c p) n -> p c n", p=128), writes=(h_b,))
                for s2 in range(DC // 2):
                    si = s2 % 2
                    cx.dma("sp", wsl[si][:, :, :], wo.slab(s2), reads=(wo.b,), writes=(wsl_b[si],))
                    for mi in range(2):
                        m = s2 * 2 + mi
                        yp = yb4[m % 4]
                        for k in range(DC):
                            mm(psb[yp][:, :], wsl[si][:, k, mi * 128:(mi + 1) * 128], hbuf[:, k, :], k == 0, k == DC - 1,
                               reads=(wsl_b[si], h_b), writes=(psB[yp],))
                        act(lambda m=m, yp=yp: nc.scalar.copy(out=xy[:, m, :], in_=psb[yp][:, :]), reads=(psB[yp],), writes=(xy_b,))
                resid_tile(t, ls)
            for e in ("pe", "act", "dve", "sp", "pool"):
                cx.drain(e, wsl_b + [hC[1][1]])


    def ffn_layer(li, win, wdn):
        ls = li * 2 + 1
        with ExitStack() as fes:
            NB = 2
            wg = [fes.enter_context(nc.sbuf_tensor("wg%d_%d" % (li, i), [128, DC, 256], BF16)) for i in range(NB)]
            wu = [fes.enter_context(nc.sbuf_tensor("wu%d_%d" % (li, i), [128, DC, 256], BF16)) for i in range(NB)]
            wg_b = [Buf("wg%d" % i) for i in range(NB)]
            wu_b = [Buf("wu%d" % i) for i in range(NB)]
            wd = [fes.enter_context(nc.sbuf_tensor("wd%d_%d" % (li, i), [128, FC // 2, 256], BF16)) for i in range(3)]
            wd_b = [Buf("wd%d" % i) for i in range(3)]
            abuf = fes.enter_context(nc.sbuf_tensor("abuf%d" % li, [128, FC, T], BF16))
            a_b = [Buf("a%d" % j) for j in range(FC)]
            gb = [fes.enter_context(nc.sbuf_tensor("gb%d_%d" % (li, i), [128, T + 2], F32)) for i in range(2)]
            gb_b = [Buf("gb%d" % i) for i in range(2)]
            sg = [fes.enter_context(nc.sbuf_tensor("sg%d_%d" % (li, i), [128, T], BF16)) for i in range(2)]
            sg_b = [Buf("sg%d" % i) for i in range(2)]
            halo = fes.enter_context(nc.sbuf_tensor("halo%d" % li, [128, FC, 2], F32))
            halo_b = Buf("halo")
            dve(lambda: nc.vector.memset(halo[:, :, :], 0.0), writes=(halo_b,))
            yb = [5, 6, 1, 2]

            def tap(tp, j):
                i = (li * 3 + tp) * FC + j
                return fdwT[:, i:i + 1]

            norm_tile(0, ls)
            hbuf, h_b = hbuf0, hbuf0_b
            for t in range(NT):
                for s2 in range(FC // 2):
                    i3 = s2 % NB
                    cx.dma("sp", wg[i3][:, :, :], win.slab(s2), reads=(win.b,), writes=(wg_b[i3],))
                    cx.dma("sp", wu[i3][:, :, :], win.slab(FC // 2 + s2), reads=(win.b,), writes=(wu_b[i3],))
                    for jj in range(2):
                        j = s2 * 2 + jj
                        i2 = j % 2
                        gp, up = 1 + i2, 3 + i2
                        for k in range(DC):
                            mm(psb[gp][:, :], wg[i3][:, k, jj * 128:(jj + 1) * 128], hbuf[:, k, :], k == 0, k == DC - 1,
                               reads=(wg_b[i3], h_b), writes=(psB[gp],))
                        for k in range(DC):
                            mm(psb[up][:, :], wu[i3][:, k, jj * 128:(jj + 1) * 128], hbuf[:, k, :], k == 0, k == DC - 1,
                               reads=(wu_b[i3], h_b), writes=(psB[up],))
                        g = gb[i2]
                        act(lambda g=g, gp=gp: nc.scalar.copy(out=g[:, 2:T + 2], in_=psb[gp][:, :]),
                            reads=(psB[gp],), writes=(gb_b[i2],))
                        act(lambda g=g, j=j: nc.scalar.copy(out=g[:, 0:2], in_=halo[:, j, :]),
                            reads=(halo_b,), writes=(gb_b[i2],))
                        act(lambda gp=gp, i2=i2, j=j: nc.scalar.activation(out=tmpf[i2][:, :], in_=psb[gp][:, :],
                                                                           func=AF.Identity, scale=tap(2, j)),
                            reads=(psB[gp], consts_b), writes=(tmpf_b[i2],))
                        dve(lambda g=g, i2=i2, j=j: nc.vector.scalar_tensor_tensor(
                            out=tmpf[i2][:, :], in0=g[:, 1:T + 1], scalar=tap(1, j), in1=tmpf[i2][:, :],
                            op0=ALU.mult, op1=ALU.add), reads=(gb_b[i2], tmpf_b[i2], consts_b), writes=(tmpf_b[i2],))
                        dve(lambda g=g, i2=i2, j=j: nc.vector.scalar_tensor_tensor(
                            out=tmpf[i2][:, :], in0=g[:, 0:T], scalar=tap(0, j), in1=tmpf[i2][:, :],
                            op0=ALU.mult, op1=ALU.add), reads=(gb_b[i2], tmpf_b[i2], consts_b), writes=(tmpf_b[i2],))
                        dve(lambda g=g, j=j: nc.vector.tensor_copy(out=halo[:, j, :], in_=g[:, T:T + 2]),
                            reads=(gb_b[i2],), writes=(halo_b,))
                        act(lambda i2=i2: nc.scalar.activation(out=sg[i2][:, :], in_=tmpf[i2][:, :], func=AF.Silu),
                            reads=(tmpf_b[i2],), writes=(sg_b[i2],))
                        dve(lambda i2=i2, up=up, j=j: nc.vector.tensor_tensor(out=abuf[:, j, :], in0=sg[i2][:, :],
                                                                             in1=psb[up][:, :], op=ALU.mult),
                            reads=(sg_b[i2], psB[up]), writes=(a_b[j],))
                pc = 0
                for s2 in range(DC // 2):
                    for half in range(2):
                        if pc == 3 and t + 1 < NT:
                            norm_tile(t + 1, ls)
                        w3 = pc % 3
                        pc += 1
                        cx.dma("sp", wd[w3][:, :, :], wdn.t[:, s2, half * 22:(half + 1) * 22, :], reads=(wdn.b,),
                               writes=(wd_b[w3],))
                        for mi in range(2):
                            m = s2 * 2 + mi
                            yp = yb[m % 4]
                            for jj in range(22):
                                j = half * 22 + jj
                                mm(psb[yp][:, :], wd[w3][:, jj, mi * 128:(mi + 1) * 128], abuf[:, j, :], j == 0, j == FC - 1,
                                   reads=(wd_b[w3], a_b[j]), writes=(psB[yp],), tick=(jj == 21))
                            if half == 1:
                                act(lambda m=m, yp=yp: nc.scalar.copy(out=xy[:, m, :], in_=psb[yp][:, :]),
                                    reads=(psB[yp],), writes=(xy_b,))
                resid_tile(t, ls)
            allb = wg_b + wu_b + wd_b + a_b + gb_b + sg_b + [halo_b]
            for e in ("pe", "act", "dve", "sp", "pool"):
                cx.drain(e, allb)

    subs = []
    for (kind, slot, li) in layer_kinds:
        if with_mixer and kind == 0:
            w = [WS("w_rqk%d" % li, ret_w_in[slot], D, 4096, 256), WS("w_rvg%d" % li, ret_w_in[slot], D, 8192, 512, col0=4096),
                 WS("w_ro%d" % li, ret_w_out[slot], 4096, D, 256)]
            subs.append((lambda li=li, slot=slot, w=w: ret_layer(li, slot, w[0], w[1], w[2]), w))
        if with_mixer and kind == 1:
            w = [WS("w_c1_%d" % li, conv_w_pw1[slot], D, 2 * D, 256), WS("w_c2_%d" % li, conv_w_pw2[slot], D, D, 256)]
            subs.append((lambda li=li, slot=slot, w=w: conv_layer(li, slot, w[0], w[1]), w))
        if with_mixer and kind == 2:
            w = diff_weights(li, slot)
            subs.append((lambda li=li, slot=slot, w=w: diff_layer(li, slot, w), w))
        if with_ffn:
            w = [WS("w_fin%d" % li, ffn_w_in[li], D, 2 * DFF, 256), WS("w_fdn%d" % li, ffn_w_down[li], DFF, D, 256)]
            subs.append((lambda li=li, w=w: ffn_layer(li, w[0], w[1]), w))
    for w in subs[0][1]:
        w.issue(10 ** 9)
    for i, (run, _) in enumerate(subs):
        nxt = subs[i + 1][1] if i + 1 < len(subs) else []
        tot = sum(len(w.todo) for w in nxt)
        per = -(-tot // NT) if tot else 0

        def hook(t, nxt=nxt, per=per):
            n = per
            for w in nxt:
                k = min(n, len(w.todo))
                w.issue(k)
                n -= k
        tile_hook[0] = hook
        run()
        for w in nxt:
            w.issue(10 ** 9)
    tile_hook[0] = None

    with ExitStack() as tes:
        ob4 = tes.enter_context(nc.sbuf_tensor("ob4", [128, 4, D], F32))
        ob_b = Buf("ob4")
        for t in range(NT):
            load_x_tile(t, xy, xy_b)
            for b in range(4):
                for q in range(4):
                    pb = 1 + (q % 2)
                    for cc in range(4):
                        c = q * 4 + cc
                        tr(psb[pb][:, cc * 128:(cc + 1) * 128], xy[:, c, b * 128:(b + 1) * 128], ident[:, :],
                           reads=(xy_b, consts_b), writes=(psB[pb],), tick=(cc == 3))
                    act(lambda b=b, q=q, pb=pb: nc.scalar.copy(out=ob4[:, b, q * 512:(q + 1) * 512], in_=psb[pb][:, :]),
                        reads=(psB[pb],), writes=(ob_b,))
            cx.dma("pool", out_d[t * T:(t + 1) * T, :].rearrange("(b p) f -> p b f", p=128), ob4[:, :, :],
                   reads=(ob_b,), writes=(), sembuf=ob_b)
        cx.drain("pool", [ob_b])
        cx.drain("sp", [ob_b])
        cx.drain("act", [ob_b])
    es.close()
    return nc


LAYERS = [(i % 3, i // 3, i) for i in range(DEPTH)]


def _ropec():
    o = np.zeros((128, 4), np.float32)
    p = np.arange(128)
    o[:, 0] = (np.float32(ROPE_THETA) ** (-(2 * p).astype(np.float32) / np.float32(256))).astype(np.float32)
    o[:, 1] = (np.float32(ROPE_THETA) ** (-(2 * (p % 64)).astype(np.float32) / np.float32(128))).astype(np.float32)
    return o


def _retc():
    o = np.zeros((128, 8 * 128 * 2 + 8), np.float32)
    idx = np.arange(128, dtype=np.float64)
    for h in range(8):
        lg = np.log1p(-np.exp2(-5.0 - h))
        rel = idx[None, :] - idx[:, None]
        o[:, h * 128:(h + 1) * 128] = np.where(rel >= 0, np.exp(np.maximum(rel, 0) * lg), 0.0) / 16.0
        o[:, 1024 + h * 128:1024 + (h + 1) * 128] = np.exp((idx + 1.0) * lg)[None, :]
        o[:, 2048 + h] = np.exp((127.0 - idx) * lg) / 16.0
    return o


def make_in_map(b, S, x, c, positions, mod_w, mod_b, norm_g, ret_w_in, ret_w_out, conv_w_pw1, conv_b_pw1,
                conv_w_dw, conv_b_dw, conv_ln_g, conv_ln_b, conv_w_pw2, conv_b_pw2, diff_w_in, diff_lambda,
                diff_subln_g, diff_w_out, ffn_w_in, ffn_w_dw, ffn_w_down):
    f = lambda a: np.ascontiguousarray(np.asarray(a, dtype=np.float32))
    return {
        "x": f(x[b, :S]),
        "c": f(c[b]).reshape(DC, 128),
        "positions": np.ascontiguousarray(np.asarray(positions[b, :S], dtype=np.int32)).reshape(1, S),
        "mod_w": f(mod_w), "mod_b": f(mod_b).reshape(DEPTH * 2, 3 * D),
        "norm_g": f(norm_g).reshape(DEPTH * 4 * DC, 128),
        "ret_w_in": f(ret_w_in), "ret_w_out": f(ret_w_out),
        "conv_w_pw1": f(conv_w_pw1), "conv_b_pw1": f(conv_b_pw1).reshape(32, 128),
        "conv_w_dw": f(conv_w_dw).reshape(CONV_W * DC, 128),
        "conv_vecs": np.concatenate([f(conv_b_dw).reshape(DC, 128), f(conv_ln_g).reshape(DC, 128),
                                     f(conv_ln_b).reshape(DC, 128), f(conv_b_pw2).reshape(DC, 128)], axis=0),
        "conv_w_pw2": f(conv_w_pw2),
        "diff_w_in": f(diff_w_in), "diff_lambda": f(diff_lambda).reshape(1, 512),
        "diff_subln_g": f(diff_subln_g).reshape(1, 256), "diff_w_out": f(diff_w_out),
        "ffn_w_in": f(ffn_w_in), "ffn_w_dw": f(ffn_w_dw).reshape(DEPTH * 3 * FC, 128),
        "ffn_w_down": f(ffn_w_down),
        "identc": np.eye(128, dtype=np.float32),
        "ropec": _ropec(), "retc": _retc(),
        "tric": np.triu(np.ones((128, 128), np.float32)),
    }


def kernel(**inputs):
    S = 4096
    B = 8
    nc = build(S, LAYERS)
    in_maps = [make_in_map(b, S, **inputs) for b in range(B)]
    res = run_bass_kernel_spmd(nc, in_maps, core_ids=list(range(B)))
    return np.stack([np.asarray(r["out"], dtype=np.float32) for r in res.results], axis=0)
```

```python
import math
from contextlib import ExitStack

import numpy as np
import concourse.bass as bass
import concourse.mybir as mybir
from concourse.bass_utils import run_bass_kernel_spmd

F32 = mybir.dt.float32
BF16 = mybir.dt.bfloat16
I32 = mybir.dt.int32
AF = mybir.ActivationFunctionType
ALU = mybir.AluOpType
AX = mybir.AxisListType

D = 2048
DC = 16
T = 512
DFF = 5632
FC = 44
EPS = 1e-6
DEPTH = 4
RET_H, RET_DK, RET_DV = 8, 256, 512
DIFF_H, DIFF_DH = 8, 128
CONV_W = 31
ROPE_THETA = 10000.0


class Buf:
    __slots__ = ("name", "w", "r", "sem", "semv")

    def __init__(self, name):
        self.name = name
        self.w = None
        self.r = []
        self.sem = None
        self.semv = 0


class Ctx:
    def __init__(self, nc):
        self.nc = nc
        self.es = ExitStack()
        self.eng = {"pe": nc.tensor, "act": nc.scalar, "dve": nc.vector, "pool": nc.gpsimd, "sp": nc.sync}
        self.cnt = {}
        for n in ("pe", "act", "dve", "pool"):
            self.cnt[n] = [self.es.enter_context(nc.semaphore("c_" + n)), 0]
        self.waited = {}
        self.nsem = 4

    def sb(self, name, shape, dt):
        return self.es.enter_context(self.nc.sbuf_tensor(name, shape, dt))

    def psum(self, name, shape, dt):
        return self.es.enter_context(self.nc.psum_tensor(name, shape, dt))

    def dram(self, name, shape, dt):
        return self.nc.dram_tensor(name, shape, dt, kind="Internal").ap()

    def wait(self, cons, t):
        if t is None:
            return
        sem, val = t
        key = (cons, id(sem))
        if self.waited.get(key, 0) >= val:
            return
        if cons == "pe" and sem is self.cnt["pe"][0]:
            return
        self.eng[cons].wait_ge(sem, val)
        self.waited[key] = val

    def _deps(self, cons, reads, writes):
        for b in reads:
            self.wait(cons, b.w)
        for b in writes:
            self.wait(cons, b.w)
            for t in b.r:
                self.wait(cons, t)

    @staticmethod
    def _addr(b, t):
        for i, (s, v) in enumerate(b.r):
            if s is t[0]:
                if v < t[1]:
                    b.r[i] = t
                return
        b.r.append(t)

    def _upd(self, t, reads, writes):
        for b in reads:
            self._addr(b, t)
        for b in writes:
            b.w = t
            b.r = []

    def op(self, e, fn, reads=(), writes=(), tick=True):
        self._deps(e, reads, writes)
        inst = fn()
        c = self.cnt[e]
        if tick:
            c[1] += 1
            inst.then_inc(c[0], 1)
            t = (c[0], c[1])
        else:
            t = (c[0], c[1] + 1)
        self._upd(t, reads, writes)
        return t

    def dma(self, q, out, in_, reads=(), writes=(), sembuf=None, **kw):
        self._deps(q, reads, writes)
        b = sembuf if sembuf is not None else (writes[0] if writes else reads[0])
        if b.sem is None:
            b.sem = self.es.enter_context(self.nc.semaphore("d%d" % self.nsem))
            self.nsem += 1
        b.semv += 16
        self.eng[q].dma_start(out=out, in_=in_, **kw).then_inc(b.sem, 16)
        t = (b.sem, b.semv)
        self._upd(t, reads, writes)
        return t

    def drain(self, e, bufs):
        for b in bufs:
            self.wait(e, b.w)
            for t in b.r:
                self.wait(e, t)


def build(S, layer_kinds, with_mixer=True, with_ffn=True):
    NT = S // T
    nc = bass.Bass("TRN2", target_bir_lowering=False)
    cx = Ctx(nc)
    es = cx.es

    def din(name, shape, dt=F32):
        return nc.dram_tensor(name, shape, dt, kind="ExternalInput").ap()

    x_in = din("x", [S, D])
    c_in = din("c", [DC, 128])
    pos_in = din("positions", [1, S], I32)
    mod_w = din("mod_w", [DEPTH, 2, D, 3 * D])
    mod_b = din("mod_b", [DEPTH * 2, 3 * D])
    norm_g = din("norm_g", [DEPTH * 4 * DC, 128])
    ret_w_in = din("ret_w_in", [2, D, 12288])
    ret_w_out = din("ret_w_out", [2, 4096, D])
    conv_w_pw1 = din("conv_w_pw1", [1, D, 2 * D])
    conv_b_pw1 = din("conv_b_pw1", [32, 128])
    conv_w_dw = din("conv_w_dw", [CONV_W * DC, 128])
    conv_vecs = din("conv_vecs", [4 * DC, 128])
    conv_w_pw2 = din("conv_w_pw2", [1, D, D])
    diff_w_in = din("diff_w_in", [1, D, 3 * D])
    diff_lambda = din("diff_lambda", [1, 4 * 128])
    diff_subln_g = din("diff_subln_g", [1, 256])
    diff_w_out = din("diff_w_out", [1, D, D])
    ffn_w_in = din("ffn_w_in", [DEPTH, D, 2 * DFF])
    ffn_w_dw = din("ffn_w_dw", [DEPTH * 3 * FC, 128])
    ffn_w_down = din("ffn_w_down", [DEPTH, DFF, D])
    ident_in = din("identc", [128, 128])
    ropec_in = din("ropec", [128, 4])
    retc_in = din("retc", [128, 8 * 128 * 2 + 8])
    out_d = nc.dram_tensor("out", [S, D], F32, kind="ExternalOutput").ap()
    kinds = [k for (k, _, _) in layer_kinds] if with_mixer else []
    tabs = {}
    tabs_b = Buf("tabs")
    if 0 in kinds:
        tabs["r"] = (cx.dram("cosR", [128, S], F32), cx.dram("sinR", [128, S], F32))
    if 2 in kinds:
        tabs["d"] = (cx.dram("cosD", [128, S], F32), cx.dram("sinD", [128, S], F32))

    xT = cx.dram("xT", [D, S], F32)
    xT_b = Buf("xT")
    modrow_d = cx.dram("modrow", [DEPTH * 2, 3 * D], F32)
    modrow_b = Buf("modrow")

    ident = cx.sb("ident", [128, 128], F32)
    ident_bf = cx.sb("ident_bf", [128, 128], BF16)
    ones_bf = cx.sb("ones_bf", [128, 128], BF16)
    cT = cx.sb("cT", [128, DC], F32)
    gT = cx.sb("gT", [128, DEPTH * 4 * DC], F32)
    modT = cx.sb("modT", [128, DEPTH * 2 * 48], F32)
    AT = cx.sb("AT", [128, DEPTH * 2 * DC], F32)
    GT = cx.sb("GT", [128, DEPTH * 2 * DC], F32)
    fdwT = cx.sb("fdwT", [128, DEPTH * 3 * FC], F32)
    epsb = cx.sb("epsb", [128, 1], F32)
    consts_b = Buf("consts")

    psb = [cx.psum("ps%d" % i, [128, 512], F32) for i in range(7)]
    psB = [Buf("ps%d" % i) for i in range(7)]
    pst = cx.psum("pst", [128, 1024], BF16)
    pst_b = [Buf("pst0"), Buf("pst1")]

    xy = cx.sb("xy", [128, DC, T], F32)
    xy_b = Buf("xy")
    hbuf0 = cx.sb("hbuf0", [128, DC, T], BF16)
    hbuf0_b = Buf("h0")
    nxc = [cx.sb("nxc%d" % i, [128, T], F32) for i in range(4)]
    nxc_b = [Buf("nxc%d" % i) for i in range(4)]
    rsq = [cx.sb("rsq%d" % i, [128, T], BF16) for i in range(2)]
    rsq_b = [Buf("rsq%d" % i) for i in range(2)]
    rtmp = [cx.sb("rtmp%d" % i, [128, T], F32) for i in range(4)]
    rtmp_b = [Buf("rtmp%d" % i) for i in range(4)]
    xT_w = []
    rstd2 = cx.sb("rstd2", [128, T], F32)
    rstd2_b = Buf("rstd2")
    ntmp = [cx.sb("ntmp%d" % i, [128, T], F32) for i in range(2)]
    ntmp_b = [Buf("ntmp%d" % i) for i in range(2)]
    sq = [cx.sb("sq%d" % i, [128, T], BF16) for i in range(2)]
    sq_b = [Buf("sq%d" % i) for i in range(2)]
    tmpf = [cx.sb("tmpf%d" % i, [128, T], F32) for i in range(2)]
    tmpf_b = [Buf("tmpf%d" % i) for i in range(2)]
    rstd = cx.sb("rstd", [128, T], F32)
    rstd_b = Buf("rstd")

    def act(fn, reads=(), writes=()):
        return cx.op("act", fn, reads, writes)

    def dve(fn, reads=(), writes=()):
        return cx.op("dve", fn, reads, writes)

    def pool(fn, reads=(), writes=()):
        return cx.op("pool", fn, reads, writes)

    def mm(out_ap, lhsT, rhs, start, stop, reads, writes, tick=None):
        return cx.op("pe", lambda: nc.tensor.matmul(out_ap, lhsT, rhs, start=start, stop=stop),
                     reads, writes, tick=(stop if tick is None else tick))

    def tr(out_ap, in_ap, idn, reads, writes, tick=True):
        return cx.op("pe", lambda: nc.tensor.transpose(out_ap, in_ap, idn), reads, writes, tick=tick)

    class WS:
        def __init__(self, name, src, K, N, SW, col0=0):
            self.KC = K // 128
            self.NS = N // SW
            self.SW = SW
            self.t = cx.dram(name, [128, self.NS, self.KC, SW], BF16)
            self.b = Buf(name)
            self.b.sem = es.enter_context(nc.semaphore("w_" + name))
            self.todo = []
            for k in range(self.KC):
                self.todo.append((self.t[:, :, k, :],
                                  src[k * 128:(k + 1) * 128, col0:col0 + N].rearrange("p (s w) -> p s w", w=SW)))
            self.b.w = (self.b.sem, 16 * len(self.todo))

        def issue(self, n):
            for _ in range(min(n, len(self.todo))):
                o, i = self.todo.pop(0)
                nc.gpsimd.dma_start(out=o, in_=i).then_inc(self.b.sem, 16)

        def slab(self, s):
            return self.t[:, s, :, :]

    tile_hook = [None]

    stg = cx.sb("stg", [128, 128], F32)
    stg_b = Buf("stg")

    def load_T(dst_ap, src_rows, R, src_b=None):
        cx.dma("sp", stg[0:R, :], src_rows, reads=(() if src_b is None else (src_b,)), writes=(stg_b,), sembuf=stg_b)
        tr(psb[0][:, 0:R], stg[0:R, :], ident[0:R, 0:R], reads=(stg_b, consts_b), writes=(psB[0],))
        dve(lambda: nc.vector.tensor_copy(out=dst_ap, in_=psb[0][:, 0:R]), reads=(psB[0],), writes=(consts_b,))

    cx.dma("sp", ident[:, :], ident_in[:, :], writes=(consts_b,))
    dve(lambda: nc.vector.tensor_copy(out=ident_bf[:, :], in_=ident[:, :]), reads=(consts_b,), writes=(consts_b,))
    dve(lambda: nc.vector.memset(ones_bf[:, :], 1.0), writes=(consts_b,))
    dve(lambda: nc.vector.memset(epsb[:, :], EPS), writes=(consts_b,))
    load_T(cT[:, :], c_in[:, :], DC)
    act(lambda: nc.scalar.activation(out=cT[:, :], in_=cT[:, :], func=AF.Silu), reads=(consts_b,), writes=(consts_b,))
    for i in range(0, DEPTH * 4 * DC, 128):
        load_T(gT[:, i:i + 128], norm_g[i:i + 128, :], 128)
    nf = DEPTH * 3 * FC
    for i in range(0, nf, 128):
        r = min(128, nf - i)
        load_T(fdwT[:, i:i + r], ffn_w_dw[i:i + r, :], r)

    with ExitStack() as mes:
        mwa = [mes.enter_context(nc.sbuf_tensor("mw%d" % i, [128, DC, 256], F32)) for i in range(2)]
        mw_b = [Buf("mw%d" % i) for i in range(2)]
        mrow = mes.enter_context(nc.sbuf_tensor("mrow", [1, 3 * D], F32))
        mrow_b = Buf("mrow")
        mbias = mes.enter_context(nc.sbuf_tensor("mbias", [1, 3 * D], F32))
        mbias_b = Buf("mbias")
        it = 0
        for (kind, slot, li) in layer_kinds:
            for sub in range(2):
                if (sub == 0 and not with_mixer) or (sub == 1 and not with_ffn):
                    continue
                ls = li * 2 + sub
                cx.dma("sp", mbias[:, :], mod_b[ls:ls + 1, :], writes=(mbias_b,))
                for n in range(24):
                    w = mwa[it % 2]
                    wb = mw_b[it % 2]
                    cx.dma("sp", w[:, :, :],
                           mod_w[li, sub, :, n * 256:(n + 1) * 256].rearrange("(k p) n -> p k n", p=128),
                           writes=(wb,))
                    pb = 1 + (it % 2)
                    for k in range(DC):
                        mm(psb[pb][0:1, 0:256], cT[:, k:k + 1], w[:, k, :], k == 0, k == DC - 1,
                           reads=(wb, consts_b), writes=(psB[pb],))
                    dve(lambda pb=pb, n=n: nc.vector.tensor_tensor(out=mrow[:, n * 256:(n + 1) * 256], in0=psb[pb][0:1, 0:256],
                                                                   in1=mbias[:, n * 256:(n + 1) * 256], op=ALU.add),
                        reads=(psB[pb], mbias_b), writes=(mrow_b,))
                    it += 1
                cx.dma("pool", modrow_d[ls:ls + 1, :], mrow[:, :], reads=(mrow_b,), writes=(modrow_b,), sembuf=mrow_b)
                load_T(modT[:, ls * 48:(ls + 1) * 48], modrow_d[ls, :].rearrange("(r p) -> r p", p=128), 48, modrow_b)
                sc = modT[:, ls * 48 + 16: ls * 48 + 32]
                gt = modT[:, ls * 48 + 32: ls * 48 + 48]
                gpre = gT[:, (li * 4 + 2 * sub) * DC:(li * 4 + 2 * sub + 1) * DC]
                gpost = gT[:, (li * 4 + 2 * sub + 1) * DC:(li * 4 + 2 * sub + 2) * DC]
                dve(lambda sc=sc, gpre=gpre, ls=ls: nc.vector.scalar_tensor_tensor(
                    out=AT[:, ls * DC:(ls + 1) * DC], in0=sc, scalar=1.0, in1=gpre, op0=ALU.add, op1=ALU.mult),
                    reads=(consts_b,), writes=(consts_b,))
                dve(lambda gt=gt, gpost=gpost, ls=ls: nc.vector.tensor_tensor(
                    out=GT[:, ls * DC:(ls + 1) * DC], in0=gt, in1=gpost, op=ALU.mult),
                    reads=(consts_b,), writes=(consts_b,))
        cx.drain("pe", mw_b)
        cx.drain("dve", [mrow_b, mbias_b] + mw_b)
        cx.drain("pool", [mrow_b])
        cx.drain("sp", [mbias_b] + mw_b)

    def wait_xT(q):
        last = {}
        for (sem, v) in xT_w:
            if id(sem) not in last or last[id(sem)][1] < v:
                last[id(sem)] = (sem, v)
        xT_w[:] = list(last.values())
        for t_ in xT_w:
            cx.wait(q, t_)

    def load_x_tile(t, dst, dst_b):
        wait_xT("sp")
        cx.dma("sp", dst[:, :, :], xT[:, t * T:(t + 1) * T].rearrange("(c p) n -> p c n", p=128),
               reads=(xT_b,), writes=(dst_b,))

    NL = 4

    def norm_tile(t, ls, dst=None):
        if tile_hook[0] is not None:
            tile_hook[0](t)
        hbuf, h_b = dst if dst is not None else (hbuf0, hbuf0_b)
        wait_xT("act")

        def issue(c):
            i = c % NL
            cx.dma("act", nxc[i][:, :], xT[c * 128:(c + 1) * 128, t * T:(t + 1) * T], reads=(xT_b,), writes=(nxc_b[i],))

        for c in range(NL):
            issue(c)
        for c in range(DC):
            i = c % 2
            n_ = c % NL
            act(lambda c=c, i=i, n_=n_: nc.scalar.activation(out=sq[i][:, :], in_=nxc[n_][:, :], func=AF.Square),
                reads=(nxc_b[n_],), writes=(sq_b[i],))
            mm(psb[0][:, :], ones_bf[:, :], sq[i][:, :], c == 0, c == DC - 1, reads=(sq_b[i], consts_b), writes=(psB[0],),
               tick=True)
            if c + NL < DC:
                issue(c + NL)
        for c in range(NL):
            issue(c)
        act(lambda: nc.scalar.activation(out=rstd[:, :], in_=psb[0][:, :], func=AF.Sqrt, bias=epsb[:, 0:1], scale=1.0 / D),
            reads=(psB[0], consts_b), writes=(rstd_b,))
        dve(lambda: nc.vector.reciprocal(out=rstd[:, :], in_=rstd[:, :]), reads=(rstd_b,), writes=(rstd_b,))
        for c in range(DC):
            i = c % 2
            n_ = c % NL
            dve(lambda c=c, i=i, n_=n_: nc.vector.scalar_tensor_tensor(
                out=ntmp[i][:, :], in0=nxc[n_][:, :], scalar=AT[:, ls * DC + c: ls * DC + c + 1], in1=rstd[:, :],
                op0=ALU.mult, op1=ALU.mult), reads=(nxc_b[n_], rstd_b, consts_b), writes=(ntmp_b[i],))
            act(lambda c=c, i=i, hbuf=hbuf: nc.scalar.activation(
                out=hbuf[:, c, :], in_=ntmp[i][:, :], func=AF.Identity,
                bias=modT[:, ls * 48 + c: ls * 48 + c + 1], scale=1.0), reads=(ntmp_b[i], consts_b), writes=(h_b,))
            if c + NL < DC:
                issue(c + NL)

    def resid_tile(t, ls):
        for c in range(DC):
            i = c % 2
            act(lambda c=c, i=i: nc.scalar.activation(out=rsq[i][:, :], in_=xy[:, c, :], func=AF.Square),
                reads=(xy_b,), writes=(rsq_b[i],))
            mm(psb[0][:, :], ones_bf[:, :], rsq[i][:, :], c == 0, c == DC - 1, reads=(rsq_b[i], consts_b), writes=(psB[0],),
               tick=True)
        act(lambda: nc.scalar.activation(out=rstd2[:, :], in_=psb[0][:, :], func=AF.Sqrt, bias=epsb[:, 0:1], scale=1.0 / D),
            reads=(psB[0], consts_b), writes=(rstd2_b,))
        dve(lambda: nc.vector.reciprocal(out=rstd2[:, :], in_=rstd2[:, :]), reads=(rstd2_b,), writes=(rstd2_b,))
        for c in range(DC):
            i = c % 4
            dve(lambda c=c, i=i: nc.vector.scalar_tensor_tensor(
                out=rtmp[i][:, :], in0=xy[:, c, :], scalar=GT[:, ls * DC + c: ls * DC + c + 1], in1=rstd2[:, :],
                op0=ALU.mult, op1=ALU.mult), reads=(xy_b, rstd2_b, consts_b), writes=(rtmp_b[i],))
            cx.dma("pool", xT[c * 128:(c + 1) * 128, t * T:(t + 1) * T], rtmp[i][:, :],
                   reads=(rtmp_b[i],), writes=(), sembuf=rtmp_b[i], accum_op=ALU.add)
            xT_w.append((rtmp_b[i].sem, rtmp_b[i].semv))

    with ExitStack() as tes:
        xin4 = tes.enter_context(nc.sbuf_tensor("xin4", [128, 4, D], F32))
        xin_b = Buf("xin4")
        for t in range(NT):
            cx.dma("sp", xin4[:, :, :], x_in[t * T:(t + 1) * T, :].rearrange("(b p) f -> p b f", p=128), writes=(xin_b,))
            for c in range(DC):
                pb = 1 + (c % 2)
                for b in range(4):
                    tr(psb[pb][:, b * 128:(b + 1) * 128], xin4[:, b, c * 128:(c + 1) * 128], ident[:, :],
                       reads=(xin_b, consts_b), writes=(psB[pb],), tick=(b == 3))
                act(lambda c=c, pb=pb: nc.scalar.copy(out=xy[:, c, :], in_=psb[pb][:, :]), reads=(psB[pb],), writes=(xy_b,))
            cx.dma("pool", xT[:, t * T:(t + 1) * T].rearrange("(c p) n -> p c n", p=128), xy[:, :, :],
                   reads=(xy_b,), writes=(xT_b,), sembuf=xy_b)
        for e in ("pe", "sp"):
            cx.drain(e, [xin_b])

    ropec = cx.sb("ropec_sb", [128, 4], F32)
    cx.dma("sp", ropec[:, :], ropec_in[:, :], writes=(consts_b,))
    TWO_PI_HI = 6.28125
    TWO_PI_LO = 2.0 * math.pi - 6.28125
    if tabs:
        with ExitStack() as tes:
            posi = tes.enter_context(nc.sbuf_tensor("posi", [128, T], I32))
            posf = tes.enter_context(nc.sbuf_tensor("posf", [128, T], F32))
            ang = tes.enter_context(nc.sbuf_tensor("ang", [128, T], F32))
            uu = tes.enter_context(nc.sbuf_tensor("uu", [128, T], F32))
            ki = tes.enter_context(nc.sbuf_tensor("ki", [128, T], I32))
            kf = tes.enter_context(nc.sbuf_tensor("kf", [128, T], F32))
            rr = [tes.enter_context(nc.sbuf_tensor("rr%d" % i, [128, T], F32)) for i in range(2)]
            tb_ = {n: Buf(n) for n in ("posi", "posf", "ang", "uu", "ki", "kf", "rr0", "rr1")}
            it = 0
            for t in range(NT):
                cx.dma("sp", posi[:, :], pos_in[0:1, t * T:(t + 1) * T].partition_broadcast(128), writes=(tb_["posi"],))
                dve(lambda: nc.vector.tensor_copy(out=posf[:, :], in_=posi[:, :]), reads=(tb_["posi"],), writes=(tb_["posf"],))
                for key, col in (("r", 0), ("d", 1)):
                    if key not in tabs:
                        continue
                    dve(lambda col=col: nc.vector.tensor_scalar(out=ang[:, :], in0=posf[:, :], scalar1=ropec[:, col:col + 1],
                                                                scalar2=None, op0=ALU.mult),
                        reads=(tb_["posf"], consts_b), writes=(tb_["ang"],))
                    for ph, which in ((math.pi / 2, 0), (0.0, 1)):
                        r = rr[it % 2]
                        rb = tb_["rr%d" % (it % 2)]
                        it += 1
                        dve(lambda ph=ph: nc.vector.tensor_scalar(out=uu[:, :], in0=ang[:, :], scalar1=1.0 / (2 * math.pi),
                                                                  scalar2=ph / (2 * math.pi), op0=ALU.mult, op1=ALU.add),
                            reads=(tb_["ang"],), writes=(tb_["uu"],))
                        dve(lambda: nc.vector.tensor_copy(out=ki[:, :], in_=uu[:, :]), reads=(tb_["uu"],), writes=(tb_["ki"],))
                        dve(lambda: nc.vector.tensor_copy(out=kf[:, :], in_=ki[:, :]), reads=(tb_["ki"],), writes=(tb_["kf"],))
                        dve(lambda r=r: nc.vector.scalar_tensor_tensor(out=r[:, :], in0=kf[:, :], scalar=-TWO_PI_HI, in1=ang[:, :],
                                                                       op0=ALU.mult, op1=ALU.add),
                            reads=(tb_["kf"], tb_["ang"]), writes=(rb,))
                        dve(lambda r=r: nc.vector.scalar_tensor_tensor(out=r[:, :], in0=kf[:, :], scalar=-TWO_PI_LO, in1=r[:, :],
                                                                       op0=ALU.mult, op1=ALU.add),
                            reads=(tb_["kf"], rb), writes=(rb,))
                        dve(lambda r=r, ph=ph: nc.vector.tensor_scalar(out=r[:, :], in0=r[:, :], scalar1=ph, scalar2=math.pi,
                                                                       op0=ALU.add, op1=ALU.min),
                            reads=(rb,), writes=(rb,))
                        dve(lambda r=r: nc.vector.tensor_scalar(out=r[:, :], in0=r[:, :], scalar1=-math.pi, scalar2=None,
                                                                op0=ALU.max), reads=(rb,), writes=(rb,))
                        act(lambda r=r: nc.scalar.activation(out=r[:, :], in_=r[:, :], func=AF.Sin), reads=(rb,), writes=(rb,))
                        cx.dma("pool", tabs[key][which][:, t * T:(t + 1) * T], r[:, :], reads=(rb,), writes=(), sembuf=rb)
            for r_ in ("rr0", "rr1"):
                tabs_b.w = None
            cx.drain("dve", list(tb_.values()))
            cx.drain("pool", list(tb_.values()))
            cx.drain("sp", list(tb_.values()))
            cx.drain("act", list(tb_.values()))
    uid = [0]

    def sbt(st, name, shape, dt):
        uid[0] += 1
        return st.enter_context(nc.sbuf_tensor("%s_%d" % (name, uid[0]), shape, dt))

    def conv_layer(li, slot, w1s, w2s):
        ls = li * 2
        with ExitStack() as fes:
            cb1T = sbt(fes, "cb1T", [128, 32], F32)
            cdwT = sbt(fes, "cdwT", [128, CONV_W * DC], F32)
            cvT = sbt(fes, "cvT", [128, 4 * DC], F32)
            load_T(cb1T[:, :], conv_b_pw1[:, :], 32)
            for i in range(0, CONV_W * DC, 128):
                r = min(128, CONV_W * DC - i)
                load_T(cdwT[:, i:i + r], conv_w_dw[i:i + r, :], r)
            load_T(cvT[:, :], conv_vecs[:, :], 64)
            wa = [sbt(fes, "wa", [128, DC, 256], BF16) for i in range(2)]
            wb = [sbt(fes, "wb", [128, DC, 256], BF16) for i in range(2)]
            wa_b = [Buf("wa") for i in range(2)]
            wb_b = [Buf("wb") for i in range(2)]
            ub = [sbt(fes, "ub", [128, T + 30], BF16) for i in range(2)]
            ub_b = [Buf("ub") for i in range(2)]
            dg = [sbt(fes, "dg", [128, CONV_W, 128], BF16) for i in range(2)]
            dg_b = [Buf("dg") for i in range(2)]
            sgc = [sbt(fes, "sgc", [128, T], F32) for i in range(2)]
            sgc_b = [Buf("sgc") for i in range(2)]
            vbuf = sbt(fes, "vbuf", [128, DC, T], F32)
            v_b = [Buf("v%d" % j) for j in range(DC)]
            chalo = sbt(fes, "chalo", [128, DC, 30], BF16)
            chalo_b = Buf("chalo")
            mu = sbt(fes, "mu", [128, T], F32)
            mu_b = Buf("mu")
            m2 = sbt(fes, "m2", [128, T], F32)
            m2_b = Buf("m2")
            dve(lambda: nc.vector.memset(chalo[:, :, :], 0.0), writes=(chalo_b,))
            hs = sbt(fes, "hs", [128, DC, T], BF16)
            hs_b = Buf("hs")
            pend = [None]
            norm_tile(0, ls)
            hbuf, h_b = hbuf0, hbuf0_b
            for t in range(NT):
                for s2 in range(DC // 2):
                    i3 = s2 % 2
                    cx.dma("sp", wa[i3][:, :, :], w1s.slab(s2), reads=(w1s.b,), writes=(wa_b[i3],))
                    cx.dma("sp", wb[i3][:, :, :], w1s.slab(DC // 2 + s2), reads=(w1s.b,), writes=(wb_b[i3],))
                    for jj in range(2):
                        j = s2 * 2 + jj
                        i2 = j % 2
                        ap_, bp_ = 1 + i2, 3 + i2
                        for k in range(DC):
                            mm(psb[ap_][:, :], wa[i3][:, k, jj * 128:(jj + 1) * 128], hbuf[:, k, :], k == 0, k == DC - 1,
                               reads=(wa_b[i3], h_b), writes=(psB[ap_],))
                        for k in range(DC):
                            mm(psb[bp_][:, :], wb[i3][:, k, jj * 128:(jj + 1) * 128], hbuf[:, k, :], k == 0, k == DC - 1,
                               reads=(wb_b[i3], h_b), writes=(psB[bp_],))
                        u = ub[i2]
                        act(lambda i2=i2, bp_=bp_, j=j: nc.scalar.activation(out=sgc[i2][:, :], in_=psb[bp_][:, :], func=AF.Sigmoid,
                                                                            bias=cb1T[:, DC + j:DC + j + 1], scale=1.0),
                            reads=(psB[bp_], consts_b), writes=(sgc_b[i2],))
                        act(lambda u=u, j=j: nc.scalar.copy(out=u[:, 0:30], in_=chalo[:, j, :]), reads=(chalo_b,), writes=(ub_b[i2],))
                        dve(lambda u=u, i2=i2, ap_=ap_, j=j: nc.vector.scalar_tensor_tensor(
                            out=u[:, 30:T + 30], in0=psb[ap_][:, :], scalar=cb1T[:, j:j + 1], in1=sgc[i2][:, :],
                            op0=ALU.add, op1=ALU.mult), reads=(psB[ap_], sgc_b[i2], consts_b), writes=(ub_b[i2],))
                        for tau in range(CONV_W):
                            pool(lambda i2=i2, j=j, tau=tau: nc.gpsimd.tensor_scalar(
                                out=dg[i2][:, tau, :], in0=ident_bf[:, :], scalar1=cdwT[:, tau * DC + j:tau * DC + j + 1], scalar2=1.0,
                                op0=ALU.mult, op1=ALU.mult), reads=(consts_b,), writes=(dg_b[i2],))
                        dve(lambda u=u, j=j: nc.vector.tensor_copy(out=chalo[:, j, :], in_=u[:, T:T + 30]),
                            reads=(ub_b[i2],), writes=(chalo_b,))

                        def conv_mm(u=u, i2=i2, j=j):
                            cp = 5 + i2
                            for tau in range(CONV_W):
                                mm(psb[cp][:, :], dg[i2][:, tau, :], u[:, tau:tau + T], tau == 0, tau == CONV_W - 1,
                                   reads=(dg_b[i2], ub_b[i2]), writes=(psB[cp],))
                            act(lambda: nc.scalar.activation(out=vbuf[:, j, :], in_=psb[cp][:, :], func=AF.Identity,
                                                             bias=cvT[:, j:j + 1], scale=1.0),
                                reads=(psB[cp], consts_b), writes=(v_b[j],))
                        if pend[0] is not None:
                            pend[0]()
                        pend[0] = conv_mm
                if pend[0] is not None:
                    pend[0]()
                    pend[0] = None
                if t + 1 < NT:
                    norm_tile(t + 1, ls)
                for j in range(DC):
                    i = j % 2
                    act(lambda j=j, i=i: nc.scalar.copy(out=sq[i][:, :], in_=vbuf[:, j, :]), reads=(v_b[j],), writes=(sq_b[i],))
                    mm(psb[0][:, :], ones_bf[:, :], sq[i][:, :], j == 0, j == DC - 1, reads=(sq_b[i], consts_b), writes=(psB[0],),
                       tick=True)
                for j in range(DC):
                    i = j % 2
                    act(lambda j=j, i=i: nc.scalar.activation(out=sq[i][:, :], in_=vbuf[:, j, :], func=AF.Square),
                        reads=(v_b[j],), writes=(sq_b[i],))
                    mm(psb[5][:, :], ones_bf[:, :], sq[i][:, :], j == 0, j == DC - 1, reads=(sq_b[i], consts_b), writes=(psB[5],),
                       tick=True)
                dve(lambda: nc.vector.tensor_scalar(out=mu[:, :], in0=psb[0][:, :], scalar1=1.0 / D, scalar2=None, op0=ALU.mult),
                    reads=(psB[0],), writes=(mu_b,))
                dve(lambda: nc.vector.tensor_tensor(out=m2[:, :], in0=mu[:, :], in1=mu[:, :], op=ALU.mult),
                    reads=(mu_b,), writes=(m2_b,))
                dve(lambda: nc.vector.scalar_tensor_tensor(out=m2[:, :], in0=psb[5][:, :], scalar=1.0 / D, in1=m2[:, :],
                                                           op0=ALU.mult, op1=ALU.subtract), reads=(psB[5], m2_b), writes=(m2_b,))
                act(lambda: nc.scalar.activation(out=rstd[:, :], in_=m2[:, :], func=AF.Sqrt, bias=epsb[:, 0:1], scale=1.0),
                    reads=(m2_b, consts_b), writes=(rstd_b,))
                dve(lambda: nc.vector.reciprocal(out=rstd[:, :], in_=rstd[:, :]), reads=(rstd_b,), writes=(rstd_b,))
                for j in range(DC):
                    i = j % 2
                    dve(lambda j=j, i=i: nc.vector.tensor_tensor(out=tmpf[i][:, :], in0=vbuf[:, j, :], in1=mu[:, :], op=ALU.subtract),
                        reads=(v_b[j], mu_b), writes=(tmpf_b[i],))
                    dve(lambda j=j, i=i: nc.vector.scalar_tensor_tensor(
                        out=tmpf[i][:, :], in0=tmpf[i][:, :], scalar=cvT[:, DC + j:DC + j + 1], in1=rstd[:, :],
                        op0=ALU.mult, op1=ALU.mult), reads=(tmpf_b[i], rstd_b, consts_b), writes=(tmpf_b[i],))
                    act(lambda j=j, i=i: nc.scalar.activation(out=hs[:, j, :], in_=tmpf[i][:, :], func=AF.Silu,
                                                              bias=cvT[:, 2 * DC + j:2 * DC + j + 1], scale=1.0),
                        reads=(tmpf_b[i], consts_b), writes=(hs_b,))
                for s2 in range(DC // 2):
                    i3 = s2 % 2
                    cx.dma("sp", wa[i3][:, :, :], w2s.slab(s2), reads=(w2s.b,), writes=(wa_b[i3],))
                    for mi in range(2):
                        m = s2 * 2 + mi
                        yp = yb4[m % 4]
                        for k in range(DC):
                            mm(psb[yp][:, :], wa[i3][:, k, mi * 128:(mi + 1) * 128], hs[:, k, :], k == 0, k == DC - 1,
                               reads=(wa_b[i3], hs_b), writes=(psB[yp],))
                        act(lambda m=m, yp=yp: nc.scalar.activation(out=xy[:, m, :], in_=psb[yp][:, :], func=AF.Identity,
                                                                    bias=cvT[:, 3 * DC + m:3 * DC + m + 1], scale=1.0),
                            reads=(psB[yp], consts_b), writes=(xy_b,))
                resid_tile(t, ls)
            allb = wa_b + wb_b + ub_b + sgc_b + v_b + dg_b + [chalo_b, mu_b, m2_b, consts_b, hs_b]
            for e in ("pe", "act", "dve", "sp", "pool"):
                cx.drain(e, allb)

    def ret_layer(li, slot, wqk, wvg, wo):
        ls = li * 2
        cosd, sind = tabs["r"]
        with ExitStack() as fes:
            retc = sbt(fes, "retc", [128, 2056], F32)
            cx.dma("sp", retc[:, :], retc_in[:, :], writes=(consts_b,))
            decT = lambda h: retc[:, h * 128:(h + 1) * 128]
            cdb = lambda h: retc[:, 1024 + h * 128:1024 + (h + 1) * 128]
            sdv = lambda h: retc[:, 2048 + h:2048 + h + 1]
            cs = [sbt(fes, "cs", [128, T], F32) for i in range(2)]
            cs_b = Buf("cs")
            wsl = [sbt(fes, "wsl", [128, DC, 512], BF16) for i in range(2)]
            wsl_b = [Buf("wsl") for i in range(2)]
            qr = sbt(fes, "qr", [128, 2, 2, T], BF16)
            kr = sbt(fes, "kr", [128, 2, 2, T], BF16)
            qr_b = [Buf("qr") for i in range(2)]
            kr_b = [Buf("kr") for i in range(2)]
            vsb = sbt(fes, "vsb", [128, 4, 2, 512], BF16)
            gsb = sbt(fes, "gsb", [128, 4, 2, 512], BF16)
            v_b = [[Buf("v") for hh in range(2)] for tb in range(4)]
            g_b = [[Buf("g") for hh in range(2)] for tb in range(4)]
            oT = sbt(fes, "oT", [128, 8, T], BF16)
            oT_b = Buf("oT")
            stf = sbt(fes, "stf", [128, 8, 2, 512], F32)
            stf_b = [Buf("stf") for h in range(8)]
            sbf = [sbt(fes, "sbf", [128, 2, 512], BF16) for i in range(2)]
            sbf_b = [Buf("sbf") for i in range(2)]
            rt = tmpf
            rt_b = tmpf_b
            pt = [sbt(fes, "pt", [128, 128], BF16) for i in range(2)]
            pt_b = [Buf("pt") for i in range(2)]
            ktm = [sbt(fes, "ktm", [128, 256], BF16) for i in range(2)]
            ktm_b = [Buf("ktm") for i in range(2)]
            qs = [sbt(fes, "qs", [128, 2, 128], BF16) for i in range(2)]
            qs_b = [Buf("qs") for i in range(2)]
            on = [sbt(fes, "on", [128, 512], BF16) for i in range(2)]
            on_b = [Buf("on") for i in range(2)]
            st6 = [sbt(fes, "st6", [128, 6], F32) for i in range(2)]
            mv = [sbt(fes, "mv", [128, 4], F32) for i in range(2)]
            mv_b = [Buf("mv") for i in range(2)]
            dve(lambda: nc.vector.memset(stf[:, :, :, :], 0.0), writes=tuple(stf_b))
            sl = [0]

            def next_slab():
                sl[0] += 1
                return sl[0] % 2

            pc = [0]
            norm_tile(0, ls)
            hbuf, h_b = hbuf0, hbuf0_b
            for t in range(NT):
                cx.dma("sp", cs[0][:, :], cosd[:, t * T:(t + 1) * T], reads=(tabs_b,), writes=(cs_b,))
                cx.dma("sp", cs[1][:, :], sind[:, t * T:(t + 1) * T], reads=(tabs_b,), writes=(cs_b,))
                for g in range(4):
                    for hh in range(2):
                        h = 2 * g + hh
                        for which in range(2):
                            si = next_slab()
                            w = wsl[si]
                            cx.dma("sp", w[:, :, 0:256], wqk.slab(which * 8 + h), reads=(wqk.b,), writes=(wsl_b[si],))
                            i2 = pc[0] % 2
                            pc[0] += 1
                            p1, p2 = 1 + i2, 3 + i2
                            for k in range(DC):
                                mm(psb[p1][:, :], w[:, k, 0:128], hbuf[:, k, :], k == 0, k == DC - 1,
                                   reads=(wsl_b[si], h_b), writes=(psB[p1],))
                            for k in range(DC):
                                mm(psb[p2][:, :], w[:, k, 128:256], hbuf[:, k, :], k == 0, k == DC - 1,
                                   reads=(wsl_b[si], h_b), writes=(psB[p2],))
                            c_, s_ = cs[0], cs[1]
                            dst = qr if which == 0 else kr
                            dst_b = (qr_b if which == 0 else kr_b)[hh]
                            for (pa, ca, pb_, cb_, half, op_) in ((p1, c_, p2, s_, 0, ALU.subtract), (p2, c_, p1, s_, 1, ALU.add)):
                                a_, b_ = 0, 1
                                dve(lambda pa=pa, ca=ca, a_=a_: nc.vector.tensor_tensor(out=rt[a_][:, :], in0=psb[pa][:, :],
                                                                                         in1=ca[:, :], op=ALU.mult),
                                    reads=(psB[pa], cs_b), writes=(rt_b[a_],))
                                dve(lambda pb_=pb_, cb_=cb_, b_=b_: nc.vector.tensor_tensor(out=rt[b_][:, :], in0=psb[pb_][:, :],
                                                                                             in1=cb_[:, :], op=ALU.mult),
                                    reads=(psB[pb_], cs_b), writes=(rt_b[b_],))
                                pool(lambda dst=dst, hh=hh, half=half, a_=a_, b_=b_, op_=op_: nc.gpsimd.tensor_tensor(
                                    out=dst[:, hh, half, :], in0=rt[a_][:, :], in1=rt[b_][:, :], op=op_),
                                    reads=(rt_b[a_], rt_b[b_]), writes=(dst_b,))
                    for hh in range(2):
                        h = 2 * g + hh
                        for which in range(2):
                            si = next_slab()
                            w = wsl[si]
                            cx.dma("sp", w[:, :, :], wvg.slab(which * 8 + h), reads=(wvg.b,), writes=(wsl_b[si],))
                            for tb in range(4):
                                i2 = pc[0] % 2
                                pc[0] += 1
                                vp = 5 + i2
                                for k in range(DC):
                                    mm(psb[vp][:, :], hbuf[:, k, tb * 128:(tb + 1) * 128], w[:, k, :], k == 0, k == DC - 1,
                                       reads=(wsl_b[si], h_b), writes=(psB[vp],))
                                if which == 0:
                                    act(lambda tb=tb, hh=hh, vp=vp: nc.scalar.copy(out=vsb[:, tb, hh, :], in_=psb[vp][:, :]),
                                        reads=(psB[vp],), writes=(v_b[tb][hh],))
                                else:
                                    act(lambda tb=tb, hh=hh, vp=vp: nc.scalar.activation(out=gsb[:, tb, hh, :], in_=psb[vp][:, :],
                                                                                        func=AF.Silu),
                                        reads=(psB[vp],), writes=(g_b[tb][hh],))
                    if g == 3 and t + 1 < NT:
                        norm_tile(t + 1, ls)
                    for tb in range(4):
                        tsl = slice(tb * 128, (tb + 1) * 128)
                        for hh in range(2):
                            h = 2 * g + hh
                            gam128 = float(np.exp(128.0 * np.log1p(-np.exp2(-5.0 - h))))
                            for half in range(2):
                                mm(psb[0][:, hh * 128:(hh + 1) * 128], kr[:, hh, half, tsl], qr[:, hh, half, tsl], half == 0, half == 1,
                                   reads=(kr_b[hh], qr_b[hh]), writes=(psB[0],))
                            dve(lambda hh=hh, h=h: nc.vector.tensor_tensor(out=pt[hh][:, :], in0=psb[0][:, hh * 128:(hh + 1) * 128],
                                                                          in1=decT(h), op=ALU.mult),
                                reads=(psB[0], consts_b), writes=(pt_b[hh],))
                            for half in range(2):
                                tr(pst[:, hh * 512 + half * 128: hh * 512 + (half + 1) * 128], kr[:, hh, half, tsl], ident_bf[:, :],
                                   reads=(kr_b[hh], consts_b), writes=(pst_b[hh],), tick=(half == 1))
                            dve(lambda hh=hh, h=h: nc.vector.tensor_scalar(out=ktm[hh][:, :], in0=pst[:, hh * 512: hh * 512 + 256],
                                                                          scalar1=sdv(h), scalar2=None, op0=ALU.mult),
                                reads=(pst_b[hh], consts_b), writes=(ktm_b[hh],))
                            for half in range(2):
                                pool(lambda hh=hh, h=h, half=half, tsl=tsl: nc.gpsimd.tensor_tensor(
                                    out=qs[hh][:, half, :], in0=qr[:, hh, half, tsl], in1=cdb(h), op=ALU.mult),
                                    reads=(qr_b[hh], consts_b), writes=(qs_b[hh],))
                            act(lambda hh=hh, h=h: nc.scalar.copy(out=sbf[hh][:, :, :], in_=stf[:, h, :, :]),
                                reads=(stf_b[h],), writes=(sbf_b[hh],))
                            op_ = 1 + hh
                            mm(psb[op_][:, :], pt[hh][:, :], vsb[:, tb, hh, :], True, False,
                               reads=(pt_b[hh], v_b[tb][hh]), writes=(psB[op_],))
                            mm(psb[op_][:, :], qs[hh][:, 0, :], sbf[hh][:, 0, :], False, False,
                               reads=(qs_b[hh], sbf_b[hh]), writes=(psB[op_],))
                            mm(psb[op_][:, :], qs[hh][:, 1, :], sbf[hh][:, 1, :], False, True,
                               reads=(qs_b[hh], sbf_b[hh]), writes=(psB[op_],))
                            for half in range(2):
                                nsb = 3 + 2 * hh + half
                                mm(psb[nsb][:, :], ktm[hh][:, half * 128:(half + 1) * 128], vsb[:, tb, hh, :], True, True,
                                   reads=(ktm_b[hh], v_b[tb][hh]), writes=(psB[nsb],))
                                dve(lambda h=h, half=half, nsb=nsb, gam128=gam128: nc.vector.scalar_tensor_tensor(
                                    out=stf[:, h, half, :], in0=stf[:, h, half, :], scalar=gam128, in1=psb[nsb][:, :],
                                    op0=ALU.mult, op1=ALU.add), reads=(psB[nsb], stf_b[h]), writes=(stf_b[h],))
                            dve(lambda hh=hh, op_=op_: nc.vector.bn_stats(out=st6[hh][:, :], in_=psb[op_][:, :]),
                                reads=(psB[op_],), writes=(mv_b[hh],))
                            dve(lambda hh=hh: nc.vector.bn_aggr(out=mv[hh][:, 0:2], in_=st6[hh][:, :]), reads=(mv_b[hh],), writes=(mv_b[hh],))
                            act(lambda hh=hh: nc.scalar.activation(out=mv[hh][:, 2:3], in_=mv[hh][:, 1:2], func=AF.Sqrt,
                                                                   bias=epsb[:, 0:1], scale=1.0),
                                reads=(mv_b[hh], consts_b), writes=(mv_b[hh],))
                            dve(lambda hh=hh: nc.vector.reciprocal(out=mv[hh][:, 2:3], in_=mv[hh][:, 2:3]), reads=(mv_b[hh],), writes=(mv_b[hh],))
                            dve(lambda hh=hh: nc.vector.scalar_tensor_tensor(out=mv[hh][:, 3:4], in0=mv[hh][:, 0:1], scalar=-1.0,
                                                                             in1=mv[hh][:, 2:3], op0=ALU.mult, op1=ALU.mult),
                                reads=(mv_b[hh],), writes=(mv_b[hh],))
                            act(lambda hh=hh, op_=op_: nc.scalar.activation(out=on[hh][:, :], in_=psb[op_][:, :], func=AF.Identity,
                                                                            bias=mv[hh][:, 3:4], scale=mv[hh][:, 2:3]),
                                reads=(psB[op_], mv_b[hh]), writes=(on_b[hh],))
                            pool(lambda hh=hh, tb=tb: nc.gpsimd.tensor_tensor(out=gsb[:, tb, hh, :], in0=on[hh][:, :],
                                                                              in1=gsb[:, tb, hh, :], op=ALU.mult),
                                 reads=(on_b[hh], g_b[tb][hh]), writes=(g_b[tb][hh],))
                    for tb in range(4):
                        for hh in range(2):
                            for q4 in range(4):
                                jc = hh * 4 + q4
                                tr(pst[:, jc * 128:(jc + 1) * 128], gsb[:, tb, hh, q4 * 128:(q4 + 1) * 128], ident_bf[:, :],
                                   reads=(g_b[tb][hh], consts_b), writes=(pst_b[hh],), tick=(q4 == 3))
                        act(lambda tb=tb: nc.scalar.copy(out=oT[:, :, tb * 128:(tb + 1) * 128],
                                                         in_=pst[:, :].rearrange("p (j n) -> p j n", j=8)),
                            reads=(pst_b[0], pst_b[1]), writes=(oT_b,))
                    for s2 in range(DC // 2):
                        si = next_slab()
                        w = wsl[si]
                        cx.dma("sp", w[:, 0:8, 0:256], wo.t[:, s2, 8 * g:8 * g + 8, :], reads=(wo.b,), writes=(wsl_b[si],))
                        for mi in range(2):
                            m = s2 * 2 + mi
                            yp = 5 + (m % 2)
                            for jj in range(8):
                                mm(psb[yp][:, :], w[:, jj, mi * 128:(mi + 1) * 128], oT[:, jj, :], jj == 0, jj == 7,
                                   reads=(wsl_b[si], oT_b), writes=(psB[yp],))
                            if g == 0:
                                act(lambda m=m, yp=yp: nc.scalar.copy(out=xy[:, m, :], in_=psb[yp][:, :]),
                                    reads=(psB[yp],), writes=(xy_b,))
                            else:
                                dve(lambda m=m, yp=yp: nc.vector.tensor_tensor(out=xy[:, m, :], in0=xy[:, m, :], in1=psb[yp][:, :],
                                                                               op=ALU.add), reads=(psB[yp], xy_b), writes=(xy_b,))
                resid_tile(t, ls)
            allb = (wsl_b + qr_b + kr_b + [b for r in v_b for b in r] + [b for r in g_b for b in r] + [oT_b, cs_b, consts_b]
                    + stf_b + sbf_b + rt_b + pt_b + ktm_b + qs_b + on_b + mv_b)
            for e in ("pe", "act", "dve", "sp", "pool"):
                cx.drain(e, allb)

    yb4 = [5, 6, 1, 2]

    tric_in = din("tric", [128, 128])

    class WSperm:
        def __init__(self, name, src):
            self.t = cx.dram(name, [128, 16, DC, 256], BF16)
            self.b = Buf(name)
            self.b.sem = es.enter_context(nc.semaphore("w_" + name))
            self.todo = []
            for k in range(DC):
                sv = src[k * 128:(k + 1) * 128, 0:4096].rearrange("p (s c hf f) -> p s c hf f", s=16, c=2, hf=2, f=64)
                for hf in range(2):
                    self.todo.append((self.t[:, :, k, hf * 128:(hf + 1) * 128].rearrange("p s (c f) -> p s c f", c=2),
                                      sv[:, :, :, hf, :]))
            self.b.w = (self.b.sem, 16 * len(self.todo))

        def issue(self, n):
            for _ in range(min(n, len(self.todo))):
                o, i = self.todo.pop(0)
                nc.gpsimd.dma_start(out=o, in_=i).then_inc(self.b.sem, 16)

        def slab(self, s):
            return self.t[:, s, :, :]

    def diff_weights(li, slot):
        return [WSperm("w_dqk%d" % li, diff_w_in[slot]), WS("w_dv%d" % li, diff_w_in[slot], D, 2048, 512, col0=4096),
                WS("w_do%d" % li, diff_w_out[slot], D, D, 256)]

    def diff_layer(li, slot, wts):
        wqk, wv, wo = wts
        ls = li * 2
        cosd, sind = tabs["d"]
        NBK = S // 128
        lam_init = 0.8 - 0.6 * math.exp(-0.3 * li)
        qT_d = cx.dram("dq%d" % li, [8, 2, 128, S], BF16)
        kT_d = cx.dram("dk%d" % li, [8, 2, 128, S], BF16)
        V_d = cx.dram("dv%d" % li, [S, 8, 256], BF16)
        oT_d = cx.dram("do%d" % li, [D, S], BF16)
        scr_b = Buf("dscr")
        with ExitStack() as fes:
            cs = [sbt(fes, "cs", [128, T], F32) for i in range(2)]
            cs_b = Buf("cs")
            wsl = [sbt(fes, "wsl", [128, DC, 512], BF16) for i in range(2)]
            wsl_b = [Buf("wsl") for i in range(2)]
            rt = [sbt(fes, "rt", [128, T], F32) for i in range(4)]
            rt_b = [Buf("rt") for i in range(4)]
            ro = [sbt(fes, "ro", [128, T], BF16) for i in range(4)]
            ro_b = [Buf("ro") for i in range(4)]
            vst = [sbt(fes, "vst", [128, 512], BF16) for i in range(2)]
            vst_b = [Buf("vst") for i in range(2)]
            sl = 0
            pc = 0
            rc = 0
            hA = [(hbuf0, hbuf0_b), (sbt(fes, "hb1", [128, DC, T], BF16), Buf("hb1"))]
            norm_tile(0, ls, hA[0])
            for t in range(NT):
                hbuf, h_b = hA[t % 2]
                cx.dma("sp", cs[0][:, :], cosd[:, t * T:(t + 1) * T], writes=(cs_b,))
                cx.dma("sp", cs[1][:, :], sind[:, t * T:(t + 1) * T], writes=(cs_b,))
                for h in range(8):
                    if h == 4 and t + 1 < NT:
                        norm_tile(t + 1, ls, hA[(t + 1) % 2])
                    for which in range(2):
                        sl += 1
                        si = sl % 2
                        w = wsl[si]
                        cx.dma("sp", w[:, :, 0:256], wqk.slab(which * 8 + h), reads=(wqk.b,), writes=(wsl_b[si],))
                        i2 = pc % 2
                        pc += 1
                        p1, p2 = 1 + i2, 3 + i2
                        for k in range(DC):
                            mm(psb[p1][:, :], w[:, k, 0:128], hbuf[:, k, :], k == 0, k == DC - 1, reads=(wsl_b[si], h_b), writes=(psB[p1],))
                        for k in range(DC):
                            mm(psb[p2][:, :], w[:, k, 128:256], hbuf[:, k, :], k == 0, k == DC - 1, reads=(wsl_b[si], h_b), writes=(psB[p2],))
                        dstd = qT_d if which == 0 else kT_d
                        for (pa, pb_, half, op_) in ((p1, p2, 0, ALU.subtract), (p2, p1, 1, ALU.add)):
                            a_, b_ = (0, 1) if half == 0 else (2, 3)
                            oi = rc % 4
                            rc += 1
                            dve(lambda pa=pa, a_=a_: nc.vector.tensor_tensor(out=rt[a_][:, :], in0=psb[pa][:, :], in1=cs[0][:, :], op=ALU.mult),
                                reads=(psB[pa], cs_b), writes=(rt_b[a_],))
                            dve(lambda pb_=pb_, b_=b_: nc.vector.tensor_tensor(out=rt[b_][:, :], in0=psb[pb_][:, :], in1=cs[1][:, :], op=ALU.mult),
                                reads=(psB[pb_], cs_b), writes=(rt_b[b_],))
                            pool(lambda oi=oi, a_=a_, b_=b_, op_=op_: nc.gpsimd.tensor_tensor(out=ro[oi][:, :], in0=rt[a_][:, :],
                                                                                             in1=rt[b_][:, :], op=op_),
                                 reads=(rt_b[a_], rt_b[b_]), writes=(ro_b[oi],))
                            cx.dma("pool", dstd[h, half, :, t * T:(t + 1) * T], ro[oi][:, :], reads=(ro_b[oi],), writes=(),
                                   sembuf=ro_b[oi])
                for s4 in range(4):
                    sl += 1
                    si = sl % 2
                    w = wsl[si]
                    cx.dma("sp", w[:, :, :], wv.slab(s4), reads=(wv.b,), writes=(wsl_b[si],))
                    for tb in range(4):
                        i2 = pc % 2
                        pc += 1
                        vp = 5 + i2
                        for k in range(DC):
                            mm(psb[vp][:, :], hbuf[:, k, tb * 128:(tb + 1) * 128], w[:, k, :], k == 0, k == DC - 1,
                               reads=(wsl_b[si], h_b), writes=(psB[vp],))
                        act(lambda i2=i2, vp=vp: nc.scalar.copy(out=vst[i2][:, :], in_=psb[vp][:, :]), reads=(psB[vp],), writes=(vst_b[i2],))
                        r0 = t * T + tb * 128
                        cx.dma("pool", V_d[r0:r0 + 128, 2 * s4:2 * s4 + 2, :], vst[i2][:, :].rearrange("p (h v) -> p h v", h=2),
                               reads=(vst_b[i2],), writes=(), sembuf=vst_b[i2])
            allb = wsl_b + rt_b + ro_b + vst_b + [cs_b, hA[1][1]]
            for e in ("pe", "act", "dve", "sp", "pool"):
                cx.drain(e, allb)
        with ExitStack() as fes:
            kT = sbt(fes, "kT", [128, 2, S], BF16)
            qT = sbt(fes, "qT", [128, 2, S], BF16)
            Vh = sbt(fes, "Vh", [128, NBK, 257], BF16)
            kqv_b = Buf("kqv")
            oTh = sbt(fes, "oTh", [128, 2, S], BF16)
            oTh_b = Buf("oTh")
            pT = [sbt(fes, "pT", [128, 512], BF16) for i in range(2)]
            pT_b = [Buf("pT") for i in range(2)]
            oc = [sbt(fes, "oc", [128, 4, 257], F32) for i in range(2)]
            oc_b = [[Buf("oc") for qb in range(4)] for i in range(2)]
            of = [sbt(fes, "of", [128, 256], F32) for i in range(2)]
            of_b = [Buf("of") for i in range(2)]
            ob = [sbt(fes, "ob", [128, 256], BF16) for i in range(2)]
            ob_b = [Buf("ob") for i in range(2)]
            junk = sbt(fes, "junk", [128, 256], F32)
            sm = [sbt(fes, "sm", [128, 8], F32) for i in range(2)]
            sm_b = [Buf("sm") for i in range(2)]
            tri = sbt(fes, "tri", [128, 128], BF16)
            trif = sbt(fes, "trif", [128, 128], F32)
            lamb = sbt(fes, "lamb", [128, 512], F32)
            lw = sbt(fes, "lw", [128, 256], F32)
            lam = sbt(fes, "lam", [128, 4], F32)
            sgl = sbt(fes, "sgl", [128, 256], F32)
            dc_b = Buf("dconst")
            cx.dma("sp", trif[:, :], tric_in[:, :], writes=(dc_b,))
            dve(lambda: nc.vector.tensor_copy(out=tri[:, :], in_=trif[:, :]), reads=(dc_b,), writes=(dc_b,))
            cx.dma("sp", lamb[:, :], diff_lambda[0:1, :].partition_broadcast(128), writes=(dc_b,))
            cx.dma("sp", sgl[:, :], diff_subln_g[0:1, :].partition_broadcast(128), writes=(dc_b,))
            dve(lambda: nc.vector.tensor_scalar(out=sgl[:, :], in0=sgl[:, :], scalar1=1.0 - lam_init, scalar2=None, op0=ALU.mult),
                reads=(dc_b,), writes=(dc_b,))
            for i in range(2):
                dve(lambda i=i: nc.vector.tensor_tensor(out=lw[:, i * 128:(i + 1) * 128], in0=lamb[:, i * 256:i * 256 + 128],
                                                        in1=lamb[:, i * 256 + 128:i * 256 + 256], op=ALU.mult),
                    reads=(dc_b,), writes=(dc_b,))
                dve(lambda i=i: nc.vector.tensor_reduce(out=lam[:, i:i + 1], in_=lw[:, i * 128:(i + 1) * 128], axis=AX.X, op=ALU.add),
                    reads=(dc_b,), writes=(dc_b,))
            act(lambda: nc.scalar.activation(out=lam[:, 0:2], in_=lam[:, 0:2], func=AF.Exp), reads=(dc_b,), writes=(dc_b,))
            dve(lambda: nc.vector.tensor_tensor(out=lam[:, 2:3], in0=lam[:, 1:2], in1=lam[:, 0:1], op=ALU.subtract),
                reads=(dc_b,), writes=(dc_b,))
            dve(lambda: nc.vector.tensor_scalar(out=lam[:, 2:3], in0=lam[:, 2:3], scalar1=-lam_init, scalar2=None, op0=ALU.add),
                reads=(dc_b,), writes=(dc_b,))
            dve(lambda: nc.vector.memset(Vh[:, :, 256:257], 1.0), writes=(kqv_b,))
            scale = float(DIFF_DH) ** -0.5
            NQ = S // 512
            kbi = 0
            fi = 0
            for h in range(8):
                for c in range(2):
                    for hf in range(2):
                        cx.dma("sp", kT[hf * 64:(hf + 1) * 64, c, :], kT_d[h, hf, c * 64:(c + 1) * 64, :], writes=(kqv_b,))
                        cx.dma("sp", qT[hf * 64:(hf + 1) * 64, c, :], qT_d[h, hf, c * 64:(c + 1) * 64, :], writes=(kqv_b,))
                cx.dma("sp", Vh[:, :, 0:256], V_d[:, h, :].rearrange("(b p) v -> p b v", p=128), writes=(kqv_b,))
                its = [(qt, c, kb) for qt in range(NQ) for c in range(2) for kb in range(4 * qt + 4)]
                slot = {}

                def front(i):
                    nonlocal kbi
                    qt, c, kb = its[i]
                    sp_ = 5 + (kbi % 2)
                    pi = kbi % 2
                    kbi += 1
                    slot[i] = pi
                    mm(psb[sp_][:, :], kT[:, c, kb * 128:(kb + 1) * 128], qT[:, c, qt * 512:(qt + 1) * 512], True, True,
                       reads=(kqv_b,), writes=(psB[sp_],))
                    act(lambda sp_=sp_, pi=pi: nc.scalar.activation(out=pT[pi][:, :], in_=psb[sp_][:, :], func=AF.Exp, scale=scale),
                        reads=(psB[sp_],), writes=(pT_b[pi],))
                    d = kb - 4 * qt
                    if d >= 0:
                        dve(lambda pi=pi, d=d: nc.vector.tensor_tensor(out=pT[pi][:, d * 128:(d + 1) * 128],
                                                                       in0=pT[pi][:, d * 128:(d + 1) * 128], in1=tri[:, :], op=ALU.mult),
                            reads=(pT_b[pi], dc_b), writes=(pT_b[pi],))

                def back(i):
                    nonlocal fi
                    qt, c, kb = its[i]
                    pi = slot.pop(i)
                    d = kb - 4 * qt
                    for qb in range(max(d, 0), 4):
                        mm(psb[1 + qb][:, 0:257], pT[pi][:, qb * 128:(qb + 1) * 128], Vh[:, kb, :], kb == 0, kb == 4 * qt + qb,
                           reads=(pT_b[pi], kqv_b), writes=(psB[1 + qb],))
                    if kb != 4 * qt + 3:
                        return
                    for qb in range(4):
                        act(lambda c=c, qb=qb: nc.scalar.copy(out=oc[c][:, qb, :], in_=psb[1 + qb][:, 0:257]),
                            reads=(psB[1 + qb],), writes=(oc_b[c][qb],))
                    if c != 1:
                        return
                    for qb in range(4):
                        f = fi % 2
                        fi += 1
                        s_ = sm[f]
                        dve(lambda s_=s_, qb=qb: nc.vector.reciprocal(out=s_[:, 0:1], in_=oc[0][:, qb, 256:257]),
                            reads=(oc_b[0][qb],), writes=(sm_b[f],))
                        dve(lambda s_=s_, qb=qb: nc.vector.reciprocal(out=s_[:, 1:2], in_=oc[1][:, qb, 256:257]),
                            reads=(oc_b[1][qb],), writes=(sm_b[f],))
                        dve(lambda s_=s_: nc.vector.tensor_tensor(out=s_[:, 1:2], in0=s_[:, 1:2], in1=lam[:, 2:3], op=ALU.mult),
                            reads=(sm_b[f], dc_b), writes=(sm_b[f],))
                        dve(lambda s_=s_, qb=qb, f=f: nc.vector.tensor_scalar(out=of[f][:, :], in0=oc[0][:, qb, 0:256], scalar1=s_[:, 0:1],
                                                                             scalar2=None, op0=ALU.mult),
                            reads=(oc_b[0][qb], sm_b[f]), writes=(of_b[f],))
                        dve(lambda s_=s_, qb=qb, f=f: nc.vector.scalar_tensor_tensor(out=of[f][:, :], in0=oc[1][:, qb, 0:256], scalar=s_[:, 1:2],
                                                                                    in1=of[f][:, :], op0=ALU.mult, op1=ALU.add),
                            reads=(oc_b[1][qb], sm_b[f], of_b[f]), writes=(of_b[f],))
                        act(lambda s_=s_, f=f: nc.scalar.activation(out=junk[:, :], in_=of[f][:, :], func=AF.Square, accum_out=s_[:, 2:3]),
                            reads=(of_b[f],), writes=(sm_b[f],))
                        act(lambda s_=s_: nc.scalar.activation(out=s_[:, 3:4], in_=s_[:, 2:3], func=AF.Sqrt, bias=epsb[:, 0:1], scale=1.0 / 256.0),
                            reads=(sm_b[f], consts_b), writes=(sm_b[f],))
                        dve(lambda s_=s_: nc.vector.reciprocal(out=s_[:, 3:4], in_=s_[:, 3:4]), reads=(sm_b[f],), writes=(sm_b[f],))
                        dve(lambda s_=s_, f=f: nc.vector.scalar_tensor_tensor(out=ob[f][:, :], in0=of[f][:, :], scalar=s_[:, 3:4], in1=sgl[:, :],
                                                                             op0=ALU.mult, op1=ALU.mult),
                            reads=(of_b[f], sm_b[f], dc_b), writes=(ob_b[f],))
                        for j in range(2):
                            tr(pst[:, f * 512 + j * 128: f * 512 + (j + 1) * 128], ob[f][:, j * 128:(j + 1) * 128], ident_bf[:, :],
                               reads=(ob_b[f], consts_b), writes=(pst_b[f],), tick=(j == 1))
                        q0 = qt * 512 + qb * 128
                        act(lambda f=f, q0=q0: nc.scalar.copy(out=oTh[:, :, q0:q0 + 128],
                                                              in_=pst[:, f * 512: f * 512 + 256].rearrange("p (j n) -> p j n", j=2)),
                            reads=(pst_b[f],), writes=(oTh_b,))

                front(0)
                for i in range(len(its)):
                    if i + 1 < len(its):
                        front(i + 1)
                    back(i)
                cx.dma("pool", oT_d[h * 256:(h + 1) * 256, :].rearrange("(j p) n -> p j n", p=128), oTh[:, :, :],
                       reads=(oTh_b,), writes=(), sembuf=oTh_b)
            allb = [kqv_b, oTh_b, dc_b] + pT_b + [b for r in oc_b for b in r] + of_b + ob_b + sm_b
            for e in ("pe", "act", "dve", "sp", "pool"):
                cx.drain(e, allb)
        with ExitStack() as fes:
            wsl = [sbt(fes, "wsl", [128, DC, 256], BF16) for i in range(2)]
            wsl_b = [Buf("wsl") for i in range(2)]
            hC = [(hbuf0, hbuf0_b), (sbt(fes, "hc1", [128, DC, T], BF16), Buf("hc1"))]
            for t in range(NT):
                hbuf, h_b = hC[t % 2]
                cx.dma("sp", hbuf[:, :, :], oT_d[:, t * T:(t + 1) * T].rearrange("(c p) n -> p c n", p=128), writes=(h_b,))
                for s2 in range(DC // 2):
                    si = s2 % 2
                    cx.dma("sp", wsl[si][:, :, :], wo.slab(s2), reads=(wo.b,), writes=(wsl_b[si],))
                    for mi in range(2):
                        m = s2 * 2 + mi
                        yp = yb4[m % 4]
                        for k in range(DC):
                            mm(psb[yp][:, :], wsl[si][:, k, mi * 128:(mi + 1) * 128], hbuf[:, k, :], k == 0, k == DC - 1,
                               reads=(wsl_b[si], h_b), writes=(psB[yp],))
                        act(lambda m=m, yp=yp: nc.scalar.copy(out=xy[:, m, :], in_=psb[yp][:, :]), reads=(psB[yp],), writes=(xy_b,))
                resid_tile(t, ls)
            for e in ("pe", "act", "dve", "sp", "pool"):
                cx.drain(e, wsl_b + [hC[1][1]])


    def ffn_layer(li, win, wdn):
        ls = li * 2 + 1
        with ExitStack() as fes:
            NB = 2
            wg = [fes.enter_context(nc.sbuf_tensor("wg%d_%d" % (li, i), [128, DC, 256], BF16)) for i in range(NB)]
            wu = [fes.enter_context(nc.sbuf_tensor("wu%d_%d" % (li, i), [128, DC, 256], BF16)) for i in range(NB)]
            wg_b = [Buf("wg%d" % i) for i in range(NB)]
            wu_b = [Buf("wu%d" % i) for i in range(NB)]
            wd = [fes.enter_context(nc.sbuf_tensor("wd%d_%d" % (li, i), [128, FC // 2, 256], BF16)) for i in range(3)]
            wd_b = [Buf("wd%d" % i) for i in range(3)]
            abuf = fes.enter_context(nc.sbuf_tensor("abuf%d" % li, [128, FC, T], BF16))
            a_b = [Buf("a%d" % j) for j in range(FC)]
            gb = [fes.enter_context(nc.sbuf_tensor("gb%d_%d" % (li, i), [128, T + 2], F32)) for i in range(2)]
            gb_b = [Buf("gb%d" % i) for i in range(2)]
            sg = [fes.enter_context(nc.sbuf_tensor("sg%d_%d" % (li, i), [128, T], BF16)) for i in range(2)]
            sg_b = [Buf("sg%d" % i) for i in range(2)]
            halo = fes.enter_context(nc.sbuf_tensor("halo%d" % li, [128, FC, 2], F32))
            halo_b = Buf("halo")
            dve(lambda: nc.vector.memset(halo[:, :, :], 0.0), writes=(halo_b,))
            yb = [5, 6, 1, 2]

            def tap(tp, j):
                i = (li * 3 + tp) * FC + j
                return fdwT[:, i:i + 1]

            norm_tile(0, ls)
            hbuf, h_b = hbuf0, hbuf0_b
            for t in range(NT):
                for s2 in range(FC // 2):
                    i3 = s2 % NB
                    cx.dma("sp", wg[i3][:, :, :], win.slab(s2), reads=(win.b,), writes=(wg_b[i3],))
                    cx.dma("sp", wu[i3][:, :, :], win.slab(FC // 2 + s2), reads=(win.b,), writes=(wu_b[i3],))
                    for jj in range(2):
                        j = s2 * 2 + jj
                        i2 = j % 2
                        gp, up = 1 + i2, 3 + i2
                        for k in range(DC):
                            mm(psb[gp][:, :], wg[i3][:, k, jj * 128:(jj + 1) * 128], hbuf[:, k, :], k == 0, k == DC - 1,
                               reads=(wg_b[i3], h_b), writes=(psB[gp],))
                        for k in range(DC):
                            mm(psb[up][:, :], wu[i3][:, k, jj * 128:(jj + 1) * 128], hbuf[:, k, :], k == 0, k == DC - 1,
                               reads=(wu_b[i3], h_b), writes=(psB[up],))
                        g = gb[i2]
                        act(lambda g=g, gp=gp: nc.scalar.copy(out=g[:, 2:T + 2], in_=psb[gp][:, :]),
                            reads=(psB[gp],), writes=(gb_b[i2],))
                        act(lambda g=g, j=j: nc.scalar.copy(out=g[:, 0:2], in_=halo[:, j, :]),
                            reads=(halo_b,), writes=(gb_b[i2],))
                        act(lambda gp=gp, i2=i2, j=j: nc.scalar.activation(out=tmpf[i2][:, :], in_=psb[gp][:, :],
                                                                           func=AF.Identity, scale=tap(2, j)),
                            reads=(psB[gp], consts_b), writes=(tmpf_b[i2],))
                        dve(lambda g=g, i2=i2, j=j: nc.vector.scalar_tensor_tensor(
                            out=tmpf[i2][:, :], in0=g[:, 1:T + 1], scalar=tap(1, j), in1=tmpf[i2][:, :],
                            op0=ALU.mult, op1=ALU.add), reads=(gb_b[i2], tmpf_b[i2], consts_b), writes=(tmpf_b[i2],))
                        dve(lambda g=g, i2=i2, j=j: nc.vector.scalar_tensor_tensor(
                            out=tmpf[i2][:, :], in0=g[:, 0:T], scalar=tap(0, j), in1=tmpf[i2][:, :],
                            op0=ALU.mult, op1=ALU.add), reads=(gb_b[i2], tmpf_b[i2], consts_b), writes=(tmpf_b[i2],))
                        dve(lambda g=g, j=j: nc.vector.tensor_copy(out=halo[:, j, :], in_=g[:, T:T + 2]),
                            reads=(gb_b[i2],), writes=(halo_b,))
                        act(lambda i2=i2: nc.scalar.activation(out=sg[i2][:, :], in_=tmpf[i2][:, :], func=AF.Silu),
                            reads=(tmpf_b[i2],), writes=(sg_b[i2],))
                        dve(lambda i2=i2, up=up, j=j: nc.vector.tensor_tensor(out=abuf[:, j, :], in0=sg[i2][:, :],
                                                                             in1=psb[up][:, :], op=ALU.mult),
                            reads=(sg_b[i2], psB[up]), writes=(a_b[j],))
                pc = 0
                for s2 in range(DC // 2):
                    for half in range(2):
                        if pc == 3 and t + 1 < NT:
                            norm_tile(t + 1, ls)
                        w3 = pc % 3
                        pc += 1
                        cx.dma("sp", wd[w3][:, :, :], wdn.t[:, s2, half * 22:(half + 1) * 22, :], reads=(wdn.b,),
                               writes=(wd_b[w3],))
                        for mi in range(2):
                            m = s2 * 2 + mi
                            yp = yb[m % 4]
                            for jj in range(22):
                                j = half * 22 + jj
                                mm(psb[yp][:, :], wd[w3][:, jj, mi * 128:(mi + 1) * 128], abuf[:, j, :], j == 0, j == FC - 1,
                                   reads=(wd_b[w3], a_b[j]), writes=(psB[yp],), tick=(jj == 21))
                            if half == 1:
                                act(lambda m=m, yp=yp: nc.scalar.copy(out=xy[:, m, :], in_=psb[yp][:, :]),
                                    reads=(psB[yp],), writes=(xy_b,))
                resid_tile(t, ls)
            allb = wg_b + wu_b + wd_b + a_b + gb_b + sg_b + [halo_b]
            for e in ("pe", "act", "dve", "sp", "pool"):
                cx.drain(e, allb)

    subs = []
    for (kind, slot, li) in layer_kinds:
        if with_mixer and kind == 0:
            w = [WS("w_rqk%d" % li, ret_w_in[slot], D, 4096, 256), WS("w_rvg%d" % li, ret_w_in[slot], D, 8192, 512, col0=4096),
                 WS("w_ro%d" % li, ret_w_out[slot], 4096, D, 256)]
            subs.append((lambda li=li, slot=slot, w=w: ret_layer(li, slot, w[0], w[1], w[2]), w))
        if with_mixer and kind == 1:
            w = [WS("w_c1_%d" % li, conv_w_pw1[slot], D, 2 * D, 256), WS("w_c2_%d" % li, conv_w_pw2[slot], D, D, 256)]
            subs.append((lambda li=li, slot=slot, w=w: conv_layer(li, slot, w[0], w[1]), w))
        if with_mixer and kind == 2:
            w = diff_weights(li, slot)
            subs.append((lambda li=li, slot=slot, w=w: diff_layer(li, slot, w), w))
        if with_ffn:
            w = [WS("w_fin%d" % li, ffn_w_in[li], D, 2 * DFF, 256), WS("w_fdn%d" % li, ffn_w_down[li], DFF, D, 256)]
            subs.append((lambda li=li, w=w: ffn_layer(li, w[0], w[1]), w))
    for w in subs[0][1]:
        w.issue(10 ** 9)
    for i, (run, _) in enumerate(subs):
        nxt = subs[i + 1][1] if i + 1 < len(subs) else []
        tot = sum(len(w.todo) for w in nxt)
        per = -(-tot // NT) if tot else 0

        def hook(t, nxt=nxt, per=per):
            n = per
            for w in nxt:
                k = min(n, len(w.todo))
                w.issue(k)
                n -= k
        tile_hook[0] = hook
        run()
        for w in nxt:
            w.issue(10 ** 9)
    tile_hook[0] = None

    with ExitStack() as tes:
        ob4 = tes.enter_context(nc.sbuf_tensor("ob4", [128, 4, D], F32))
        ob_b = Buf("ob4")
        for t in range(NT):
            load_x_tile(t, xy, xy_b)
            for b in range(4):
                for q in range(4):
                    pb = 1 + (q % 2)
                    for cc in range(4):
                        c = q * 4 + cc
                        tr(psb[pb][:, cc * 128:(cc + 1) * 128], xy[:, c, b * 128:(b + 1) * 128], ident[:, :],
                           reads=(xy_b, consts_b), writes=(psB[pb],), tick=(cc == 3))
                    act(lambda b=b, q=q, pb=pb: nc.scalar.copy(out=ob4[:, b, q * 512:(q + 1) * 512], in_=psb[pb][:, :]),
                        reads=(psB[pb],), writes=(ob_b,))
            cx.dma("pool", out_d[t * T:(t + 1) * T, :].rearrange("(b p) f -> p b f", p=128), ob4[:, :, :],
                   reads=(ob_b,), writes=(), sembuf=ob_b)
        cx.drain("pool", [ob_b])
        cx.drain("sp", [ob_b])
        cx.drain("act", [ob_b])
    es.close()
    return nc


LAYERS = [(i % 3, i // 3, i) for i in range(DEPTH)]


def _ropec():
    o = np.zeros((128, 4), np.float32)
    p = np.arange(128)
    o[:, 0] = (np.float32(ROPE_THETA) ** (-(2 * p).astype(np.float32) / np.float32(256))).astype(np.float32)
    o[:, 1] = (np.float32(ROPE_THETA) ** (-(2 * (p % 64)).astype(np.float32) / np.float32(128))).astype(np.float32)
    return o


def _retc():
    o = np.zeros((128, 8 * 128 * 2 + 8), np.float32)
    idx = np.arange(128, dtype=np.float64)
    for h in range(8):
        lg = np.log1p(-np.exp2(-5.0 - h))
        rel = idx[None, :] - idx[:, None]
        o[:, h * 128:(h + 1) * 128] = np.where(rel >= 0, np.exp(np.maximum(rel, 0) * lg), 0.0) / 16.0
        o[:, 1024 + h * 128:1024 + (h + 1) * 128] = np.exp((idx + 1.0) * lg)[None, :]
        o[:, 2048 + h] = np.exp((127.0 - idx) * lg) / 16.0
    return o


def make_in_map(b, S, x, c, positions, mod_w, mod_b, norm_g, ret_w_in, ret_w_out, conv_w_pw1, conv_b_pw1,
                conv_w_dw, conv_b_dw, conv_ln_g, conv_ln_b, conv_w_pw2, conv_b_pw2, diff_w_in, diff_lambda,
                diff_subln_g, diff_w_out, ffn_w_in, ffn_w_dw, ffn_w_down):
    f = lambda a: np.ascontiguousarray(np.asarray(a, dtype=np.float32))
    return {
        "x": f(x[b, :S]),
        "c": f(c[b]).reshape(DC, 128),
        "positions": np.ascontiguousarray(np.asarray(positions[b, :S], dtype=np.int32)).reshape(1, S),
        "mod_w": f(mod_w), "mod_b": f(mod_b).reshape(DEPTH * 2, 3 * D),
        "norm_g": f(norm_g).reshape(DEPTH * 4 * DC, 128),
        "ret_w_in": f(ret_w_in), "ret_w_out": f(ret_w_out),
        "conv_w_pw1": f(conv_w_pw1), "conv_b_pw1": f(conv_b_pw1).reshape(32, 128),
        "conv_w_dw": f(conv_w_dw).reshape(CONV_W * DC, 128),
        "conv_vecs": np.concatenate([f(conv_b_dw).reshape(DC, 128), f(conv_ln_g).reshape(DC, 128),
                                     f(conv_ln_b).reshape(DC, 128), f(conv_b_pw2).reshape(DC, 128)], axis=0),
        "conv_w_pw2": f(conv_w_pw2),
        "diff_w_in": f(diff_w_in), "diff_lambda": f(diff_lambda).reshape(1, 512),
        "diff_subln_g": f(diff_subln_g).reshape(1, 256), "diff_w_out": f(diff_w_out),
        "ffn_w_in": f(ffn_w_in), "ffn_w_dw": f(ffn_w_dw).reshape(DEPTH * 3 * FC, 128),
        "ffn_w_down": f(ffn_w_down),
        "identc": np.eye(128, dtype=np.float32),
        "ropec": _ropec(), "retc": _retc(),
        "tric": np.triu(np.ones((128, 128), np.float32)),
    }


def kernel(**inputs):
    S = 4096
    B = 8
    nc = build(S, LAYERS)
    in_maps = [make_in_map(b, S, **inputs) for b in range(B)]
    res = run_bass_kernel_spmd(nc, in_maps, core_ids=list(range(B)))
    return np.stack([np.asarray(r["out"], dtype=np.float32) for r in res.results], axis=0)
```

```python
import math
from contextlib import ExitStack

import numpy as np
import concourse.bass as bass
import concourse.mybir as mybir
from concourse.bass_utils import run_bass_kernel_spmd

F32 = mybir.dt.float32
BF16 = mybir.dt.bfloat16
I32 = mybir.dt.int32
AF = mybir.ActivationFunctionType
ALU = mybir.AluOpType
AX = mybir.AxisListType

D = 2048
DC = 16
T = 512
DFF = 5632
FC = 44
EPS = 1e-6
DEPTH = 4
RET_H, RET_DK, RET_DV = 8, 256, 512
DIFF_H, DIFF_DH = 8, 128
CONV_W = 31
ROPE_THETA = 10000.0


class Buf:
    __slots__ = ("name", "w", "r", "sem", "semv")

    def __init__(self, name):
        self.name = name
        self.w = None
        self.r = []
        self.sem = None
        self.semv = 0


class Ctx:
    def __init__(self, nc):
        self.nc = nc
        self.es = ExitStack()
        self.eng = {"pe": nc.tensor, "act": nc.scalar, "dve": nc.vector, "pool": nc.gpsimd, "sp": nc.sync}
        self.cnt = {}
        for n in ("pe", "act", "dve", "pool"):
            self.cnt[n] = [self.es.enter_context(nc.semaphore("c_" + n)), 0]
        self.waited = {}
        self.nsem = 4

    def sb(self, name, shape, dt):
        return self.es.enter_context(self.nc.sbuf_tensor(name, shape, dt))

    def psum(self, name, shape, dt):
        return self.es.enter_context(self.nc.psum_tensor(name, shape, dt))

    def dram(self, name, shape, dt):
        return self.nc.dram_tensor(name, shape, dt, kind="Internal").ap()

    def wait(self, cons, t):
        if t is None:
            return
        sem, val = t
        key = (cons, id(sem))
        if self.waited.get(key, 0) >= val:
            return
        if cons == "pe" and sem is self.cnt["pe"][0]:
            return
        self.eng[cons].wait_ge(sem, val)
        self.waited[key] = val

    def _deps(self, cons, reads, writes):
        for b in reads:
            self.wait(cons, b.w)
        for b in writes:
            self.wait(cons, b.w)
            for t in b.r:
                self.wait(cons, t)

    @staticmethod
    def _addr(b, t):
        for i, (s, v) in enumerate(b.r):
            if s is t[0]:
                if v < t[1]:
                    b.r[i] = t
                return
        b.r.append(t)

    def _upd(self, t, reads, writes):
        for b in reads:
            self._addr(b, t)
        for b in writes:
            b.w = t
            b.r = []

    def op(self, e, fn, reads=(), writes=(), tick=True):
        self._deps(e, reads, writes)
        inst = fn()
        c = self.cnt[e]
        if tick:
            c[1] += 1
            inst.then_inc(c[0], 1)
            t = (c[0], c[1])
        else:
            t = (c[0], c[1] + 1)
        self._upd(t, reads, writes)
        return t

    def dma(self, q, out, in_, reads=(), writes=(), sembuf=None, **kw):
        self._deps(q, reads, writes)
        b = sembuf if sembuf is not None else (writes[0] if writes else reads[0])
        if b.sem is None:
            b.sem = self.es.enter_context(self.nc.semaphore("d%d" % self.nsem))
            self.nsem += 1
        b.semv += 16
        self.eng[q].dma_start(out=out, in_=in_, **kw).then_inc(b.sem, 16)
        t = (b.sem, b.semv)
        self._upd(t, reads, writes)
        return t

    def drain(self, e, bufs):
        for b in bufs:
            self.wait(e, b.w)
            for t in b.r:
                self.wait(e, t)


def build(S, layer_kinds, with_mixer=True, with_ffn=True):
    NT = S // T
    nc = bass.Bass("TRN2", target_bir_lowering=False)
    cx = Ctx(nc)
    es = cx.es

    def din(name, shape, dt=F32):
        return nc.dram_tensor(name, shape, dt, kind="ExternalInput").ap()

    x_in = din("x", [S, D])
    c_in = din("c", [DC, 128])
    pos_in = din("positions", [1, S], I32)
    mod_w = din("mod_w", [DEPTH, 2, D, 3 * D])
    mod_b = din("mod_b", [DEPTH * 2, 3 * D])
    norm_g = din("norm_g", [DEPTH * 4 * DC, 128])
    ret_w_in = din("ret_w_in", [2, D, 12288])
    ret_w_out = din("ret_w_out", [2, 4096, D])
    conv_w_pw1 = din("conv_w_pw1", [1, D, 2 * D])
    conv_b_pw1 = din("conv_b_pw1", [32, 128])
    conv_w_dw = din("conv_w_dw", [CONV_W * DC, 128])
    conv_vecs = din("conv_vecs", [4 * DC, 128])
    conv_w_pw2 = din("conv_w_pw2", [1, D, D])
    diff_w_in = din("diff_w_in", [1, D, 3 * D])
    diff_lambda = din("diff_lambda", [1, 4 * 128])
    diff_subln_g = din("diff_subln_g", [1, 256])
    diff_w_out = din("diff_w_out", [1, D, D])
    ffn_w_in = din("ffn_w_in", [DEPTH, D, 2 * DFF])
    ffn_w_dw = din("ffn_w_dw", [DEPTH * 3 * FC, 128])
    ffn_w_down = din("ffn_w_down", [DEPTH, DFF, D])
    ident_in = din("identc", [128, 128])
    ropec_in = din("ropec", [128, 4])
    retc_in = din("retc", [128, 8 * 128 * 2 + 8])
    out_d = nc.dram_tensor("out", [S, D], F32, kind="ExternalOutput").ap()
    kinds = [k for (k, _, _) in layer_kinds] if with_mixer else []
    tabs = {}
    tabs_b = Buf("tabs")
    if 0 in kinds:
        tabs["r"] = (cx.dram("cosR", [128, S], F32), cx.dram("sinR", [128, S], F32))
    if 2 in kinds:
        tabs["d"] = (cx.dram("cosD", [128, S], F32), cx.dram("sinD", [128, S], F32))

    xT = cx.dram("xT", [D, S], F32)
    xT_b = Buf("xT")
    modrow_d = cx.dram("modrow", [DEPTH * 2, 3 * D], F32)
    modrow_b = Buf("modrow")

    ident = cx.sb("ident", [128, 128], F32)
    ident_bf = cx.sb("ident_bf", [128, 128], BF16)
    ones_bf = cx.sb("ones_bf", [128, 128], BF16)
    cT = cx.sb("cT", [128, DC], F32)
    gT = cx.sb("gT", [128, DEPTH * 4 * DC], F32)
    modT = cx.sb("modT", [128, DEPTH * 2 * 48], F32)
    AT = cx.sb("AT", [128, DEPTH * 2 * DC], F32)
    GT = cx.sb("GT", [128, DEPTH * 2 * DC], F32)
    fdwT = cx.sb("fdwT", [128, DEPTH * 3 * FC], F32)
    epsb = cx.sb("epsb", [128, 1], F32)
    consts_b = Buf("consts")

    psb = [cx.psum("ps%d" % i, [128, 512], F32) for i in range(7)]
    psB = [Buf("ps%d" % i) for i in range(7)]
    pst = cx.psum("pst", [128, 1024], BF16)
    pst_b = [Buf("pst0"), Buf("pst1")]

    xy = cx.sb("xy", [128, DC, T], F32)
    xy_b = Buf("xy")
    hbuf0 = cx.sb("hbuf0", [128, DC, T], BF16)
    hbuf0_b = Buf("h0")
    nxc = [cx.sb("nxc%d" % i, [128, T], F32) for i in range(4)]
    nxc_b = [Buf("nxc%d" % i) for i in range(4)]
    rsq = [cx.sb("rsq%d" % i, [128, T], BF16) for i in range(2)]
    rsq_b = [Buf("rsq%d" % i) for i in range(2)]
    rtmp = [cx.sb("rtmp%d" % i, [128, T], F32) for i in range(4)]
    rtmp_b = [Buf("rtmp%d" % i) for i in range(4)]
    xT_w = []
    rstd2 = cx.sb("rstd2", [128, T], F32)
    rstd2_b = Buf("rstd2")
    ntmp = [cx.sb("ntmp%d" % i, [128, T], F32) for i in range(2)]
    ntmp_b = [Buf("ntmp%d" % i) for i in range(2)]
    sq = [cx.sb("sq%d" % i, [128, T], BF16) for i in range(2)]
    sq_b = [Buf("sq%d" % i) for i in range(2)]
    tmpf = [cx.sb("tmpf%d" % i, [128, T], F32) for i in range(2)]
    tmpf_b = [Buf("tmpf%d" % i) for i in range(2)]
    rstd = cx.sb("rstd", [128, T], F32)
    rstd_b = Buf("rstd")

    def act(fn, reads=(), writes=()):
        return cx.op("act", fn, reads, writes)

    def dve(fn, reads=(), writes=()):
        return cx.op("dve", fn, reads, writes)

    def pool(fn, reads=(), writes=()):
        return cx.op("pool", fn, reads, writes)

    def mm(out_ap, lhsT, rhs, start, stop, reads, writes, tick=None):
        return cx.op("pe", lambda: nc.tensor.matmul(out_ap, lhsT, rhs, start=start, stop=stop),
                     reads, writes, tick=(stop if tick is None else tick))

    def tr(out_ap, in_ap, idn, reads, writes, tick=True):
        return cx.op("pe", lambda: nc.tensor.transpose(out_ap, in_ap, idn), reads, writes, tick=tick)

    class WS:
        def __init__(self, name, src, K, N, SW, col0=0):
            self.KC = K // 128
            self.NS = N // SW
            self.SW = SW
            self.t = cx.dram(name, [128, self.NS, self.KC, SW], BF16)
            self.b = Buf(name)
            self.b.sem = es.enter_context(nc.semaphore("w_" + name))
            self.todo = []
            for k in range(self.KC):
                self.todo.append((self.t[:, :, k, :],
                                  src[k * 128:(k + 1) * 128, col0:col0 + N].rearrange("p (s w) -> p s w", w=SW)))
            self.b.w = (self.b.sem, 16 * len(self.todo))

        def issue(self, n):
            for _ in range(min(n, len(self.todo))):
                o, i = self.todo.pop(0)
                nc.gpsimd.dma_start(out=o, in_=i).then_inc(self.b.sem, 16)

        def slab(self, s):
            return self.t[:, s, :, :]

    tile_hook = [None]

    stg = cx.sb("stg", [128, 128], F32)
    stg_b = Buf("stg")

    def load_T(dst_ap, src_rows, R, src_b=None):
        cx.dma("sp", stg[0:R, :], src_rows, reads=(() if src_b is None else (src_b,)), writes=(stg_b,), sembuf=stg_b)
        tr(psb[0][:, 0:R], stg[0:R, :], ident[0:R, 0:R], reads=(stg_b, consts_b), writes=(psB[0],))
        dve(lambda: nc.vector.tensor_copy(out=dst_ap, in_=psb[0][:, 0:R]), reads=(psB[0],), writes=(consts_b,))

    cx.dma("sp", ident[:, :], ident_in[:, :], writes=(consts_b,))
    dve(lambda: nc.vector.tensor_copy(out=ident_bf[:, :], in_=ident[:, :]), reads=(consts_b,), writes=(consts_b,))
    dve(lambda: nc.vector.memset(ones_bf[:, :], 1.0), writes=(consts_b,))
    dve(lambda: nc.vector.memset(epsb[:, :], EPS), writes=(consts_b,))
    load_T(cT[:, :], c_in[:, :], DC)
    act(lambda: nc.scalar.activation(out=cT[:, :], in_=cT[:, :], func=AF.Silu), reads=(consts_b,), writes=(consts_b,))
    for i in range(0, DEPTH * 4 * DC, 128):
        load_T(gT[:, i:i + 128], norm_g[i:i + 128, :], 128)
    nf = DEPTH * 3 * FC
    for i in range(0, nf, 128):
        r = min(128, nf - i)
        load_T(fdwT[:, i:i + r], ffn_w_dw[i:i + r, :], r)

    with ExitStack() as mes:
        mwa = [mes.enter_context(nc.sbuf_tensor("mw%d" % i, [128, DC, 256], F32)) for i in range(2)]
        mw_b = [Buf("mw%d" % i) for i in range(2)]
        mrow = mes.enter_context(nc.sbuf_tensor("mrow", [1, 3 * D], F32))
        mrow_b = Buf("mrow")
        mbias = mes.enter_context(nc.sbuf_tensor("mbias", [1, 3 * D], F32))
        mbias_b = Buf("mbias")
        it = 0
        for (kind, slot, li) in layer_kinds:
            for sub in range(2):
                if (sub == 0 and not with_mixer) or (sub == 1 and not with_ffn):
                    continue
                ls = li * 2 + sub
                cx.dma("sp", mbias[:, :], mod_b[ls:ls + 1, :], writes=(mbias_b,))
                for n in range(24):
                    w = mwa[it % 2]
                    wb = mw_b[it % 2]
                    cx.dma("sp", w[:, :, :],
                           mod_w[li, sub, :, n * 256:(n + 1) * 256].rearrange("(k p) n -> p k n", p=128),
                           writes=(wb,))
                    pb = 1 + (it % 2)
                    for k in range(DC):
                        mm(psb[pb][0:1, 0:256], cT[:, k:k + 1], w[:, k, :], k == 0, k == DC - 1,
                           reads=(wb, consts_b), writes=(psB[pb],))
                    dve(lambda pb=pb, n=n: nc.vector.tensor_tensor(out=mrow[:, n * 256:(n + 1) * 256], in0=psb[pb][0:1, 0:256],
                                                                   in1=mbias[:, n * 256:(n + 1) * 256], op=ALU.add),
                        reads=(psB[pb], mbias_b), writes=(mrow_b,))
                    it += 1
                cx.dma("pool", modrow_d[ls:ls + 1, :], mrow[:, :], reads=(mrow_b,), writes=(modrow_b,), sembuf=mrow_b)
                load_T(modT[:, ls * 48:(ls + 1) * 48], modrow_d[ls, :].rearrange("(r p) -> r p", p=128), 48, modrow_b)
                sc = modT[:, ls * 48 + 16: ls * 48 + 32]
                gt = modT[:, ls * 48 + 32: ls * 48 + 48]
                gpre = gT[:, (li * 4 + 2 * sub) * DC:(li * 4 + 2 * sub + 1) * DC]
                gpost = gT[:, (li * 4 + 2 * sub + 1) * DC:(li * 4 + 2 * sub + 2) * DC]
                dve(lambda sc=sc, gpre=gpre, ls=ls: nc.vector.scalar_tensor_tensor(
                    out=AT[:, ls * DC:(ls + 1) * DC], in0=sc, scalar=1.0, in1=gpre, op0=ALU.add, op1=ALU.mult),
                    reads=(consts_b,), writes=(consts_b,))
                dve(lambda gt=gt, gpost=gpost, ls=ls: nc.vector.tensor_tensor(
                    out=GT[:, ls * DC:(ls + 1) * DC], in0=gt, in1=gpost, op=ALU.mult),
                    reads=(consts_b,), writes=(consts_b,))
        cx.drain("pe", mw_b)
        cx.drain("dve", [mrow_b, mbias_b] + mw_b)
        cx.drain("pool", [mrow_b])
        cx.drain("sp", [mbias_b] + mw_b)

    def wait_xT(q):
        last = {}
        for (sem, v) in xT_w:
            if id(sem) not in last or last[id(sem)][1] < v:
                last[id(sem)] = (sem, v)
        xT_w[:] = list(last.values())
        for t_ in xT_w:
            cx.wait(q, t_)

    def load_x_tile(t, dst, dst_b):
        wait_xT("sp")
        cx.dma("sp", dst[:, :, :], xT[:, t * T:(t + 1) * T].rearrange("(c p) n -> p c n", p=128),
               reads=(xT_b,), writes=(dst_b,))

    NL = 4
    pend_resid = [None]

    def defer_resid(t, ls):
        flush_resid()
        pend_resid[0] = (t, ls)

    def flush_resid():
        if pend_resid[0] is not None:
            t_, ls_ = pend_resid[0]
            pend_resid[0] = None
            resid_tile(t_, ls_)


    def norm_tile(t, ls, dst=None):
        if tile_hook[0] is not None:
            tile_hook[0](t)
        hbuf, h_b = dst if dst is not None else (hbuf0, hbuf0_b)
        wait_xT("act")

        def issue(c):
            i = c % NL
            cx.dma("act", nxc[i][:, :], xT[c * 128:(c + 1) * 128, t * T:(t + 1) * T], reads=(xT_b,), writes=(nxc_b[i],))

        for c in range(NL):
            issue(c)
        for c in range(DC):
            i = c % 2
            n_ = c % NL
            act(lambda c=c, i=i, n_=n_: nc.scalar.activation(out=sq[i][:, :], in_=nxc[n_][:, :], func=AF.Square),
                reads=(nxc_b[n_],), writes=(sq_b[i],))
            mm(psb[0][:, :], ones_bf[:, :], sq[i][:, :], c == 0, c == DC - 1, reads=(sq_b[i], consts_b), writes=(psB[0],),
               tick=True)
            if c + NL < DC:
                issue(c + NL)
        for c in range(NL):
            issue(c)
        act(lambda: nc.scalar.activation(out=rstd[:, :], in_=psb[0][:, :], func=AF.Sqrt, bias=epsb[:, 0:1], scale=1.0 / D),
            reads=(psB[0], consts_b), writes=(rstd_b,))
        dve(lambda: nc.vector.reciprocal(out=rstd[:, :], in_=rstd[:, :]), reads=(rstd_b,), writes=(rstd_b,))
        for c in range(DC):
            i = c % 2
            n_ = c % NL
            dve(lambda c=c, i=i, n_=n_: nc.vector.scalar_tensor_tensor(
                out=ntmp[i][:, :], in0=nxc[n_][:, :], scalar=AT[:, ls * DC + c: ls * DC + c + 1], in1=rstd[:, :],
                op0=ALU.mult, op1=ALU.mult), reads=(nxc_b[n_], rstd_b, consts_b), writes=(ntmp_b[i],))
            act(lambda c=c, i=i, hbuf=hbuf: nc.scalar.activation(
                out=hbuf[:, c, :], in_=ntmp[i][:, :], func=AF.Identity,
                bias=modT[:, ls * 48 + c: ls * 48 + c + 1], scale=1.0), reads=(ntmp_b[i], consts_b), writes=(h_b,))
            if c + NL < DC:
                issue(c + NL)

    def resid_tile(t, ls):
        for c in range(DC):
            i = c % 2
            act(lambda c=c, i=i: nc.scalar.activation(out=rsq[i][:, :], in_=xy[:, c, :], func=AF.Square),
                reads=(xy_b,), writes=(rsq_b[i],))
            mm(psb[0][:, :], ones_bf[:, :], rsq[i][:, :], c == 0, c == DC - 1, reads=(rsq_b[i], consts_b), writes=(psB[0],),
               tick=True)
        act(lambda: nc.scalar.activation(out=rstd2[:, :], in_=psb[0][:, :], func=AF.Sqrt, bias=epsb[:, 0:1], scale=1.0 / D),
            reads=(psB[0], consts_b), writes=(rstd2_b,))
        dve(lambda: nc.vector.reciprocal(out=rstd2[:, :], in_=rstd2[:, :]), reads=(rstd2_b,), writes=(rstd2_b,))
        for c in range(DC):
            i = c % 4
            dve(lambda c=c, i=i: nc.vector.scalar_tensor_tensor(
                out=rtmp[i][:, :], in0=xy[:, c, :], scalar=GT[:, ls * DC + c: ls * DC + c + 1], in1=rstd2[:, :],
                op0=ALU.mult, op1=ALU.mult), reads=(xy_b, rstd2_b, consts_b), writes=(rtmp_b[i],))
            cx.dma("pool", xT[c * 128:(c + 1) * 128, t * T:(t + 1) * T], rtmp[i][:, :],
                   reads=(rtmp_b[i],), writes=(), sembuf=rtmp_b[i], accum_op=ALU.add)
            xT_w.append((rtmp_b[i].sem, rtmp_b[i].semv))

    with ExitStack() as tes:
        xin4 = tes.enter_context(nc.sbuf_tensor("xin4", [128, 4, D], F32))
        xin_b = Buf("xin4")
        for t in range(NT):
            cx.dma("sp", xin4[:, :, :], x_in[t * T:(t + 1) * T, :].rearrange("(b p) f -> p b f", p=128), writes=(xin_b,))
            for c in range(DC):
                pb = 1 + (c % 2)
                for b in range(4):
                    tr(psb[pb][:, b * 128:(b + 1) * 128], xin4[:, b, c * 128:(c + 1) * 128], ident[:, :],
                       reads=(xin_b, consts_b), writes=(psB[pb],), tick=(b == 3))
                act(lambda c=c, pb=pb: nc.scalar.copy(out=xy[:, c, :], in_=psb[pb][:, :]), reads=(psB[pb],), writes=(xy_b,))
            cx.dma("pool", xT[:, t * T:(t + 1) * T].rearrange("(c p) n -> p c n", p=128), xy[:, :, :],
                   reads=(xy_b,), writes=(xT_b,), sembuf=xy_b)
        for e in ("pe", "sp"):
            cx.drain(e, [xin_b])

    ropec = cx.sb("ropec_sb", [128, 4], F32)
    cx.dma("sp", ropec[:, :], ropec_in[:, :], writes=(consts_b,))
    TWO_PI_HI = 6.28125
    TWO_PI_LO = 2.0 * math.pi - 6.28125
    if tabs:
        with ExitStack() as tes:
            posi = tes.enter_context(nc.sbuf_tensor("posi", [128, T], I32))
            posf = tes.enter_context(nc.sbuf_tensor("posf", [128, T], F32))
            ang = tes.enter_context(nc.sbuf_tensor("ang", [128, T], F32))
            uu = tes.enter_context(nc.sbuf_tensor("uu", [128, T], F32))
            ki = tes.enter_context(nc.sbuf_tensor("ki", [128, T], I32))
            kf = tes.enter_context(nc.sbuf_tensor("kf", [128, T], F32))
            rr = [tes.enter_context(nc.sbuf_tensor("rr%d" % i, [128, T], F32)) for i in range(2)]
            tb_ = {n: Buf(n) for n in ("posi", "posf", "ang", "uu", "ki", "kf", "rr0", "rr1")}
            it = 0
            for t in range(NT):
                cx.dma("sp", posi[:, :], pos_in[0:1, t * T:(t + 1) * T].partition_broadcast(128), writes=(tb_["posi"],))
                dve(lambda: nc.vector.tensor_copy(out=posf[:, :], in_=posi[:, :]), reads=(tb_["posi"],), writes=(tb_["posf"],))
                for key, col in (("r", 0), ("d", 1)):
                    if key not in tabs:
                        continue
                    dve(lambda col=col: nc.vector.tensor_scalar(out=ang[:, :], in0=posf[:, :], scalar1=ropec[:, col:col + 1],
                                                                scalar2=None, op0=ALU.mult),
                        reads=(tb_["posf"], consts_b), writes=(tb_["ang"],))
                    for ph, which in ((math.pi / 2, 0), (0.0, 1)):
                        r = rr[it % 2]
                        rb = tb_["rr%d" % (it % 2)]
                        it += 1
                        dve(lambda ph=ph: nc.vector.tensor_scalar(out=uu[:, :], in0=ang[:, :], scalar1=1.0 / (2 * math.pi),
                                                                  scalar2=ph / (2 * math.pi), op0=ALU.mult, op1=ALU.add),
                            reads=(tb_["ang"],), writes=(tb_["uu"],))
                        dve(lambda: nc.vector.tensor_copy(out=ki[:, :], in_=uu[:, :]), reads=(tb_["uu"],), writes=(tb_["ki"],))
                        dve(lambda: nc.vector.tensor_copy(out=kf[:, :], in_=ki[:, :]), reads=(tb_["ki"],), writes=(tb_["kf"],))
                        dve(lambda r=r: nc.vector.scalar_tensor_tensor(out=r[:, :], in0=kf[:, :], scalar=-TWO_PI_HI, in1=ang[:, :],
                                                                       op0=ALU.mult, op1=ALU.add),
                            reads=(tb_["kf"], tb_["ang"]), writes=(rb,))
                        dve(lambda r=r: nc.vector.scalar_tensor_tensor(out=r[:, :], in0=kf[:, :], scalar=-TWO_PI_LO, in1=r[:, :],
                                                                       op0=ALU.mult, op1=ALU.add),
                            reads=(tb_["kf"], rb), writes=(rb,))
                        dve(lambda r=r, ph=ph: nc.vector.tensor_scalar(out=r[:, :], in0=r[:, :], scalar1=ph, scalar2=math.pi,
                                                                       op0=ALU.add, op1=ALU.min),
                            reads=(rb,), writes=(rb,))
                        dve(lambda r=r: nc.vector.tensor_scalar(out=r[:, :], in0=r[:, :], scalar1=-math.pi, scalar2=None,
                                                                op0=ALU.max), reads=(rb,), writes=(rb,))
                        act(lambda r=r: nc.scalar.activation(out=r[:, :], in_=r[:, :], func=AF.Sin), reads=(rb,), writes=(rb,))
                        cx.dma("pool", tabs[key][which][:, t * T:(t + 1) * T], r[:, :], reads=(rb,), writes=(), sembuf=rb)
            for r_ in ("rr0", "rr1"):
                tabs_b.w = None
            cx.drain("dve", list(tb_.values()))
            cx.drain("pool", list(tb_.values()))
            cx.drain("sp", list(tb_.values()))
            cx.drain("act", list(tb_.values()))
    uid = [0]

    def sbt(st, name, shape, dt):
        uid[0] += 1
        return st.enter_context(nc.sbuf_tensor("%s_%d" % (name, uid[0]), shape, dt))

    def conv_layer(li, slot, w1s, w2s):
        ls = li * 2
        with ExitStack() as fes:
            cb1T = sbt(fes, "cb1T", [128, 32], F32)
            cdwT = sbt(fes, "cdwT", [128, CONV_W * DC], F32)
            cvT = sbt(fes, "cvT", [128, 4 * DC], F32)
            load_T(cb1T[:, :], conv_b_pw1[:, :], 32)
            for i in range(0, CONV_W * DC, 128):
                r = min(128, CONV_W * DC - i)
                load_T(cdwT[:, i:i + r], conv_w_dw[i:i + r, :], r)
            load_T(cvT[:, :], conv_vecs[:, :], 64)
            wa = [sbt(fes, "wa", [128, DC, 256], BF16) for i in range(2)]
            wb = [sbt(fes, "wb", [128, DC, 256], BF16) for i in range(2)]
            wa_b = [Buf("wa") for i in range(2)]
            wb_b = [Buf("wb") for i in range(2)]
            ub = [sbt(fes, "ub", [128, T + 30], BF16) for i in range(2)]
            ub_b = [Buf("ub") for i in range(2)]
            dg = [sbt(fes, "dg", [128, CONV_W, 128], BF16) for i in range(2)]
            dg_b = [Buf("dg") for i in range(2)]
            sgc = [sbt(fes, "sgc", [128, T], F32) for i in range(2)]
            sgc_b = [Buf("sgc") for i in range(2)]
            vbuf = sbt(fes, "vbuf", [128, DC, T], F32)
            v_b = [Buf("v%d" % j) for j in range(DC)]
            chalo = sbt(fes, "chalo", [128, DC, 30], BF16)
            chalo_b = Buf("chalo")
            mu = sbt(fes, "mu", [128, T], F32)
            mu_b = Buf("mu")
            m2 = sbt(fes, "m2", [128, T], F32)
            m2_b = Buf("m2")
            dve(lambda: nc.vector.memset(chalo[:, :, :], 0.0), writes=(chalo_b,))
            hs = sbt(fes, "hs", [128, DC, T], BF16)
            hs_b = Buf("hs")
            pend = [None]
            norm_tile(0, ls)
            hbuf, h_b = hbuf0, hbuf0_b
            for t in range(NT):
                for s2 in range(DC // 2):
                    if s2 == 2:
                        flush_resid()
                    i3 = s2 % 2
                    cx.dma("sp", wa[i3][:, :, :], w1s.slab(s2), reads=(w1s.b,), writes=(wa_b[i3],))
                    cx.dma("sp", wb[i3][:, :, :], w1s.slab(DC // 2 + s2), reads=(w1s.b,), writes=(wb_b[i3],))
                    for jj in range(2):
                        j = s2 * 2 + jj
                        i2 = j % 2
                        ap_, bp_ = 1 + i2, 3 + i2
                        for k in range(DC):
                            mm(psb[ap_][:, :], wa[i3][:, k, jj * 128:(jj + 1) * 128], hbuf[:, k, :], k == 0, k == DC - 1,
                               reads=(wa_b[i3], h_b), writes=(psB[ap_],))
                        for k in range(DC):
                            mm(psb[bp_][:, :], wb[i3][:, k, jj * 128:(jj + 1) * 128], hbuf[:, k, :], k == 0, k == DC - 1,
                               reads=(wb_b[i3], h_b), writes=(psB[bp_],))
                        u = ub[i2]
                        act(lambda i2=i2, bp_=bp_, j=j: nc.scalar.activation(out=sgc[i2][:, :], in_=psb[bp_][:, :], func=AF.Sigmoid,
                                                                            bias=cb1T[:, DC + j:DC + j + 1], scale=1.0),
                            reads=(psB[bp_], consts_b), writes=(sgc_b[i2],))
                        act(lambda u=u, j=j: nc.scalar.copy(out=u[:, 0:30], in_=chalo[:, j, :]), reads=(chalo_b,), writes=(ub_b[i2],))
                        dve(lambda u=u, i2=i2, ap_=ap_, j=j: nc.vector.scalar_tensor_tensor(
                            out=u[:, 30:T + 30], in0=psb[ap_][:, :], scalar=cb1T[:, j:j + 1], in1=sgc[i2][:, :],
                            op0=ALU.add, op1=ALU.mult), reads=(psB[ap_], sgc_b[i2], consts_b), writes=(ub_b[i2],))
                        for tau in range(CONV_W):
                            pool(lambda i2=i2, j=j, tau=tau: nc.gpsimd.tensor_scalar(
                                out=dg[i2][:, tau, :], in0=ident_bf[:, :], scalar1=cdwT[:, tau * DC + j:tau * DC + j + 1], scalar2=1.0,
                                op0=ALU.mult, op1=ALU.mult), reads=(consts_b,), writes=(dg_b[i2],))
                        dve(lambda u=u, j=j: nc.vector.tensor_copy(out=chalo[:, j, :], in_=u[:, T:T + 30]),
                            reads=(ub_b[i2],), writes=(chalo_b,))

                        def conv_mm(u=u, i2=i2, j=j):
                            cp = 5 + i2
                            for tau in range(CONV_W):
                                mm(psb[cp][:, :], dg[i2][:, tau, :], u[:, tau:tau + T], tau == 0, tau == CONV_W - 1,
                                   reads=(dg_b[i2], ub_b[i2]), writes=(psB[cp],))
                            act(lambda: nc.scalar.activation(out=vbuf[:, j, :], in_=psb[cp][:, :], func=AF.Identity,
                                                             bias=cvT[:, j:j + 1], scale=1.0),
                                reads=(psB[cp], consts_b), writes=(v_b[j],))
                        if pend[0] is not None:
                            pend[0]()
                        pend[0] = conv_mm
                if pend[0] is not None:
                    pend[0]()
                    pend[0] = None
                if t + 1 < NT:
                    norm_tile(t + 1, ls)
                for j in range(DC):
                    i = j % 2
                    act(lambda j=j, i=i: nc.scalar.copy(out=sq[i][:, :], in_=vbuf[:, j, :]), reads=(v_b[j],), writes=(sq_b[i],))
                    mm(psb[0][:, :], ones_bf[:, :], sq[i][:, :], j == 0, j == DC - 1, reads=(sq_b[i], consts_b), writes=(psB[0],),
                       tick=True)
                for j in range(DC):
                    i = j % 2
                    act(lambda j=j, i=i: nc.scalar.activation(out=sq[i][:, :], in_=vbuf[:, j, :], func=AF.Square),
                        reads=(v_b[j],), writes=(sq_b[i],))
                    mm(psb[5][:, :], ones_bf[:, :], sq[i][:, :], j == 0, j == DC - 1, reads=(sq_b[i], consts_b), writes=(psB[5],),
                       tick=True)
                dve(lambda: nc.vector.tensor_scalar(out=mu[:, :], in0=psb[0][:, :], scalar1=1.0 / D, scalar2=None, op0=ALU.mult),
                    reads=(psB[0],), writes=(mu_b,))
                dve(lambda: nc.vector.tensor_tensor(out=m2[:, :], in0=mu[:, :], in1=mu[:, :], op=ALU.mult),
                    reads=(mu_b,), writes=(m2_b,))
                dve(lambda: nc.vector.scalar_tensor_tensor(out=m2[:, :], in0=psb[5][:, :], scalar=1.0 / D, in1=m2[:, :],
                                                           op0=ALU.mult, op1=ALU.subtract), reads=(psB[5], m2_b), writes=(m2_b,))
                act(lambda: nc.scalar.activation(out=rstd[:, :], in_=m2[:, :], func=AF.Sqrt, bias=epsb[:, 0:1], scale=1.0),
                    reads=(m2_b, consts_b), writes=(rstd_b,))
                dve(lambda: nc.vector.reciprocal(out=rstd[:, :], in_=rstd[:, :]), reads=(rstd_b,), writes=(rstd_b,))
                for j in range(DC):
                    i = j % 2
                    dve(lambda j=j, i=i: nc.vector.tensor_tensor(out=tmpf[i][:, :], in0=vbuf[:, j, :], in1=mu[:, :], op=ALU.subtract),
                        reads=(v_b[j], mu_b), writes=(tmpf_b[i],))
                    dve(lambda j=j, i=i: nc.vector.scalar_tensor_tensor(
                        out=tmpf[i][:, :], in0=tmpf[i][:, :], scalar=cvT[:, DC + j:DC + j + 1], in1=rstd[:, :],
                        op0=ALU.mult, op1=ALU.mult), reads=(tmpf_b[i], rstd_b, consts_b), writes=(tmpf_b[i],))
                    act(lambda j=j, i=i: nc.scalar.activation(out=hs[:, j, :], in_=tmpf[i][:, :], func=AF.Silu,
                                                              bias=cvT[:, 2 * DC + j:2 * DC + j + 1], scale=1.0),
                        reads=(tmpf_b[i], consts_b), writes=(hs_b,))
                for s2 in range(DC // 2):
                    i3 = s2 % 2
                    cx.dma("sp", wa[i3][:, :, :], w2s.slab(s2), reads=(w2s.b,), writes=(wa_b[i3],))
                    for mi in range(2):
                        m = s2 * 2 + mi
                        yp = yb4[m % 4]
                        for k in range(DC):
                            mm(psb[yp][:, :], wa[i3][:, k, mi * 128:(mi + 1) * 128], hs[:, k, :], k == 0, k == DC - 1,
                               reads=(wa_b[i3], hs_b), writes=(psB[yp],))
                        act(lambda m=m, yp=yp: nc.scalar.activation(out=xy[:, m, :], in_=psb[yp][:, :], func=AF.Identity,
                                                                    bias=cvT[:, 3 * DC + m:3 * DC + m + 1], scale=1.0),
                            reads=(psB[yp], consts_b), writes=(xy_b,))
                defer_resid(t, ls)
            flush_resid()
            allb = wa_b + wb_b + ub_b + sgc_b + v_b + dg_b + [chalo_b, mu_b, m2_b, consts_b, hs_b]
            for e in ("pe", "act", "dve", "sp", "pool"):
                cx.drain(e, allb)

    def ret_layer(li, slot, wqk, wvg, wo):
        ls = li * 2
        cosd, sind = tabs["r"]
        with ExitStack() as fes:
            retc = sbt(fes, "retc", [128, 2056], F32)
            cx.dma("sp", retc[:, :], retc_in[:, :], writes=(consts_b,))
            decT = lambda h: retc[:, h * 128:(h + 1) * 128]
            cdb = lambda h: retc[:, 1024 + h * 128:1024 + (h + 1) * 128]
            sdv = lambda h: retc[:, 2048 + h:2048 + h + 1]
            cs = [sbt(fes, "cs", [128, T], F32) for i in range(2)]
            cs_b = Buf("cs")
            wsl = [sbt(fes, "wsl", [128, DC, 512], BF16) for i in range(2)]
            wsl_b = [Buf("wsl") for i in range(2)]
            qr = sbt(fes, "qr", [128, 2, 2, T], BF16)
            kr = sbt(fes, "kr", [128, 2, 2, T], BF16)
            qr_b = [Buf("qr") for i in range(2)]
            kr_b = [Buf("kr") for i in range(2)]
            vsb = sbt(fes, "vsb", [128, 4, 2, 512], BF16)
            gsb = sbt(fes, "gsb", [128, 4, 2, 512], BF16)
            v_b = [[Buf("v") for hh in range(2)] for tb in range(4)]
            g_b = [[Buf("g") for hh in range(2)] for tb in range(4)]
            oT = sbt(fes, "oT", [128, 8, T], BF16)
            oT_b = Buf("oT")
            stf = sbt(fes, "stf", [128, 8, 2, 512], F32)
            stf_b = [Buf("stf") for h in range(8)]
            sbf = [sbt(fes, "sbf", [128, 2, 512], BF16) for i in range(2)]
            sbf_b = [Buf("sbf") for i in range(2)]
            rt = tmpf
            rt_b = tmpf_b
            pt = [sbt(fes, "pt", [128, 128], BF16) for i in range(2)]
            pt_b = [Buf("pt") for i in range(2)]
            ktm = [sbt(fes, "ktm", [128, 256], BF16) for i in range(2)]
            ktm_b = [Buf("ktm") for i in range(2)]
            qs = [sbt(fes, "qs", [128, 2, 128], BF16) for i in range(2)]
            qs_b = [Buf("qs") for i in range(2)]
            on = [sbt(fes, "on", [128, 512], BF16) for i in range(2)]
            on_b = [Buf("on") for i in range(2)]
            st6 = [sbt(fes, "st6", [128, 6], F32) for i in range(2)]
            mv = [sbt(fes, "mv", [128, 4], F32) for i in range(2)]
            mv_b = [Buf("mv") for i in range(2)]
            dve(lambda: nc.vector.memset(stf[:, :, :, :], 0.0), writes=tuple(stf_b))
            sl = [0]

            def next_slab():
                sl[0] += 1
                return sl[0] % 2

            pc = [0]
            norm_tile(0, ls)
            hbuf, h_b = hbuf0, hbuf0_b
            for t in range(NT):
                cx.dma("sp", cs[0][:, :], cosd[:, t * T:(t + 1) * T], reads=(tabs_b,), writes=(cs_b,))
                cx.dma("sp", cs[1][:, :], sind[:, t * T:(t + 1) * T], reads=(tabs_b,), writes=(cs_b,))
                for g in range(4):
                    for hh in range(2):
                        h = 2 * g + hh
                        for which in range(2):
                            si = next_slab()
                            w = wsl[si]
                            cx.dma("sp", w[:, :, 0:256], wqk.slab(which * 8 + h), reads=(wqk.b,), writes=(wsl_b[si],))
                            i2 = pc[0] % 2
                            pc[0] += 1
                            p1, p2 = 1 + i2, 3 + i2
                            for k in range(DC):
                                mm(psb[p1][:, :], w[:, k, 0:128], hbuf[:, k, :], k == 0, k == DC - 1,
                                   reads=(wsl_b[si], h_b), writes=(psB[p1],))
                            for k in range(DC):
                                mm(psb[p2][:, :], w[:, k, 128:256], hbuf[:, k, :], k == 0, k == DC - 1,
                                   reads=(wsl_b[si], h_b), writes=(psB[p2],))
                            c_, s_ = cs[0], cs[1]
                            dst = qr if which == 0 else kr
                            dst_b = (qr_b if which == 0 else kr_b)[hh]
                            for (pa, ca, pb_, cb_, half, op_) in ((p1, c_, p2, s_, 0, ALU.subtract), (p2, c_, p1, s_, 1, ALU.add)):
                                a_, b_ = 0, 1
                                dve(lambda pa=pa, ca=ca, a_=a_: nc.vector.tensor_tensor(out=rt[a_][:, :], in0=psb[pa][:, :],
                                                                                         in1=ca[:, :], op=ALU.mult),
                                    reads=(psB[pa], cs_b), writes=(rt_b[a_],))
                                dve(lambda pb_=pb_, cb_=cb_, b_=b_: nc.vector.tensor_tensor(out=rt[b_][:, :], in0=psb[pb_][:, :],
                                                                                             in1=cb_[:, :], op=ALU.mult),
                                    reads=(psB[pb_], cs_b), writes=(rt_b[b_],))
                                pool(lambda dst=dst, hh=hh, half=half, a_=a_, b_=b_, op_=op_: nc.gpsimd.tensor_tensor(
                                    out=dst[:, hh, half, :], in0=rt[a_][:, :], in1=rt[b_][:, :], op=op_),
                                    reads=(rt_b[a_], rt_b[b_]), writes=(dst_b,))
                    if g == 0:
                        flush_resid()
                    for hh in range(2):
                        h = 2 * g + hh
                        for which in range(2):
                            si = next_slab()
                            w = wsl[si]
                            cx.dma("sp", w[:, :, :], wvg.slab(which * 8 + h), reads=(wvg.b,), writes=(wsl_b[si],))
                            for tb in range(4):
                                i2 = pc[0] % 2
                                pc[0] += 1
                                vp = 5 + i2
                                for k in range(DC):
                                    mm(psb[vp][:, :], hbuf[:, k, tb * 128:(tb + 1) * 128], w[:, k, :], k == 0, k == DC - 1,
                                       reads=(wsl_b[si], h_b), writes=(psB[vp],))
                                if which == 0:
                                    act(lambda tb=tb, hh=hh, vp=vp: nc.scalar.copy(out=vsb[:, tb, hh, :], in_=psb[vp][:, :]),
                                        reads=(psB[vp],), writes=(v_b[tb][hh],))
                                else:
                                    act(lambda tb=tb, hh=hh, vp=vp: nc.scalar.activation(out=gsb[:, tb, hh, :], in_=psb[vp][:, :],
                                                                                        func=AF.Silu),
                                        reads=(psB[vp],), writes=(g_b[tb][hh],))
                    if g == 3 and t + 1 < NT:
                        norm_tile(t + 1, ls)
                    for tb in range(4):
                        tsl = slice(tb * 128, (tb + 1) * 128)
                        for hh in range(2):
                            h = 2 * g + hh
                            gam128 = float(np.exp(128.0 * np.log1p(-np.exp2(-5.0 - h))))
                            for half in range(2):
                                mm(psb[0][:, hh * 128:(hh + 1) * 128], kr[:, hh, half, tsl], qr[:, hh, half, tsl], half == 0, half == 1,
                                   reads=(kr_b[hh], qr_b[hh]), writes=(psB[0],))
                            dve(lambda hh=hh, h=h: nc.vector.tensor_tensor(out=pt[hh][:, :], in0=psb[0][:, hh * 128:(hh + 1) * 128],
                                                                          in1=decT(h), op=ALU.mult),
                                reads=(psB[0], consts_b), writes=(pt_b[hh],))
                            for half in range(2):
                                tr(pst[:, hh * 512 + half * 128: hh * 512 + (half + 1) * 128], kr[:, hh, half, tsl], ident_bf[:, :],
                                   reads=(kr_b[hh], consts_b), writes=(pst_b[hh],), tick=(half == 1))
                            dve(lambda hh=hh, h=h: nc.vector.tensor_scalar(out=ktm[hh][:, :], in0=pst[:, hh * 512: hh * 512 + 256],
                                                                          scalar1=sdv(h), scalar2=None, op0=ALU.mult),
                                reads=(pst_b[hh], consts_b), writes=(ktm_b[hh],))
                            for half in range(2):
                                pool(lambda hh=hh, h=h, half=half, tsl=tsl: nc.gpsimd.tensor_tensor(
                                    out=qs[hh][:, half, :], in0=qr[:, hh, half, tsl], in1=cdb(h), op=ALU.mult),
                                    reads=(qr_b[hh], consts_b), writes=(qs_b[hh],))
                            act(lambda hh=hh, h=h: nc.scalar.copy(out=sbf[hh][:, :, :], in_=stf[:, h, :, :]),
                                reads=(stf_b[h],), writes=(sbf_b[hh],))
                            op_ = 1 + hh
                            mm(psb[op_][:, :], pt[hh][:, :], vsb[:, tb, hh, :], True, False,
                               reads=(pt_b[hh], v_b[tb][hh]), writes=(psB[op_],))
                            mm(psb[op_][:, :], qs[hh][:, 0, :], sbf[hh][:, 0, :], False, False,
                               reads=(qs_b[hh], sbf_b[hh]), writes=(psB[op_],))
                            mm(psb[op_][:, :], qs[hh][:, 1, :], sbf[hh][:, 1, :], False, True,
                               reads=(qs_b[hh], sbf_b[hh]), writes=(psB[op_],))
                            for half in range(2):
                                nsb = 3 + 2 * hh + half
                                mm(psb[nsb][:, :], ktm[hh][:, half * 128:(half + 1) * 128], vsb[:, tb, hh, :], True, True,
                                   reads=(ktm_b[hh], v_b[tb][hh]), writes=(psB[nsb],))
                                dve(lambda h=h, half=half, nsb=nsb, gam128=gam128: nc.vector.scalar_tensor_tensor(
                                    out=stf[:, h, half, :], in0=stf[:, h, half, :], scalar=gam128, in1=psb[nsb][:, :],
                                    op0=ALU.mult, op1=ALU.add), reads=(psB[nsb], stf_b[h]), writes=(stf_b[h],))
                            dve(lambda hh=hh, op_=op_: nc.vector.bn_stats(out=st6[hh][:, :], in_=psb[op_][:, :]),
                                reads=(psB[op_],), writes=(mv_b[hh],))
                            dve(lambda hh=hh: nc.vector.bn_aggr(out=mv[hh][:, 0:2], in_=st6[hh][:, :]), reads=(mv_b[hh],), writes=(mv_b[hh],))
                            act(lambda hh=hh: nc.scalar.activation(out=mv[hh][:, 2:3], in_=mv[hh][:, 1:2], func=AF.Sqrt,
                                                                   bias=epsb[:, 0:1], scale=1.0),
                                reads=(mv_b[hh], consts_b), writes=(mv_b[hh],))
                            dve(lambda hh=hh: nc.vector.reciprocal(out=mv[hh][:, 2:3], in_=mv[hh][:, 2:3]), reads=(mv_b[hh],), writes=(mv_b[hh],))
                            dve(lambda hh=hh: nc.vector.scalar_tensor_tensor(out=mv[hh][:, 3:4], in0=mv[hh][:, 0:1], scalar=-1.0,
                                                                             in1=mv[hh][:, 2:3], op0=ALU.mult, op1=ALU.mult),
                                reads=(mv_b[hh],), writes=(mv_b[hh],))
                            act(lambda hh=hh, op_=op_: nc.scalar.activation(out=on[hh][:, :], in_=psb[op_][:, :], func=AF.Identity,
                                                                            bias=mv[hh][:, 3:4], scale=mv[hh][:, 2:3]),
                                reads=(psB[op_], mv_b[hh]), writes=(on_b[hh],))
                            pool(lambda hh=hh, tb=tb: nc.gpsimd.tensor_tensor(out=gsb[:, tb, hh, :], in0=on[hh][:, :],
                                                                              in1=gsb[:, tb, hh, :], op=ALU.mult),
                                 reads=(on_b[hh], g_b[tb][hh]), writes=(g_b[tb][hh],))
                    for tb in range(4):
                        for hh in range(2):
                            for q4 in range(4):
                                jc = hh * 4 + q4
                                tr(pst[:, jc * 128:(jc + 1) * 128], gsb[:, tb, hh, q4 * 128:(q4 + 1) * 128], ident_bf[:, :],
                                   reads=(g_b[tb][hh], consts_b), writes=(pst_b[hh],), tick=(q4 == 3))
                        act(lambda tb=tb: nc.scalar.copy(out=oT[:, :, tb * 128:(tb + 1) * 128],
                                                         in_=pst[:, :].rearrange("p (j n) -> p j n", j=8)),
                            reads=(pst_b[0], pst_b[1]), writes=(oT_b,))
                    for s2 in range(DC // 2):
                        si = next_slab()
                        w = wsl[si]
                        cx.dma("sp", w[:, 0:8, 0:256], wo.t[:, s2, 8 * g:8 * g + 8, :], reads=(wo.b,), writes=(wsl_b[si],))
                        for mi in range(2):
                            m = s2 * 2 + mi
                            yp = 5 + (m % 2)
                            for jj in range(8):
                                mm(psb[yp][:, :], w[:, jj, mi * 128:(mi + 1) * 128], oT[:, jj, :], jj == 0, jj == 7,
                                   reads=(wsl_b[si], oT_b), writes=(psB[yp],))
                            if g == 0:
                                act(lambda m=m, yp=yp: nc.scalar.copy(out=xy[:, m, :], in_=psb[yp][:, :]),
                                    reads=(psB[yp],), writes=(xy_b,))
                            else:
                                dve(lambda m=m, yp=yp: nc.vector.tensor_tensor(out=xy[:, m, :], in0=xy[:, m, :], in1=psb[yp][:, :],
                                                                               op=ALU.add), reads=(psB[yp], xy_b), writes=(xy_b,))
                defer_resid(t, ls)
            flush_resid()
            allb = (wsl_b + qr_b + kr_b + [b for r in v_b for b in r] + [b for r in g_b for b in r] + [oT_b, cs_b, consts_b]
                    + stf_b + sbf_b + rt_b + pt_b + ktm_b + qs_b + on_b + mv_b)
            for e in ("pe", "act", "dve", "sp", "pool"):
                cx.drain(e, allb)

    yb4 = [5, 6, 1, 2]

    tric_in = din("tric", [128, 128])

    class WSperm:
        def __init__(self, name, src):
            self.t = cx.dram(name, [128, 16, DC, 256], BF16)
            self.b = Buf(name)
            self.b.sem = es.enter_context(nc.semaphore("w_" + name))
            self.todo = []
            for k in range(DC):
                sv = src[k * 128:(k + 1) * 128, 0:4096].rearrange("p (s c hf f) -> p s c hf f", s=16, c=2, hf=2, f=64)
                for hf in range(2):
                    self.todo.append((self.t[:, :, k, hf * 128:(hf + 1) * 128].rearrange("p s (c f) -> p s c f", c=2),
                                      sv[:, :, :, hf, :]))
            self.b.w = (self.b.sem, 16 * len(self.todo))

        def issue(self, n):
            for _ in range(min(n, len(self.todo))):
                o, i = self.todo.pop(0)
                nc.gpsimd.dma_start(out=o, in_=i).then_inc(self.b.sem, 16)

        def slab(self, s):
            return self.t[:, s, :, :]

    def diff_weights(li, slot):
        return [WSperm("w_dqk%d" % li, diff_w_in[slot]), WS("w_dv%d" % li, diff_w_in[slot], D, 2048, 512, col0=4096),
                WS("w_do%d" % li, diff_w_out[slot], D, D, 256)]

    def diff_layer(li, slot, wts):
        wqk, wv, wo = wts
        ls = li * 2
        cosd, sind = tabs["d"]
        NBK = S // 128
        lam_init = 0.8 - 0.6 * math.exp(-0.3 * li)
        qT_d = cx.dram("dq%d" % li, [8, 2, 128, S], BF16)
        kT_d = cx.dram("dk%d" % li, [8, 2, 128, S], BF16)
        V_d = cx.dram("dv%d" % li, [S, 8, 256], BF16)
        oT_d = cx.dram("do%d" % li, [D, S], BF16)
        scr_b = Buf("dscr")
        with ExitStack() as fes:
            cs = [sbt(fes, "cs", [128, T], F32) for i in range(2)]
            cs_b = Buf("cs")
            wsl = [sbt(fes, "wsl", [128, DC, 512], BF16) for i in range(2)]
            wsl_b = [Buf("wsl") for i in range(2)]
            rt = [sbt(fes, "rt", [128, T], F32) for i in range(4)]
            rt_b = [Buf("rt") for i in range(4)]
            ro = [sbt(fes, "ro", [128, T], BF16) for i in range(4)]
            ro_b = [Buf("ro") for i in range(4)]
            vst = [sbt(fes, "vst", [128, 512], BF16) for i in range(2)]
            vst_b = [Buf("vst") for i in range(2)]
            sl = 0
            pc = 0
            rc = 0
            hA = [(hbuf0, hbuf0_b), (sbt(fes, "hb1", [128, DC, T], BF16), Buf("hb1"))]
            norm_tile(0, ls, hA[0])
            for t in range(NT):
                hbuf, h_b = hA[t % 2]
                cx.dma("sp", cs[0][:, :], cosd[:, t * T:(t + 1) * T], writes=(cs_b,))
                cx.dma("sp", cs[1][:, :], sind[:, t * T:(t + 1) * T], writes=(cs_b,))
                for h in range(8):
                    if h == 4 and t + 1 < NT:
                        norm_tile(t + 1, ls, hA[(t + 1) % 2])
                    for which in range(2):
                        sl += 1
                        si = sl % 2
                        w = wsl[si]
                        cx.dma("sp", w[:, :, 0:256], wqk.slab(which * 8 + h), reads=(wqk.b,), writes=(wsl_b[si],))
                        i2 = pc % 2
                        pc += 1
                        p1, p2 = 1 + i2, 3 + i2
                        for k in range(DC):
                            mm(psb[p1][:, :], w[:, k, 0:128], hbuf[:, k, :], k == 0, k == DC - 1, reads=(wsl_b[si], h_b), writes=(psB[p1],))
                        for k in range(DC):
                            mm(psb[p2][:, :], w[:, k, 128:256], hbuf[:, k, :], k == 0, k == DC - 1, reads=(wsl_b[si], h_b), writes=(psB[p2],))
                        dstd = qT_d if which == 0 else kT_d
                        for (pa, pb_, half, op_) in ((p1, p2, 0, ALU.subtract), (p2, p1, 1, ALU.add)):
                            a_, b_ = (0, 1) if half == 0 else (2, 3)
                            oi = rc % 4
                            rc += 1
                            dve(lambda pa=pa, a_=a_: nc.vector.tensor_tensor(out=rt[a_][:, :], in0=psb[pa][:, :], in1=cs[0][:, :], op=ALU.mult),
                                reads=(psB[pa], cs_b), writes=(rt_b[a_],))
                            dve(lambda pb_=pb_, b_=b_: nc.vector.tensor_tensor(out=rt[b_][:, :], in0=psb[pb_][:, :], in1=cs[1][:, :], op=ALU.mult),
                                reads=(psB[pb_], cs_b), writes=(rt_b[b_],))
                            pool(lambda oi=oi, a_=a_, b_=b_, op_=op_: nc.gpsimd.tensor_tensor(out=ro[oi][:, :], in0=rt[a_][:, :],
                                                                                             in1=rt[b_][:, :], op=op_),
                                 reads=(rt_b[a_], rt_b[b_]), writes=(ro_b[oi],))
                            cx.dma("pool", dstd[h, half, :, t * T:(t + 1) * T], ro[oi][:, :], reads=(ro_b[oi],), writes=(),
                                   sembuf=ro_b[oi])
                for s4 in range(4):
                    sl += 1
                    si = sl % 2
                    w = wsl[si]
                    cx.dma("sp", w[:, :, :], wv.slab(s4), reads=(wv.b,), writes=(wsl_b[si],))
                    for tb in range(4):
                        i2 = pc % 2
                        pc += 1
                        vp = 5 + i2
                        for k in range(DC):
                            mm(psb[vp][:, :], hbuf[:, k, tb * 128:(tb + 1) * 128], w[:, k, :], k == 0, k == DC - 1,
                               reads=(wsl_b[si], h_b), writes=(psB[vp],))
                        act(lambda i2=i2, vp=vp: nc.scalar.copy(out=vst[i2][:, :], in_=psb[vp][:, :]), reads=(psB[vp],), writes=(vst_b[i2],))
                        r0 = t * T + tb * 128
                        cx.dma("pool", V_d[r0:r0 + 128, 2 * s4:2 * s4 + 2, :], vst[i2][:, :].rearrange("p (h v) -> p h v", h=2),
                               reads=(vst_b[i2],), writes=(), sembuf=vst_b[i2])
            allb = wsl_b + rt_b + ro_b + vst_b + [cs_b, hA[1][1]]
            for e in ("pe", "act", "dve", "sp", "pool"):
                cx.drain(e, allb)
        with ExitStack() as fes:
            kT = sbt(fes, "kT", [128, 2, S], BF16)
            qT = sbt(fes, "qT", [128, 2, S], BF16)
            Vh = sbt(fes, "Vh", [128, NBK, 257], BF16)
            kqv_b = Buf("kqv")
            oTh = sbt(fes, "oTh", [128, 2, S], BF16)
            oTh_b = Buf("oTh")
            pT = [sbt(fes, "pT", [128, 512], BF16) for i in range(2)]
            pT_b = [Buf("pT") for i in range(2)]
            oc = [sbt(fes, "oc", [128, 4, 257], F32) for i in range(2)]
            oc_b = [[Buf("oc") for qb in range(4)] for i in range(2)]
            of = [sbt(fes, "of", [128, 256], F32) for i in range(2)]
            of_b = [Buf("of") for i in range(2)]
            ob = [sbt(fes, "ob", [128, 256], BF16) for i in range(2)]
            ob_b = [Buf("ob") for i in range(2)]
            junk = sbt(fes, "junk", [128, 256], F32)
            sm = [sbt(fes, "sm", [128, 8], F32) for i in range(2)]
            sm_b = [Buf("sm") for i in range(2)]
            tri = sbt(fes, "tri", [128, 128], BF16)
            trif = sbt(fes, "trif", [128, 128], F32)
            lamb = sbt(fes, "lamb", [128, 512], F32)
            lw = sbt(fes, "lw", [128, 256], F32)
            lam = sbt(fes, "lam", [128, 4], F32)
            sgl = sbt(fes, "sgl", [128, 256], F32)
            dc_b = Buf("dconst")
            cx.dma("sp", trif[:, :], tric_in[:, :], writes=(dc_b,))
            dve(lambda: nc.vector.tensor_copy(out=tri[:, :], in_=trif[:, :]), reads=(dc_b,), writes=(dc_b,))
            cx.dma("sp", lamb[:, :], diff_lambda[0:1, :].partition_broadcast(128), writes=(dc_b,))
            cx.dma("sp", sgl[:, :], diff_subln_g[0:1, :].partition_broadcast(128), writes=(dc_b,))
            dve(lambda: nc.vector.tensor_scalar(out=sgl[:, :], in0=sgl[:, :], scalar1=1.0 - lam_init, scalar2=None, op0=ALU.mult),
                reads=(dc_b,), writes=(dc_b,))
            for i in range(2):
                dve(lambda i=i: nc.vector.tensor_tensor(out=lw[:, i * 128:(i + 1) * 128], in0=lamb[:, i * 256:i * 256 + 128],
                                                        in1=lamb[:, i * 256 + 128:i * 256 + 256], op=ALU.mult),
                    reads=(dc_b,), writes=(dc_b,))
                dve(lambda i=i: nc.vector.tensor_reduce(out=lam[:, i:i + 1], in_=lw[:, i * 128:(i + 1) * 128], axis=AX.X, op=ALU.add),
                    reads=(dc_b,), writes=(dc_b,))
            act(lambda: nc.scalar.activation(out=lam[:, 0:2], in_=lam[:, 0:2], func=AF.Exp), reads=(dc_b,), writes=(dc_b,))
            dve(lambda: nc.vector.tensor_tensor(out=lam[:, 2:3], in0=lam[:, 1:2], in1=lam[:, 0:1], op=ALU.subtract),
                reads=(dc_b,), writes=(dc_b,))
            dve(lambda: nc.vector.tensor_scalar(out=lam[:, 2:3], in0=lam[:, 2:3], scalar1=-lam_init, scalar2=None, op0=ALU.add),
                reads=(dc_b,), writes=(dc_b,))
            dve(lambda: nc.vector.memset(Vh[:, :, 256:257], 1.0), writes=(kqv_b,))
            scale = float(DIFF_DH) ** -0.5
            NQ = S // 512
            kbi = 0
            fi = 0
            for h in range(8):
                for c in range(2):
                    for hf in range(2):
                        cx.dma("sp", kT[hf * 64:(hf + 1) * 64, c, :], kT_d[h, hf, c * 64:(c + 1) * 64, :], writes=(kqv_b,))
                        cx.dma("sp", qT[hf * 64:(hf + 1) * 64, c, :], qT_d[h, hf, c * 64:(c + 1) * 64, :], writes=(kqv_b,))
                cx.dma("sp", Vh[:, :, 0:256], V_d[:, h, :].rearrange("(b p) v -> p b v", p=128), writes=(kqv_b,))
                its = [(qt, c, kb) for qt in range(NQ) for c in range(2) for kb in range(4 * qt + 4)]
                slot = {}

                def front(i):
                    nonlocal kbi
                    qt, c, kb = its[i]
                    sp_ = 5 + (kbi % 2)
                    pi = kbi % 2
                    kbi += 1
                    slot[i] = pi
                    mm(psb[sp_][:, :], kT[:, c, kb * 128:(kb + 1) * 128], qT[:, c, qt * 512:(qt + 1) * 512], True, True,
                       reads=(kqv_b,), writes=(psB[sp_],))
                    act(lambda sp_=sp_, pi=pi: nc.scalar.activation(out=pT[pi][:, :], in_=psb[sp_][:, :], func=AF.Exp, scale=scale),
                        reads=(psB[sp_],), writes=(pT_b[pi],))
                    d = kb - 4 * qt
                    if d >= 0:
                        dve(lambda pi=pi, d=d: nc.vector.tensor_tensor(out=pT[pi][:, d * 128:(d + 1) * 128],
                                                                       in0=pT[pi][:, d * 128:(d + 1) * 128], in1=tri[:, :], op=ALU.mult),
                            reads=(pT_b[pi], dc_b), writes=(pT_b[pi],))

                def back(i):
                    nonlocal fi
                    qt, c, kb = its[i]
                    pi = slot.pop(i)
                    d = kb - 4 * qt
                    for qb in range(max(d, 0), 4):
                        mm(psb[1 + qb][:, 0:257], pT[pi][:, qb * 128:(qb + 1) * 128], Vh[:, kb, :], kb == 0, kb == 4 * qt + qb,
                           reads=(pT_b[pi], kqv_b), writes=(psB[1 + qb],))
                    if kb != 4 * qt + 3:
                        return
                    for qb in range(4):
                        act(lambda c=c, qb=qb: nc.scalar.copy(out=oc[c][:, qb, :], in_=psb[1 + qb][:, 0:257]),
                            reads=(psB[1 + qb],), writes=(oc_b[c][qb],))
                    if c != 1:
                        return
                    for qb in range(4):
                        f = fi % 2
                        fi += 1
                        s_ = sm[f]
                        dve(lambda s_=s_, qb=qb: nc.vector.reciprocal(out=s_[:, 0:1], in_=oc[0][:, qb, 256:257]),
                            reads=(oc_b[0][qb],), writes=(sm_b[f],))
                        dve(lambda s_=s_, qb=qb: nc.vector.reciprocal(out=s_[:, 1:2], in_=oc[1][:, qb, 256:257]),
                            reads=(oc_b[1][qb],), writes=(sm_b[f],))
                        dve(lambda s_=s_: nc.vector.tensor_tensor(out=s_[:, 1:2], in0=s_[:, 1:2], in1=lam[:, 2:3], op=ALU.mult),
                            reads=(sm_b[f], dc_b), writes=(sm_b[f],))
                        dve(lambda s_=s_, qb=qb, f=f: nc.vector.tensor_scalar(out=of[f][:, :], in0=oc[0][:, qb, 0:256], scalar1=s_[:, 0:1],
                                                                             scalar2=None, op0=ALU.mult),
                            reads=(oc_b[0][qb], sm_b[f]), writes=(of_b[f],))
                        dve(lambda s_=s_, qb=qb, f=f: nc.vector.scalar_tensor_tensor(out=of[f][:, :], in0=oc[1][:, qb, 0:256], scalar=s_[:, 1:2],
                                                                                    in1=of[f][:, :], op0=ALU.mult, op1=ALU.add),
                            reads=(oc_b[1][qb], sm_b[f], of_b[f]), writes=(of_b[f],))
                        act(lambda s_=s_, f=f: nc.scalar.activation(out=junk[:, :], in_=of[f][:, :], func=AF.Square, accum_out=s_[:, 2:3]),
                            reads=(of_b[f],), writes=(sm_b[f],))
                        act(lambda s_=s_: nc.scalar.activation(out=s_[:, 3:4], in_=s_[:, 2:3], func=AF.Sqrt, bias=epsb[:, 0:1], scale=1.0 / 256.0),
                            reads=(sm_b[f], consts_b), writes=(sm_b[f],))
                        dve(lambda s_=s_: nc.vector.reciprocal(out=s_[:, 3:4], in_=s_[:, 3:4]), reads=(sm_b[f],), writes=(sm_b[f],))
                        dve(lambda s_=s_, f=f: nc.vector.scalar_tensor_tensor(out=ob[f][:, :], in0=of[f][:, :], scalar=s_[:, 3:4], in1=sgl[:, :],
                                                                             op0=ALU.mult, op1=ALU.mult),
                            reads=(of_b[f], sm_b[f], dc_b), writes=(ob_b[f],))
                        for j in range(2):
                            tr(pst[:, f * 512 + j * 128: f * 512 + (j + 1) * 128], ob[f][:, j * 128:(j + 1) * 128], ident_bf[:, :],
                               reads=(ob_b[f], consts_b), writes=(pst_b[f],), tick=(j == 1))
                        q0 = qt * 512 + qb * 128
                        act(lambda f=f, q0=q0: nc.scalar.copy(out=oTh[:, :, q0:q0 + 128],
                                                              in_=pst[:, f * 512: f * 512 + 256].rearrange("p (j n) -> p j n", j=2)),
                            reads=(pst_b[f],), writes=(oTh_b,))

                front(0)
                for i in range(len(its)):
                    if i + 1 < len(its):
                        front(i + 1)
                    back(i)
                cx.dma("pool", oT_d[h * 256:(h + 1) * 256, :].rearrange("(j p) n -> p j n", p=128), oTh[:, :, :],
                       reads=(oTh_b,), writes=(), sembuf=oTh_b)
            allb = [kqv_b, oTh_b, dc_b] + pT_b + [b for r in oc_b for b in r] + of_b + ob_b + sm_b
            for e in ("pe", "act", "dve", "sp", "pool"):
                cx.drain(e, allb)
        with ExitStack() as fes:
            wsl = [sbt(fes, "wsl", [128, DC, 256], BF16) for i in range(2)]
            wsl_b = [Buf("wsl") for i in range(2)]
            hC = [(hbuf0, hbuf0_b), (sbt(fes, "hc1", [128, DC, T], BF16), Buf("hc1"))]
            for t in range(NT):
                hbuf, h_b = hC[t % 2]
                cx.dma("sp", hbuf[:, :, :], oT_d[:, t * T:(t + 1) * T].rearrange("(c p) n -> p c n", p=128), writes=(h_b,))
                for s2 in range(DC // 2):
                    si = s2 % 2
                    cx.dma("sp", wsl[si][:, :, :], wo.slab(s2), reads=(wo.b,), writes=(wsl_b[si],))
                    for mi in range(2):
                        m = s2 * 2 + mi
                        yp = yb4[m % 4]
                        for k in range(DC):
                            mm(psb[yp][:, :], wsl[si][:, k, mi * 128:(mi + 1) * 128], hbuf[:, k, :], k == 0, k == DC - 1,
                               reads=(wsl_b[si], h_b), writes=(psB[yp],))
                        act(lambda m=m, yp=yp: nc.scalar.copy(out=xy[:, m, :], in_=psb[yp][:, :]), reads=(psB[yp],), writes=(xy_b,))
                resid_tile(t, ls)
            for e in ("pe", "act", "dve", "sp", "pool"):
                cx.drain(e, wsl_b + [hC[1][1]])


    def ffn_layer(li, win, wdn):
        ls = li * 2 + 1
        with ExitStack() as fes:
            NB = 2
            wg = [fes.enter_context(nc.sbuf_tensor("wg%d_%d" % (li, i), [128, DC, 256], BF16)) for i in range(NB)]
            wu = [fes.enter_context(nc.sbuf_tensor("wu%d_%d" % (li, i), [128, DC, 256], BF16)) for i in range(NB)]
            wg_b = [Buf("wg%d" % i) for i in range(NB)]
            wu_b = [Buf("wu%d" % i) for i in range(NB)]
            wd = [fes.enter_context(nc.sbuf_tensor("wd%d_%d" % (li, i), [128, FC // 2, 256], BF16)) for i in range(3)]
            wd_b = [Buf("wd%d" % i) for i in range(3)]
            abuf = fes.enter_context(nc.sbuf_tensor("abuf%d" % li, [128, FC, T], BF16))
            a_b = [Buf("a%d" % j) for j in range(FC)]
            gb = [fes.enter_context(nc.sbuf_tensor("gb%d_%d" % (li, i), [128, T + 2], F32)) for i in range(2)]
            gb_b = [Buf("gb%d" % i) for i in range(2)]
            sg = [fes.enter_context(nc.sbuf_tensor("sg%d_%d" % (li, i), [128, T], BF16)) for i in range(2)]
            sg_b = [Buf("sg%d" % i) for i in range(2)]
            halo = fes.enter_context(nc.sbuf_tensor("halo%d" % li, [128, FC, 2], F32))
            halo_b = Buf("halo")
            dve(lambda: nc.vector.memset(halo[:, :, :], 0.0), writes=(halo_b,))
            yb = [5, 6, 1, 2]

            def tap(tp, j):
                i = (li * 3 + tp) * FC + j
                return fdwT[:, i:i + 1]

            norm_tile(0, ls)
            hbuf, h_b = hbuf0, hbuf0_b
            for t in range(NT):
                for s2 in range(FC // 2):
                    if s2 == 2:
                        flush_resid()
                    i3 = s2 % NB
                    cx.dma("sp", wg[i3][:, :, :], win.slab(s2), reads=(win.b,), writes=(wg_b[i3],))
                    cx.dma("sp", wu[i3][:, :, :], win.slab(FC // 2 + s2), reads=(win.b,), writes=(wu_b[i3],))
                    for jj in range(2):
                        j = s2 * 2 + jj
                        i2 = j % 2
                        gp, up = 1 + i2, 3 + i2
                        for k in range(DC):
                            mm(psb[gp][:, :], wg[i3][:, k, jj * 128:(jj + 1) * 128], hbuf[:, k, :], k == 0, k == DC - 1,
                               reads=(wg_b[i3], h_b), writes=(psB[gp],))
                        for k in range(DC):
                            mm(psb[up][:, :], wu[i3][:, k, jj * 128:(jj + 1) * 128], hbuf[:, k, :], k == 0, k == DC - 1,
                               reads=(wu_b[i3], h_b), writes=(psB[up],))
                        g = gb[i2]
                        act(lambda g=g, gp=gp: nc.scalar.copy(out=g[:, 2:T + 2], in_=psb[gp][:, :]),
                            reads=(psB[gp],), writes=(gb_b[i2],))
                        act(lambda g=g, j=j: nc.scalar.copy(out=g[:, 0:2], in_=halo[:, j, :]),
                            reads=(halo_b,), writes=(gb_b[i2],))
                        act(lambda gp=gp, i2=i2, j=j: nc.scalar.activation(out=tmpf[i2][:, :], in_=psb[gp][:, :],
                                                                           func=AF.Identity, scale=tap(2, j)),
                            reads=(psB[gp], consts_b), writes=(tmpf_b[i2],))
                        dve(lambda g=g, i2=i2, j=j: nc.vector.scalar_tensor_tensor(
                            out=tmpf[i2][:, :], in0=g[:, 1:T + 1], scalar=tap(1, j), in1=tmpf[i2][:, :],
                            op0=ALU.mult, op1=ALU.add), reads=(gb_b[i2], tmpf_b[i2], consts_b), writes=(tmpf_b[i2],))
                        dve(lambda g=g, i2=i2, j=j: nc.vector.scalar_tensor_tensor(
                            out=tmpf[i2][:, :], in0=g[:, 0:T], scalar=tap(0, j), in1=tmpf[i2][:, :],
                            op0=ALU.mult, op1=ALU.add), reads=(gb_b[i2], tmpf_b[i2], consts_b), writes=(tmpf_b[i2],))
                        dve(lambda g=g, j=j: nc.vector.tensor_copy(out=halo[:, j, :], in_=g[:, T:T + 2]),
                            reads=(gb_b[i2],), writes=(halo_b,))
                        act(lambda i2=i2: nc.scalar.activation(out=sg[i2][:, :], in_=tmpf[i2][:, :], func=AF.Silu),
                            reads=(tmpf_b[i2],), writes=(sg_b[i2],))
                        dve(lambda i2=i2, up=up, j=j: nc.vector.tensor_tensor(out=abuf[:, j, :], in0=sg[i2][:, :],
                                                                             in1=psb[up][:, :], op=ALU.mult),
                            reads=(sg_b[i2], psB[up]), writes=(a_b[j],))
                pc = 0
                for s2 in range(DC // 2):
                    for half in range(2):
                        if pc == 3 and t + 1 < NT:
                            norm_tile(t + 1, ls)
                        w3 = pc % 3
                        pc += 1
                        cx.dma("sp", wd[w3][:, :, :], wdn.t[:, s2, half * 22:(half + 1) * 22, :], reads=(wdn.b,),
                               writes=(wd_b[w3],))
                        for mi in range(2):
                            m = s2 * 2 + mi
                            yp = yb[m % 4]
                            for jj in range(22):
                                j = half * 22 + jj
                                mm(psb[yp][:, :], wd[w3][:, jj, mi * 128:(mi + 1) * 128], abuf[:, j, :], j == 0, j == FC - 1,
                                   reads=(wd_b[w3], a_b[j]), writes=(psB[yp],), tick=(jj == 21))
                            if half == 1:
                                act(lambda m=m, yp=yp: nc.scalar.copy(out=xy[:, m, :], in_=psb[yp][:, :]),
                                    reads=(psB[yp],), writes=(xy_b,))
                defer_resid(t, ls)
            flush_resid()
            allb = wg_b + wu_b + wd_b + a_b + gb_b + sg_b + [halo_b]
            for e in ("pe", "act", "dve", "sp", "pool"):
                cx.drain(e, allb)

    subs = []
    for (kind, slot, li) in layer_kinds:
        if with_mixer and kind == 0:
            w = [WS("w_rqk%d" % li, ret_w_in[slot], D, 4096, 256), WS("w_rvg%d" % li, ret_w_in[slot], D, 8192, 512, col0=4096),
                 WS("w_ro%d" % li, ret_w_out[slot], 4096, D, 256)]
            subs.append((lambda li=li, slot=slot, w=w: ret_layer(li, slot, w[0], w[1], w[2]), w))
        if with_mixer and kind == 1:
            w = [WS("w_c1_%d" % li, conv_w_pw1[slot], D, 2 * D, 256), WS("w_c2_%d" % li, conv_w_pw2[slot], D, D, 256)]
            subs.append((lambda li=li, slot=slot, w=w: conv_layer(li, slot, w[0], w[1]), w))
        if with_mixer and kind == 2:
            w = diff_weights(li, slot)
            subs.append((lambda li=li, slot=slot, w=w: diff_layer(li, slot, w), w))
        if with_ffn:
            w = [WS("w_fin%d" % li, ffn_w_in[li], D, 2 * DFF, 256), WS("w_fdn%d" % li, ffn_w_down[li], DFF, D, 256)]
            subs.append((lambda li=li, w=w: ffn_layer(li, w[0], w[1]), w))
    for w in subs[0][1]:
        w.issue(10 ** 9)
    for i, (run, _) in enumerate(subs):
        nxt = subs[i + 1][1] if i + 1 < len(subs) else []
        tot = sum(len(w.todo) for w in nxt)
        per = -(-tot // NT) if tot else 0

        def hook(t, nxt=nxt, per=per):
            n = per
            for w in nxt:
                k = min(n, len(w.todo))
                w.issue(k)
                n -= k
        tile_hook[0] = hook
        run()
        for w in nxt:
            w.issue(10 ** 9)
    tile_hook[0] = None

    with ExitStack() as tes:
        ob4 = tes.enter_context(nc.sbuf_tensor("ob4", [128, 4, D], F32))
        ob_b = Buf("ob4")
        for t in range(NT):
            load_x_tile(t, xy, xy_b)
            for b in range(4):
                for q in range(4):
                    pb = 1 + (q % 2)
                    for cc in range(4):
                        c = q * 4 + cc
                        tr(psb[pb][:, cc * 128:(cc + 1) * 128], xy[:, c, b * 128:(b + 1) * 128], ident[:, :],
                           reads=(xy_b, consts_b), writes=(psB[pb],), tick=(cc == 3))
                    act(lambda b=b, q=q, pb=pb: nc.scalar.copy(out=ob4[:, b, q * 512:(q + 1) * 512], in_=psb[pb][:, :]),
                        reads=(psB[pb],), writes=(ob_b,))
            cx.dma("pool", out_d[t * T:(t + 1) * T, :].rearrange("(b p) f -> p b f", p=128), ob4[:, :, :],
                   reads=(ob_b,), writes=(), sembuf=ob_b)
        cx.drain("pool", [ob_b])
        cx.drain("sp", [ob_b])
        cx.drain("act", [ob_b])
    es.close()
    return nc


LAYERS = [(i % 3, i // 3, i) for i in range(DEPTH)]


def _ropec():
    o = np.zeros((128, 4), np.float32)
    p = np.arange(128)
    o[:, 0] = (np.float32(ROPE_THETA) ** (-(2 * p).astype(np.float32) / np.float32(256))).astype(np.float32)
    o[:, 1] = (np.float32(ROPE_THETA) ** (-(2 * (p % 64)).astype(np.float32) / np.float32(128))).astype(np.float32)
    return o


def _retc():
    o = np.zeros((128, 8 * 128 * 2 + 8), np.float32)
    idx = np.arange(128, dtype=np.float64)
    for h in range(8):
        lg = np.log1p(-np.exp2(-5.0 - h))
        rel = idx[None, :] - idx[:, None]
        o[:, h * 128:(h + 1) * 128] = np.where(rel >= 0, np.exp(np.maximum(rel, 0) * lg), 0.0) / 16.0
        o[:, 1024 + h * 128:1024 + (h + 1) * 128] = np.exp((idx + 1.0) * lg)[None, :]
        o[:, 2048 + h] = np.exp((127.0 - idx) * lg) / 16.0
    return o


def make_in_map(b, S, x, c, positions, mod_w, mod_b, norm_g, ret_w_in, ret_w_out, conv_w_pw1, conv_b_pw1,
                conv_w_dw, conv_b_dw, conv_ln_g, conv_ln_b, conv_w_pw2, conv_b_pw2, diff_w_in, diff_lambda,
                diff_subln_g, diff_w_out, ffn_w_in, ffn_w_dw, ffn_w_down):
    f = lambda a: np.ascontiguousarray(np.asarray(a, dtype=np.float32))
    return {
        "x": f(x[b, :S]),
        "c": f(c[b]).reshape(DC, 128),
        "positions": np.ascontiguousarray(np.asarray(positions[b, :S], dtype=np.int32)).reshape(1, S),
        "mod_w": f(mod_w), "mod_b": f(mod_b).reshape(DEPTH * 2, 3 * D),
        "norm_g": f(norm_g).reshape(DEPTH * 4 * DC, 128),
        "ret_w_in": f(ret_w_in), "ret_w_out": f(ret_w_out),
        "conv_w_pw1": f(conv_w_pw1), "conv_b_pw1": f(conv_b_pw1).reshape(32, 128),
        "conv_w_dw": f(conv_w_dw).reshape(CONV_W * DC, 128),
        "conv_vecs": np.concatenate([f(conv_b_dw).reshape(DC, 128), f(conv_ln_g).reshape(DC, 128),
                                     f(conv_ln_b).reshape(DC, 128), f(conv_b_pw2).reshape(DC, 128)], axis=0),
        "conv_w_pw2": f(conv_w_pw2),
        "diff_w_in": f(diff_w_in), "diff_lambda": f(diff_lambda).reshape(1, 512),
        "diff_subln_g": f(diff_subln_g).reshape(1, 256), "diff_w_out": f(diff_w_out),
        "ffn_w_in": f(ffn_w_in), "ffn_w_dw": f(ffn_w_dw).reshape(DEPTH * 3 * FC, 128),
        "ffn_w_down": f(ffn_w_down),
        "identc": np.eye(128, dtype=np.float32),
        "ropec": _ropec(), "retc": _retc(),
        "tric": np.triu(np.ones((128, 128), np.float32)),
    }


def kernel(**inputs):
    S = 4096
    B = 8
    nc = build(S, LAYERS)
    in_maps = [make_in_map(b, S, **inputs) for b in range(B)]
    res = run_bass_kernel_spmd(nc, in_maps, core_ids=list(range(B)))
    return np.stack([np.asarray(r["out"], dtype=np.float32) for r in res.results], axis=0)
```
